# Optimizing a Trainium2 kernel written in Bass

```python
import math
import jax, jax.numpy as jnp
from jax import lax
import numpy as np

D_MODEL = 1024
BATCH = 8
SEQ = 4096
DEPTH = 2

GLA_HEADS = 4
GLA_DK = D_MODEL // 2
GLA_DV = D_MODEL
GLA_DKH = GLA_DK // GLA_HEADS
GLA_DVH = GLA_DV // GLA_HEADS
GATE_RANK = 16
GATE_TAU = 16.0
CHUNK = 64
CONV_DIM = D_MODEL
CONV_WIDTH = 3
IN_SIZES = (GLA_DK, GLA_DK, GLA_DV, GATE_RANK, GLA_DV, CONV_DIM, CONV_DIM, CONV_DIM, D_MODEL, D_MODEL)
IN_COLS = sum(IN_SIZES)
IN_SPLITS = tuple(int(s) for s in np.cumsum(IN_SIZES)[:-1])
N_GROUPS = 8
EXPERTS_PER_GROUP = 8
N_EXPERTS = N_GROUPS * EXPERTS_PER_GROUP
TOP_K = 2
D_EXPERT = D_MODEL // 2
MOE_BLOCK = 256
EPS = 1e-6

kernel_name = "hybrid_gla_shortconv_hiermoe_adaln"


def rmsnorm(x, g):
    xf = x.astype(jnp.float32)
    y = xf * lax.rsqrt(jnp.mean(xf * xf, axis=-1, keepdims=True) + EPS)
    return (y * g.astype(jnp.float32)).astype(x.dtype)


def gla_chunked(q, k, v, log_a):
    Bsz, S, H, dk = q.shape
    dv = v.shape[-1]
    n = S // CHUNK

    def to_chunks(t):
        return t.reshape(Bsz, n, CHUNK, H, t.shape[-1]).transpose(1, 0, 3, 2, 4)

    qc, kc, vc, gc = to_chunks(q), to_chunks(k), to_chunks(v), to_chunks(log_a)
    b = jnp.cumsum(gc, axis=3)
    b_last = b[:, :, :, -1:, :]
    q_t = qc * jnp.exp(b)
    k_t = kc * jnp.exp(-b)
    k_s = kc * jnp.exp(b_last - b)
    causal = jnp.tril(jnp.ones((CHUNK, CHUNK), dtype=bool))
    attn = jnp.einsum('nbhid,nbhjd->nbhij', q_t, k_t)
    attn = jnp.where(causal, attn, 0.0)
    o_intra = jnp.einsum('nbhij,nbhjv->nbhiv', attn, vc)
    decay = jnp.exp(b_last)[:, :, :, 0, :]

    def step(state, inp):
        q_i, k_i, v_i, d_i = inp
        o_i = jnp.einsum('bhid,bhdv->bhiv', q_i, state)
        state = d_i[..., None] * state + jnp.einsum('bhid,bhiv->bhdv', k_i, v_i)
        return state, o_i

    s0 = jnp.zeros((Bsz, H, dk, dv), jnp.float32)
    _, o_inter = lax.scan(step, s0, (q_t, k_s, vc, decay))
    o = o_intra + o_inter
    return o.transpose(1, 0, 3, 2, 4).reshape(Bsz, S, H, dv)


def causal_short_conv(u, w):
    S = u.shape[1]
    up = jnp.pad(u, ((0, 0), (CONV_WIDTH - 1, 0), (0, 0)))
    return sum(w[i] * up[:, i:i + S] for i in range(CONV_WIDTH))


def token_mixer(h, w_in, gate_w2, gate_b, gla_norm_g, conv_w, w_gla_out, w_conv_out, w_out):
    Bsz, S, _ = h.shape
    z = h @ w_in
    q, k, v, g_dn, r, conv_b, conv_c, conv_h, gate_a, gate_c = jnp.split(z, IN_SPLITS, axis=-1)
    f32 = jnp.float32
    q = q.astype(f32).reshape(Bsz, S, GLA_HEADS, GLA_DKH) * (GLA_DKH ** -0.5)
    k = k.astype(f32).reshape(Bsz, S, GLA_HEADS, GLA_DKH)
    v = v.astype(f32).reshape(Bsz, S, GLA_HEADS, GLA_DVH)
    log_a = jax.nn.log_sigmoid((g_dn @ gate_w2 + gate_b).astype(f32)) / GATE_TAU
    log_a = log_a.reshape(Bsz, S, GLA_HEADS, GLA_DKH)
    o = gla_chunked(q, k, v, log_a)
    o = rmsnorm(o, gla_norm_g.reshape(GLA_HEADS, GLA_DVH)).reshape(Bsz, S, GLA_DV)
    o = o.astype(h.dtype) * jax.nn.silu(r)
    y_gla = o @ w_gla_out
    conv_out = causal_short_conv(conv_c * conv_h, conv_w)
    y_conv = (conv_b * conv_out) @ w_conv_out
    y = jax.nn.sigmoid(gate_a) * y_gla + jax.nn.sigmoid(gate_c) * y_conv
    return y @ w_out


def hier_moe(h, rg_w, rg_b, re_w, re_b, w1, w3, w2):
    Bsz, S, D = h.shape
    N = Bsz * S
    NK = N * TOP_K
    t = h.reshape(N, D)
    p_group = jax.nn.softmax((t @ rg_w + rg_b).astype(jnp.float32), axis=-1)
    g_w, g_idx = lax.top_k(p_group, 1)
    e_logits = (t @ re_w + re_b).astype(jnp.float32).reshape(N, N_GROUPS, EXPERTS_PER_GROUP)
    e_logits = jnp.take_along_axis(e_logits, g_idx[:, :, None], axis=1)[:, 0]
    p_exp = jax.nn.softmax(e_logits, axis=-1)
    e_w, e_loc = lax.top_k(p_exp, TOP_K)
    e_w = e_w / jnp.sum(e_w, axis=-1, keepdims=True) * g_w
    e_id = g_idx * EXPERTS_PER_GROUP + e_loc
    flat_e = e_id.reshape(NK)
    flat_w = e_w.reshape(NK)
    flat_tok = jnp.repeat(jnp.arange(N, dtype=jnp.int32), TOP_K)
    order = jnp.argsort(flat_e)
    se = flat_e[order]
    counts = jnp.bincount(flat_e, length=N_EXPERTS).astype(jnp.int32)
    padded = ((counts + MOE_BLOCK - 1) // MOE_BLOCK) * MOE_BLOCK
    pad_end = jnp.cumsum(padded)
    pad_start = pad_end - padded
    start = jnp.cumsum(counts) - counts
    dest = pad_start[se] + (jnp.arange(NK, dtype=jnp.int32) - start[se])
    n_blocks = (NK + MOE_BLOCK - 1) // MOE_BLOCK + N_EXPERTS
    P = n_blocks * MOE_BLOCK
    buf_tok = jnp.full((P,), N, jnp.int32).at[dest].set(flat_tok[order])
    buf_w = jnp.zeros((P,), h.dtype).at[dest].set(flat_w[order].astype(h.dtype))
    blk_e = jnp.searchsorted(pad_end, jnp.arange(n_blocks, dtype=jnp.int32) * MOE_BLOCK, side='right')
    blk_e = jnp.minimum(blk_e, N_EXPERTS - 1)
    t_pad = jnp.concatenate([t, jnp.zeros((1, D), t.dtype)], axis=0)
    xb = t_pad[buf_tok].reshape(n_blocks, MOE_BLOCK, D)

    def expert_block(args):
        xblk, e = args
        return (jax.nn.silu(xblk @ w1[e]) * (xblk @ w3[e])) @ w2[e]

    yb = lax.map(expert_block, (xb, blk_e)).reshape(P, D)
    out = jnp.zeros((N + 1, D), h.dtype).at[buf_tok].add(yb * buf_w[:, None])[:N]
    return out.reshape(Bsz, S, D)


def setup_inputs(seed: int = 0) -> dict:
    key = jax.random.key(seed)
    ks = jax.random.split(key, 24)
    D, L = D_MODEL, DEPTH

    def nrm(k, shape, scale):
        return jax.random.normal(k, shape, jnp.float32) * scale

    return {
        "x": nrm(ks[0], (BATCH, SEQ, D), 1.0),
        "c": nrm(ks[1], (BATCH, D), 1.0),
        "mod_w": nrm(ks[2], (L, D, 6 * D), 0.5 * D ** -0.5),
        "mod_b": nrm(ks[3], (L, 6 * D), 0.02),
        "norm1_g": 1.0 + nrm(ks[4], (L, D), 0.02),
        "w_in": nrm(ks[5], (L, D, IN_COLS), D ** -0.5),
        "gate_w2": nrm(ks[6], (L, GATE_RANK, GLA_DK), GATE_RANK ** -0.5),
        "gate_b": nrm(ks[7], (L, GLA_DK), 0.1),
        "gla_norm_g": 1.0 + nrm(ks[8], (L, GLA_DV), 0.02),
        "conv_w": nrm(ks[9], (L, CONV_WIDTH, CONV_DIM), CONV_WIDTH ** -0.5),
        "w_gla_out": nrm(ks[10], (L, GLA_DV, D), GLA_DV ** -0.5),
        "w_conv_out": nrm(ks[11], (L, CONV_DIM, D), CONV_DIM ** -0.5),
        "w_out": nrm(ks[12], (L, D, D), D ** -0.5),
        "norm2_g": 1.0 + nrm(ks[13], (L, D), 0.02),
        "router_group_w": nrm(ks[14], (L, D, N_GROUPS), D ** -0.5),
        "router_group_b": nrm(ks[15], (L, N_GROUPS), 0.01),
        "router_expert_w": nrm(ks[16], (L, D, N_EXPERTS), D ** -0.5),
        "router_expert_b": nrm(ks[17], (L, N_EXPERTS), 0.01),
        "expert_w1": nrm(ks[18], (L, N_EXPERTS, D, D_EXPERT), D ** -0.5),
        "expert_w3": nrm(ks[19], (L, N_EXPERTS, D, D_EXPERT), D ** -0.5),
        "expert_w2": nrm(ks[20], (L, N_EXPERTS, D_EXPERT, D), D_EXPERT ** -0.5),
        "final_norm_g": 1.0 + nrm(ks[21], (D,), 0.02),
    }


def reference(x, c, mod_w, mod_b, norm1_g, w_in, gate_w2, gate_b, gla_norm_g, conv_w,
              w_gla_out, w_conv_out, w_out, norm2_g, router_group_w, router_group_b,
              router_expert_w, router_expert_b, expert_w1, expert_w3, expert_w2, final_norm_g):
    for l in range(DEPTH):
        mod = jax.nn.silu(c) @ mod_w[l] + mod_b[l]
        sh1, sc1, g1, sh2, sc2, g2 = jnp.split(mod, 6, axis=-1)
        h = rmsnorm(x, norm1_g[l]) * (1.0 + sc1[:, None]) + sh1[:, None]
        y = token_mixer(h, w_in[l], gate_w2[l], gate_b[l], gla_norm_g[l], conv_w[l],
                        w_gla_out[l], w_conv_out[l], w_out[l])
        x = x + g1[:, None] * y
        h = rmsnorm(x, norm2_g[l]) * (1.0 + sc2[:, None]) + sh2[:, None]
        y = hier_moe(h, router_group_w[l], router_group_b[l], router_expert_w[l],
                     router_expert_b[l], expert_w1[l], expert_w3[l], expert_w2[l])
        x = x + g2[:, None] * y
    return rmsnorm(x, final_norm_g)
```

```python
import numpy as np
import concourse.bass as bass
import concourse.mybir as mybir
from concourse.bass_utils import run_bass_kernel_spmd

F32 = mybir.dt.float32
BF16 = mybir.dt.bfloat16
I32 = mybir.dt.int32
ALU = mybir.AluOpType
AF = mybir.ActivationFunctionType
AX = mybir.AxisListType

D = 1024
SEQ = 4096
NTILE = SEQ // 128
DEPTH = 2
NE = 64
DEXP = 512
SROWS = 256
NBLK = 96
NSLOT = NBLK * SROWS
EPS = 1e-6
OOB_BIG = 16777216.0
OFF = dict(q=0, k=512, v=1024, gdn=2048, r=2064, cb=3088, cc=4112, ch=5136, ga=6160, gc=7184)
COMPUTE = ("pe", "act", "dve", "pool")


class Buf:
    __slots__ = ("name", "writer", "readers", "wsem", "wcnt", "rsem", "rcnt", "t")

    def __init__(self, name, t=None):
        self.name = name
        self.t = t
        self.writer = None
        self.readers = {}
        self.wsem = None
        self.wcnt = 0
        self.rsem = None
        self.rcnt = 0

    def __getitem__(self, idx):
        return self.t[idx]


class Sched:
    def __init__(self, nc, strict=True):
        self.nc = nc
        self.strict = strict
        self.eng = {"pe": nc.tensor, "act": nc.scalar, "dve": nc.vector,
                    "pool": nc.gpsimd, "sp": nc.sync}
        self.prog = {k: [] for k in self.eng}
        self.cnt = {k: 0 for k in COMPUTE}
        self.sem = {}
        self.seen = {k: {} for k in self.eng}
        self.stack = []
        self.serial = False
        self.dsem = {}
        for k in COMPUTE:
            self.sem[k] = self.new_sem("c_" + k)
        self.nps = 0
        self.npb = 0

    def new_sem(self, name):
        cm = self.nc.semaphore(name)
        s = cm.__enter__()
        self.stack.append(cm)
        return s

    def sbuf(self, name, shape, dt):
        cm = self.nc.sbuf_tensor(name, list(shape), dt)
        t = cm.__enter__()
        self.stack.append(cm)
        return Buf(name, t)

    def psum(self, name, shape, dt):
        cm = self.nc.psum_tensor(name, list(shape), dt)
        t = cm.__enter__()
        self.stack.append(cm)
        return Buf(name, t)

    def _wait(self, q, semkey, sem, val):
        seen = self.seen[q]
        if seen.get(semkey, 0) >= val:
            return
        seen[semkey] = val
        e = self.eng[q]
        self.prog[q].append(lambda e=e, sem=sem, val=val: (e.wait_ge(sem, val), None)[1])

    def _dep(self, q, d):
        if d is None:
            return
        if d[0] == "eng":
            _, k, c = d
            if k == q and (k == "pe" or not self.strict):
                return
            self._wait(q, k, self.sem[k], c)
        else:
            sem, n = self.dsem[d[1]]
            self._wait(q, d[1], sem, 16 * n)

    @staticmethod
    def _addreader(b, tag):
        b.readers[tag[1]] = tag

    def _serial(self, q):
        for k in COMPUTE:
            if self.cnt[k] > 0 and k != q:
                self._wait(q, k, self.sem[k], self.cnt[k])
        for key, (sem, n) in self.dsem.items():
            self._wait(q, key, sem, 16 * n)

    def op(self, q, fn, reads=(), writes=()):
        if self.serial:
            self._serial(q)
        for b in reads:
            self._dep(q, b.writer)
        for b in writes:
            self._dep(q, b.writer)
            for r in b.readers.values():
                self._dep(q, r)
        self.cnt[q] += 1
        c = self.cnt[q]
        sem = self.sem[q]
        self.prog[q].append(lambda fn=fn, sem=sem: fn().then_inc(sem, 1))
        tag = ("eng", q, c)
        for b in reads:
            self._addreader(b, tag)
        for b in writes:
            b.writer = tag
            b.readers = {}

    def dma(self, q, fn, reads=(), writes=(), part=False):
        if self.serial:
            self._serial(q)
        for b in reads:
            self._dep(q, b.writer)
        for b in writes:
            if not (part and b.writer is not None and b.writer[0] == "dma"):
                self._dep(q, b.writer)
            for r in b.readers.values():
                self._dep(q, r)
        if writes and writes[0].t is not None:
            o = writes[0]
            key = "w_" + o.name
        else:
            o = [b for b in reads if b.t is not None][0]
            key = "r_" + o.name
        if key not in self.dsem:
            self.dsem[key] = [self.new_sem(key), 0]
        ent = self.dsem[key]
        ent[1] += 1
        sem = ent[0]
        self.prog[q].append(lambda fn=fn, sem=sem: fn().then_inc(sem, 16))
        tag = ("dma", key)
        for b in reads:
            self._addreader(b, tag)
        for b in writes:
            b.writer = tag
            b.readers = {}
        return tag

    def barrier(self):
        for q in self.eng:
            for k in COMPUTE:
                if self.cnt[k] > 0 and (k != q or (self.strict and k != "pe")):
                    self._wait(q, k, self.sem[k], self.cnt[k])
            for key, (sem, n) in self.dsem.items():
                self._wait(q, key, sem, 16 * n)

    def raw(self, q, fn):
        self.prog[q].append(lambda fn=fn: (fn(), None)[1])

    def wait_for(self, q, bufs):
        for b in bufs:
            self._dep(q, b.writer)
            for r in b.readers.values():
                self._dep(q, r)

    def emit(self):
        with self.nc.Block() as block:
            for k, attr in (("sp", "sync"), ("act", "scalar"), ("dve", "vector"),
                            ("pool", "gpsimd"), ("pe", "tensor")):
                lst = self.prog[k]

                def body(_e, lst=lst):
                    for f in lst:
                        f()
                getattr(block, attr)(body)

    def close(self):
        while self.stack:
            self.stack.pop().__exit__(None, None, None)


class K:
    def __init__(self, nlayers=DEPTH, ntile=NTILE, stages=("A", "B", "M"), dbg=None, strict=True, serial=False):
        self.nlayers = nlayers
        self.NT = ntile
        self.stages = stages
        self.dbg = dbg or {}
        nc = self.nc = bass.Bass("TRN2", target_bir_lowering=False)
        S = self.S = Sched(nc, strict=strict)
        S.serial = serial
        L = DEPTH

        def inp(name, shape, dt=F32):
            return nc.dram_tensor(name, list(shape), dt, kind="ExternalInput").ap()
        self.x = inp("x", [SEQ, D])
        self.c = inp("c", [1, D])
        self.mod_w = inp("mod_w", [L, D, 6 * D])
        self.mod_b = inp("mod_b", [L, 6 * D])
        self.norm1_g = inp("norm1_g", [L, D])
        self.w_in = inp("w_in", [L, D, 8208])
        self.gate_w2 = inp("gate_w2", [L, 16, 512])
        self.gate_b = inp("gate_b", [L, 512])
        self.gla_norm_g = inp("gla_norm_g", [L, D])
        self.conv_w = inp("conv_w", [L, 3, D])
        self.w_gla_out = inp("w_gla_out", [L, D, D])
        self.w_conv_out = inp("w_conv_out", [L, D, D])
        self.w_out = inp("w_out", [L, D, D])
        self.norm2_g = inp("norm2_g", [L, D])
        self.rg_w = inp("router_group_w", [L, D, 8])
        self.rg_b = inp("router_group_b", [L, 8])
        self.re_w = inp("router_expert_w", [L, D, 64])
        self.re_b = inp("router_expert_b", [L, 64])
        self.ew1 = inp("expert_w1", [L, NE, D, DEXP])
        self.ew3 = inp("expert_w3", [L, NE, D, DEXP])
        self.ew2 = inp("expert_w2", [L, NE, DEXP, D])
        self.fng = inp("final_norm_g", [1, D])
        self.out = nc.dram_tensor("out", [SEQ, D], F32, kind="ExternalOutput").ap()

        def scr(name, shape, dt=F32):
            return nc.dram_tensor(name, list(shape), dt, kind="Internal").ap()
        self.xa = scr("xa", [SEQ, D])
        self.xb = scr("xb", [SEQ, D])
        self.xm = scr("xm", [SEQ, D])
        self.hTd = scr("hTd", [NTILE, 128, D], BF16)
        self.xbuf = scr("xbuf", [NSLOT, D], BF16)
        self.ybuf = scr("ybuf", [NSLOT, D], F32)
        self.B_x = [Buf("x%d" % i) for i in range(NTILE)]
        self.B_xa = [Buf("xa%d" % i) for i in range(NTILE)]
        self.B_xb = [Buf("xb%d" % i) for i in range(NTILE)]
        self.B_xm = [Buf("xm%d" % i) for i in range(NTILE)]
        self.B_hT = [Buf("hT%d" % i) for i in range(NTILE)]
        self.B_out = [Buf("out%d" % i) for i in range(NTILE)]
        self.B_xbuf = Buf("xbuf")
        self.B_ybuf = Buf("ybuf")
        self.B_dbg = Buf("dbg")
        self.dbg_out = {}
        for name, (shape, dt) in self.dbg.items():
            self.dbg_out[name] = nc.dram_tensor("dbg_" + name, list(shape), dt, kind="ExternalOutput").ap()

        self.PS = [S.psum("ps%d" % i, [128, 512], F32) for i in range(6)]
        self.PB = [S.psum("pb%d" % i, [128, 1024], BF16) for i in range(2)]
        self._ips = 0
        self._ipb = 0
        self.ARENA = 164 * 1024
        self.arena = S.sbuf("arena", [128, self.ARENA // 2], BF16)
        self.aoff = 0
        self.consts()
        self.common = {}

    def ps(self):
        b = self.PS[self._ips % len(self.PS)]
        self._ips += 1
        return b

    def pb(self):
        b = self.PB[self._ipb % len(self.PB)]
        self._ipb += 1
        return b

    def tap(self, name, buf, ap=None):
        if name not in self.dbg_out:
            return
        S, nc = self.S, self.nc
        dst = self.dbg_out[name]
        src = buf.t[:] if ap is None else ap
        S.dma("sp", lambda: nc.sync.dma_start(out=dst, in_=src), reads=[buf], writes=[self.B_dbg], part=True)

    def consts(self):
        S, nc = self.S, self.nc
        io = S.sbuf("io", [128, 128], I32)
        iof = S.sbuf("iof", [128, 128], F32)
        self.ident = S.sbuf("ident", [128, 128], BF16)
        self.identf = S.sbuf("identf", [128, 128], F32)
        self.Uinc = S.sbuf("Uinc", [128, 128], F32)
        self.Urev = S.sbuf("Urev", [128, 128], F32)
        self.Ltri = S.sbuf("Ltri", [128, 128], F32)
        self.ones = S.sbuf("ones", [128, 128], F32)
        self.mask4 = S.sbuf("mask4", [128, 512], F32)
        S.op("pool", lambda: nc.gpsimd.iota(io[:], pattern=[[1, 128]], base=0, channel_multiplier=-1), writes=[io])
        S.op("dve", lambda: nc.vector.tensor_copy(out=iof[:], in_=io[:]), reads=[io], writes=[iof])
        S.op("dve", lambda: nc.vector.tensor_single_scalar(out=self.ident[:], in_=iof[:], scalar=0.0, op=ALU.is_equal), reads=[iof], writes=[self.ident])
        S.op("dve", lambda: nc.vector.tensor_single_scalar(out=self.identf[:], in_=iof[:], scalar=0.0, op=ALU.is_equal), reads=[iof], writes=[self.identf])
        S.op("dve", lambda: nc.vector.tensor_single_scalar(out=self.Ltri[:], in_=iof[:], scalar=0.0, op=ALU.is_ge), reads=[iof], writes=[self.Ltri])
        S.op("dve", lambda: nc.vector.tensor_scalar(out=self.Uinc[:], in0=iof[:], scalar1=0.0, scalar2=-1.0 / 16, op0=ALU.is_ge, op1=ALU.mult), reads=[iof], writes=[self.Uinc])
        S.op("dve", lambda: nc.vector.tensor_scalar(out=self.Urev[:], in0=iof[:], scalar1=0.0, scalar2=-1.0 / 16, op0=ALU.is_lt, op1=ALU.mult), reads=[iof], writes=[self.Urev])
        S.op("dve", lambda: nc.vector.memset(self.ones[:], 1.0), writes=[self.ones])
        for h in range(4):
            S.op("dve", lambda h=h: nc.vector.tensor_copy(out=self.mask4[:, h * 128:(h + 1) * 128], in_=self.Ltri[:]), reads=[self.Ltri], writes=[self.mask4])
        self.io_ = io
        cT = S.sbuf("cT", [128, 8], F32)
        self.scT = S.sbuf("scT", [128, 8, 128], F32)
        S.dma("sp", lambda: nc.sync.dma_start(out=cT[:], in_=self.c.rearrange("o (k p) -> p (o k)", p=128), allow_slow_non_contiguous=True), writes=[cT])
        S.op("act", lambda: nc.scalar.activation(out=cT[:], in_=cT[:], func=AF.Silu), reads=[cT], writes=[cT])
        S.op("dve", lambda: nc.vector.tensor_copy(out=self.scT[:], in_=cT[:].unsqueeze(2).to_broadcast([128, 8, 128])), reads=[cT], writes=[self.scT])
        self._imod = 0
        self.SH = S.sbuf("SH", [128, D], F32)
        self.G = S.sbuf("G", [128, D], F32)
        self.GATE = S.sbuf("GATE", [128, D], F32)
        self.xt = [S.sbuf("xt%d" % i, [128, D], F32) for i in range(2)]
        self.junk = S.sbuf("junk", [128, D], BF16)
        self.t32 = [S.sbuf("t32_%d" % i, [128, D], F32) for i in range(2)]
        self.rowrep = self.t32[0]
        self.small = {}

    def al(self, name, shape, dt):
        esz = 2 if dt == BF16 else 4
        n = 1
        for s in shape[1:]:
            n *= s
        nbytes = n * esz
        off = self.aoff
        self.aoff += (nbytes + 31) // 32 * 32
        assert self.aoff <= self.ARENA, (name, self.aoff)
        ap = self.arena.t[0:shape[0], off // 2:(off + nbytes) // 2]
        if dt != BF16:
            ap = ap.bitcast(dt)
        if len(shape) == 3:
            ap = ap.rearrange("p (a b) -> p a b", a=shape[1])
        return Buf(name, ap)

    def arelease(self, mark=0):
        self.S.barrier()
        self.aoff = mark

    def sm(self, name, shape, dt=F32):
        if name not in self.small:
            self.small[name] = self.S.sbuf(name, shape, dt)
        return self.small[name]

    def mod_half(self, l, half, norm_g):
        S, nc = self.S, self.nc
        dsts = [self.SH, self.G, self.GATE]
        mark = self.aoff
        modw = [self.al("modw%d" % i, [128, 8, 512], F32) for i in range(2)]
        modbias = [self.al("modbias%d" % i, [128, 512], F32) for i in range(2)]
        S.dma("sp", lambda: nc.sync.dma_start(out=self.rowrep[:], in_=norm_g[l, :].partition_broadcast(128)), writes=[self.rowrep])
        for j in range(6):
            col = half * 3072 + j * 512
            wb = modw[self._imod % 2]
            bb = modbias[self._imod % 2]
            self._imod += 1
            S.dma("sp", lambda wb=wb, col=col: nc.sync.dma_start(out=wb[:], in_=self.mod_w[l, :, col:col + 512].rearrange("(k p) n -> p k n", p=128)), writes=[wb])
            S.dma("sp", lambda bb=bb, col=col: nc.sync.dma_start(out=bb[:], in_=self.mod_b[l, col:col + 512].partition_broadcast(128)), writes=[bb])
            p = self.ps()

            def mm(p=p, wb=wb):
                for k in range(8):
                    i = nc.tensor.matmul(out=p[:], lhsT=self.scT[:, k, :], rhs=wb[:, k, :], start=(k == 0), stop=(k == 7))
                return i
            S.op("pe", mm, reads=[self.scT, wb], writes=[p])
            dst = dsts[j // 2]
            sl = slice((j % 2) * 512, (j % 2) * 512 + 512)
            S.op("dve", lambda p=p, bb=bb, dst=dst, sl=sl: nc.vector.tensor_tensor(out=dst[:, sl], in0=p[:], in1=bb[:], op=ALU.add), reads=[p, bb], writes=[dst])
        S.op("dve", lambda: nc.vector.scalar_tensor_tensor(out=self.G[:], in0=self.G[:], scalar=1.0, in1=self.rowrep[:], op0=ALU.add, op1=ALU.mult), reads=[self.G, self.rowrep], writes=[self.G])
        self.arelease(mark)

    def load_w(self, dst, src_rows_cols, dst_sl=None):
        S, nc = self.S, self.nc
        n = src_rows_cols.shape[1]
        o0 = 0 if dst_sl is None else dst_sl
        for c0 in range(0, n, 512):
            c1 = min(n, c0 + 512)
            S.dma("pool", lambda c0=c0, c1=c1: nc.gpsimd.dma_start(out=dst[:, :, o0 + c0:o0 + c1], in_=src_rows_cols[:, c0:c1].rearrange("(k p) n -> p k n", p=128)), writes=[dst], part=True)

    def rstd_from_ss(self, ss, n, rstd):
        S, nc = self.S, self.nc
        S.op("dve", lambda: nc.vector.tensor_scalar(out=rstd[:], in0=ss[:], scalar1=1.0 / n, scalar2=EPS, op0=ALU.mult, op1=ALU.add), reads=[ss], writes=[rstd])
        S.op("act", lambda: nc.scalar.activation(out=rstd[:], in_=rstd[:], func=AF.Sqrt), reads=[rstd], writes=[rstd])
        S.op("dve", lambda: nc.vector.reciprocal(out=rstd[:], in_=rstd[:]), reads=[rstd], writes=[rstd])

    def norm_mod(self, xt, hout, key):
        S, nc = self.S, self.nc
        ss = self.sm("ss_" + key, [128, 1])
        rstd = self.sm("rstd_" + key, [128, 1])
        S.op("act", lambda: nc.scalar.activation(out=self.junk[:], in_=xt[:], func=AF.Square), reads=[xt], writes=[self.junk])
        S.op("dve", lambda: nc.vector.reduce_sum(out=ss[:], in_=self.junk[:], axis=AX.X), reads=[self.junk], writes=[ss])
        self.rstd_from_ss(ss, D, rstd)
        t = self.t32[0]
        S.op("dve", lambda: nc.vector.scalar_tensor_tensor(out=t[:], in0=xt[:], scalar=rstd[:, 0:1], in1=self.G[:], op0=ALU.mult, op1=ALU.mult), reads=[xt, rstd, self.G], writes=[t])
        S.op("dve", lambda: nc.vector.tensor_tensor(out=hout[:], in0=t[:], in1=self.SH[:], op=ALU.add), reads=[t, self.SH], writes=[hout])

    def transpose8(self, src, dst, n=8):
        S, nc = self.S, self.nc
        p = self.pb()

        def tr():
            for k in range(n):
                i = nc.tensor.transpose(out=p[:, k * 128:(k + 1) * 128], in_=src[:, k * 128:(k + 1) * 128], identity=self.ident[:])
            return i
        S.op("pe", tr, reads=[src, self.ident], writes=[p])
        S.op("act", lambda: nc.scalar.copy(out=dst[:, 0:n * 128], in_=p[:, 0:n * 128]), reads=[p], writes=[dst])

    def transpose_strided(self, src, dst, n):
        S, nc = self.S, self.nc
        p = self.pb()

        def tr():
            for j in range(n):
                i = nc.tensor.transpose(out=p[:, j * 128:(j + 1) * 128], in_=src[:, j:n * 128:n], identity=self.ident[:])
            return i
        S.op("pe", tr, reads=[src, self.ident], writes=[p])
        S.op("act", lambda: nc.scalar.copy(out=dst[:, 0:n * 128], in_=p[:, 0:n * 128]), reads=[p], writes=[dst])

    def mm_flat(self, lhsT, W, rowlen, col, out_ps, nk):
        nc = self.nc

        def mm():
            for j in range(nk):
                i = nc.tensor.matmul(out=out_ps[:], lhsT=lhsT[:, j * 128:(j + 1) * 128], rhs=W[:, j * rowlen + col:j * rowlen + col + 512], start=(j == 0), stop=(j == nk - 1))
            return i
        self.S.op("pe", mm, reads=[lhsT, W], writes=[out_ps])

    def mm_tok(self, lhsT, W, col, out_ps, nk=8):
        nc = self.nc

        def mm():
            for k in range(nk):
                i = nc.tensor.matmul(out=out_ps[:], lhsT=lhsT[:, k * 128:(k + 1) * 128], rhs=W[:, k, col:col + 512], start=(k == 0), stop=(k == nk - 1))
            return i
        self.S.op("pe", mm, reads=[lhsT, W], writes=[out_ps])

    def out_proj_residual(self, yb, xin, gate_first, xout_dram, B_out, i):
        S, nc = self.S, self.nc
        yT = self.sm("yT", [128, D], BF16)
        self.transpose8(yb, yT)
        xo = self.t32[1]
        for half in range(2):
            p = self.ps()
            self.mm_tok(yT, self.WO, half * 512, p)
            sl = slice(half * 512, half * 512 + 512)
            S.op("dve", lambda p=p, sl=sl: nc.vector.tensor_tensor(out=xo[:, sl], in0=p[:], in1=self.GATE[:, sl], op=ALU.mult), reads=[p, self.GATE], writes=[xo])
        S.op("pool", lambda: nc.gpsimd.tensor_tensor(out=xo[:], in0=xo[:], in1=xin[:], op=ALU.add), reads=[xo, xin], writes=[xo])
        S.dma("sp", lambda: nc.sync.dma_start(out=xout_dram[i * 128:(i + 1) * 128, :], in_=xo[:]), reads=[xo], writes=[B_out[i]])

    def pass_a(self, l, xin, B_xin, xout, B_xout):
        S, nc = self.S, self.nc
        NT = self.NT
        WA = self.al("WA", [128, 8, 4112], BF16)
        self.W2 = self.al("W2", [128, 8, D], BF16)
        self.WO = self.al("WO", [128, 8, D], BF16)
        wi = self.w_in[l]
        self.load_w(WA, wi[:, 0:2048], 0)
        self.load_w(WA, wi[:, 2064:3088], 2048)
        self.load_w(WA, wi[:, 6160:7184], 3072)
        self.load_w(WA, wi[:, 2048:2064], 4096)
        self.load_w(self.W2, self.w_gla_out[l])
        self.load_w(self.WO, self.w_out[l])
        cq, ck, cv, cr, cga, cgd = 0, 512, 1024, 2048, 3072, 4096
        gw = self.al("gw", [32, 512], F32)
        S.op("dve", lambda: nc.vector.memset(gw[:], 0.0), writes=[gw])
        S.dma("sp", lambda: nc.sync.dma_start(out=gw[0:16, :], in_=self.gate_w2[l]), writes=[gw])
        S.dma("sp", lambda: nc.sync.dma_start(out=gw[16:17, :], in_=self.gate_b[l:l + 1, :]), writes=[gw], part=True)
        gng = self.al("gng", [128, D], F32)
        S.dma("sp", lambda: nc.sync.dma_start(out=gng[:], in_=self.gla_norm_g[l, :].partition_broadcast(128)), writes=[gng])
        St = self.al("St", [128, 4, 256], F32)
        Sb = self.al("Sb", [128, 4, 256], BF16)
        S.op("dve", lambda: nc.vector.memset(St[:], 0.0), writes=[St])
        S.op("dve", lambda: nc.vector.memset(Sb[:], 0.0), writes=[Sb])
        gda = [self.al("gda%d" % j, [32, 128], F32) for j in range(2)]
        for j in range(2):
            S.op("dve", lambda j=j: nc.vector.memset(gda[j][:], 0.0), writes=[gda[j]])
            S.dma("sp", lambda j=j: nc.sync.dma_start(out=gda[j][16:17, :], in_=self.ones[0:1, :]), reads=[self.ones], writes=[gda[j]])
        hb = self.al("hb", [128, D], BF16)
        hT = [self.al("hT%d" % j, [128, D], BF16) for j in range(2)]
        xts = [self.xt[0], self.xt[1], self.al("xt2", [128, D], F32)]
        qraw = [self.al("qraw%d" % j, [128, 512], F32) for j in range(2)]
        kraw = [self.al("kraw%d" % j, [128, 512], F32) for j in range(2)]
        ktok = [self.al("ktok%d" % j, [128, 512], BF16) for j in range(2)]
        vb = [self.al("vb%d" % j, [128, D], BF16) for j in range(2)]
        sr = [self.al("sr%d" % j, [128, D], BF16) for j in range(2)]
        sga = [self.al("sga%d" % j, [128, D], BF16) for j in range(2)]
        sp_ = self.al("sp_", [128, 512], F32)
        Eb = self.al("Eb", [128, 512], F32)
        Enb = self.al("Enb", [128, 512], F32)
        Es = self.al("Es", [128, 512], F32)
        qTt = self.al("qTt", [128, 512], BF16)
        kTt = self.al("kTt", [128, 512], BF16)
        ks = self.al("ks", [128, 512], BF16)
        am = self.al("am", [128, 512], BF16)
        on = self.al("on", [128, D], BF16)
        og = self.al("og", [128, D], BF16)
        ogT = self.al("ogT", [128, D], BF16)
        yg = self.al("yg", [128, D], BF16)
        junk2 = self.al("junk2", [128, D], BF16)
        ss4 = self.al("ss4", [128, 4], F32)
        rs4 = self.al("rs4", [128, 4], F32)
        xps = [self.PS[4], self.PS[5]]
        yps = self.PS[0:4]
        cnt = {"x": 0, "y": 0}

        def px():
            cnt["x"] += 1
            return xps[cnt["x"] % 2]

        def py():
            cnt["y"] += 1
            return yps[cnt["y"] % 4]

        def mmk(lhsT, col, p):
            def mm():
                for k in range(8):
                    ins = nc.tensor.matmul(out=p[:], lhsT=lhsT[:, k * 128:(k + 1) * 128], rhs=WA[:, k, col:col + 512], start=(k == 0), stop=(k == 7))
                return ins
            S.op("pe", mm, reads=[lhsT, WA], writes=[p])

        def fm(p, c0, h_T, m=128, nh=4):
            def f():
                for h in range(nh):
                    for k in range(8):
                        ins = nc.tensor.matmul(out=p[0:m, h * 128:(h + 1) * 128], lhsT=WA[:, k, c0 + h * m:c0 + (h + 1) * m],
                                               rhs=h_T[:, k * 128:(k + 1) * 128], start=(k == 0), stop=(k == 7))
                return ins
            S.op("pe", f, reads=[WA, h_T], writes=[p])

        def xsteps(i):
            j = i % 2
            xt, h_T = xts[i % 3], hT[j]
            st = []

            def x0a():
                S.dma("sp", lambda: nc.sync.dma_start(out=xt[:], in_=xin[i * 128:(i + 1) * 128, :]), reads=[B_xin[i]], writes=[xt])
                self.norm_mod(xt, hb, "a")
            st.append(x0a)

            def x0b():
                self.transpose8(hb, h_T)
                S.dma("sp", lambda: nc.sync.dma_start(out=self.hTd[i], in_=h_T[:]), reads=[h_T], writes=[self.B_hT[i]])
            st.append(x0b)

            def x1():
                p = px()
                fm(p, cq, h_T)
                S.op("act", lambda: nc.scalar.copy(out=qraw[j][:], in_=p[:]), reads=[p], writes=[qraw[j]])
            st.append(x1)

            def x2():
                p = px()
                fm(p, ck, h_T)
                S.op("dve", lambda: nc.vector.tensor_copy(out=kraw[j][:], in_=p[:]), reads=[p], writes=[kraw[j]])
            st.append(x2)

            def x3():
                p = px()
                fm(p, cgd, h_T, m=16, nh=1)
                S.op("act", lambda: nc.scalar.copy(out=gda[j][0:16, :], in_=p[0:16, 0:128]), reads=[p], writes=[gda[j]])
            st.append(x3)

            def x4():
                p = px()
                mmk(h_T, ck, p)
                S.op("dve", lambda: nc.vector.tensor_copy(out=ktok[j][:], in_=p[:]), reads=[p], writes=[ktok[j]])
            st.append(x4)
            for half in range(2):
                def xv(half=half):
                    p = px()
                    mmk(h_T, cv + half * 512, p)
                    S.op("act", lambda: nc.scalar.copy(out=vb[j][:, half * 512:(half + 1) * 512], in_=p[:]), reads=[p], writes=[vb[j]])
                st.append(xv)
            for half in range(2):
                def xr(half=half):
                    p = px()
                    mmk(h_T, cr + half * 512, p)
                    S.op("act", lambda: nc.scalar.activation(out=sr[j][:, half * 512:(half + 1) * 512], in_=p[:], func=AF.Silu), reads=[p], writes=[sr[j]])
                st.append(xr)
            for half in range(2):
                def xg(half=half):
                    p = px()
                    mmk(h_T, cga + half * 512, p)
                    S.op("act", lambda: nc.scalar.activation(out=sga[j][:, half * 512:(half + 1) * 512], in_=p[:], func=AF.Sigmoid), reads=[p], writes=[sga[j]])
                st.append(xg)
            return st

        def ysteps(i):
            j = i % 2
            xt = xts[i % 3]
            st = []

            def y0():
                plg = py()
                S.op("pe", lambda: nc.tensor.matmul(out=plg[:], lhsT=gda[j][:], rhs=gw[:], start=True, stop=True), reads=[gda[j], gw], writes=[plg])
                S.op("act", lambda: nc.scalar.activation(out=Es[:], in_=plg[:], func=AF.Exp, scale=-1.0), reads=[plg], writes=[Es])
                S.op("act", lambda: nc.scalar.activation(out=sp_[:], in_=Es[:], func=AF.Ln, bias=1.0), reads=[Es], writes=[sp_])
            st.append(y0)

            def y1():
                pbT, prev = py(), py()

                def cum1():
                    for h in range(4):
                        ins = nc.tensor.matmul(out=pbT[:, h * 128:(h + 1) * 128], lhsT=sp_[:, h * 128:(h + 1) * 128], rhs=self.Uinc[:], start=True, stop=True)
                    return ins
                S.op("pe", cum1, reads=[sp_, self.Uinc], writes=[pbT])
                S.op("pe", lambda: nc.tensor.matmul(out=prev[:], lhsT=self.Urev[:], rhs=sp_[:], start=True, stop=True), reads=[sp_, self.Urev], writes=[prev])
                S.op("act", lambda: nc.scalar.activation(out=Eb[:], in_=pbT[:], func=AF.Exp), reads=[pbT], writes=[Eb])
                S.op("act", lambda: nc.scalar.activation(out=Enb[:], in_=pbT[:], func=AF.Exp, scale=-1.0), reads=[pbT], writes=[Enb])
                S.op("act", lambda: nc.scalar.activation(out=Es[:], in_=prev[:], func=AF.Exp), reads=[prev], writes=[Es])
            st.append(y1)

            def y2():
                S.op("dve", lambda: nc.vector.scalar_tensor_tensor(out=qTt[:], in0=qraw[j][:], scalar=128 ** -0.5, in1=Eb[:], op0=ALU.mult, op1=ALU.mult), reads=[qraw[j], Eb], writes=[qTt])
                S.op("dve", lambda: nc.vector.tensor_tensor(out=kTt[:], in0=kraw[j][:], in1=Enb[:], op=ALU.mult), reads=[kraw[j], Enb], writes=[kTt])
                S.op("pool", lambda: nc.gpsimd.tensor_tensor(out=ks[:], in0=ktok[j][:], in1=Es[:], op=ALU.mult), reads=[ktok[j], Es], writes=[ks])
            st.append(y2)

            def y3():
                pat = py()

                def att():
                    for h in range(4):
                        ins = nc.tensor.matmul(out=pat[:, h * 128:(h + 1) * 128], lhsT=kTt[:, h * 128:(h + 1) * 128], rhs=qTt[:, h * 128:(h + 1) * 128], start=True, stop=True)
                    return ins
                S.op("pe", att, reads=[kTt, qTt], writes=[pat])
                S.op("dve", lambda: nc.vector.tensor_tensor(out=am[:], in0=pat[:], in1=self.mask4[:], op=ALU.mult), reads=[pat, self.mask4], writes=[am])
            st.append(y3)
            hold = {}

            def y4():
                po = [py(), py()]
                pds = [py(), py()]
                hold["po"] = po

                def omm():
                    for h in range(4):
                        o_ap = po[h // 2][:, (h % 2) * 256:(h % 2) * 256 + 256]
                        nc.tensor.matmul(out=o_ap, lhsT=am[:, h * 128:(h + 1) * 128], rhs=vb[j][:, h * 256:(h + 1) * 256], start=True, stop=False)
                        ins = nc.tensor.matmul(out=o_ap, lhsT=qTt[:, h * 128:(h + 1) * 128], rhs=Sb[:, h, :], start=False, stop=True)
                    return ins
                S.op("pe", omm, reads=[am, vb[j], qTt, Sb], writes=po)

                def dsm():
                    for h in range(4):
                        ins = nc.tensor.matmul(out=pds[h // 2][:, (h % 2) * 256:(h % 2) * 256 + 256], lhsT=ks[:, h * 128:(h + 1) * 128], rhs=vb[j][:, h * 256:(h + 1) * 256], start=True, stop=True)
                    return ins
                S.op("pe", dsm, reads=[ks, vb[j]], writes=pds)
                for h in range(4):
                    S.op("dve", lambda h=h: nc.vector.scalar_tensor_tensor(out=St[:, h, :], in0=St[:, h, :], scalar=Eb[:, h * 128 + 127:h * 128 + 128],
                                                                            in1=pds[h // 2][:, (h % 2) * 256:(h % 2) * 256 + 256], op0=ALU.mult, op1=ALU.add),
                         reads=[St, Eb, pds[h // 2]], writes=[St])
                S.op("pool", lambda: nc.gpsimd.tensor_copy(out=Sb[:], in_=St[:]), reads=[St], writes=[Sb])
            st.append(y4)

            def y5():
                po = hold["po"]
                for hh in range(2):
                    S.op("act", lambda hh=hh: nc.scalar.activation(out=junk2[:, hh * 512:(hh + 1) * 512], in_=po[hh][:], func=AF.Square), reads=[po[hh]], writes=[junk2])
                S.op("dve", lambda: nc.vector.reduce_sum(out=ss4[:], in_=junk2[:].rearrange("p (h v) -> p h v", h=4), axis=AX.X), reads=[junk2], writes=[ss4])
                self.rstd_from_ss(ss4, 256, rs4)
                for h in range(4):
                    S.op("dve", lambda h=h: nc.vector.scalar_tensor_tensor(out=on[:, h * 256:(h + 1) * 256], in0=po[h // 2][:, (h % 2) * 256:(h % 2) * 256 + 256], scalar=rs4[:, h:h + 1],
                                                                            in1=gng[:, h * 256:(h + 1) * 256], op0=ALU.mult, op1=ALU.mult), reads=[po[h // 2], rs4, gng], writes=[on])
                S.op("pool", lambda: nc.gpsimd.tensor_tensor(out=og[:], in0=on[:], in1=sr[j][:], op=ALU.mult), reads=[on, sr[j]], writes=[og])
            st.append(y5)

            def y6a():
                self.transpose8(og, ogT)
            st.append(y6a)

            def y6b():
                for half in range(2):
                    p = py()
                    self.mm_tok(ogT, self.W2, half * 512, p)
                    sl = slice(half * 512, half * 512 + 512)
                    S.op("dve", lambda p=p, sl=sl: nc.vector.tensor_tensor(out=yg[:, sl], in0=p[:], in1=sga[j][:, sl], op=ALU.mult), reads=[p, sga[j]], writes=[yg])
            st.append(y6b)

            yT = self.sm("yT", [128, D], BF16)

            def y7a():
                self.transpose8(yg, yT)
            st.append(y7a)

            def y7b():
                xo = self.t32[1]
                for half in range(2):
                    p = py()
                    self.mm_tok(yT, self.WO, half * 512, p)
                    sl = slice(half * 512, half * 512 + 512)
                    S.op("dve", lambda p=p, sl=sl: nc.vector.tensor_tensor(out=xo[:, sl], in0=p[:], in1=self.GATE[:, sl], op=ALU.mult), reads=[p, self.GATE], writes=[xo])
                S.op("pool", lambda: nc.gpsimd.tensor_tensor(out=xo[:], in0=xo[:], in1=xt[:], op=ALU.add), reads=[xo, xt], writes=[xo])
                S.dma("sp", lambda: nc.sync.dma_start(out=xout[i * 128:(i + 1) * 128, :], in_=xo[:]), reads=[xo], writes=[B_xout[i]])
            st.append(y7b)
            return st

        Y = [ysteps(i) for i in range(NT)]
        X = [xsteps(i) for i in range(NT)]
        for f in X[0]:
            f()
        for i in range(-1, NT):
            a, bq, c = i, i + 1, i + 2
            ha, hb_, hc = (a >= 0), (bq < NT), (c < NT)
            if hc:
                X[c][0]()
            if ha:
                Y[a][6]()
            if hb_:
                Y[bq][0]()
            if ha:
                Y[a][7]()
            if hb_:
                Y[bq][1]()
            if ha:
                Y[a][8]()
                Y[a][9]()
            if hc:
                X[c][1]()
            if hb_:
                Y[bq][2]()
                Y[bq][3]()
            if hc:
                X[c][2]()
                X[c][3]()
            if hb_:
                Y[bq][4]()
                Y[bq][5]()
            if hc:
                for f in X[c][4:]:
                    f()

    def pass_b(self, l, xin, B_xin, xout, B_xout):
        S, nc = self.S, self.nc
        WB = self.al("WB", [128, 8, 4096], BF16)
        self.W2 = self.al("W2", [128, 8, D], BF16)
        self.WO = self.al("WO", [128, 8, D], BF16)
        self.load_w(self.WO, self.w_out[l])
        wi = self.w_in[l]
        self.load_w(WB, wi[:, 3088:6160], 0)
        self.load_w(WB, wi[:, 7184:8208], 3072)
        self.load_w(self.W2, self.w_conv_out[l])
        cw = self.al("cw", [128, 8, 3], F32)
        for j in range(3):
            S.dma("sp", lambda j=j: nc.sync.dma_start(out=cw[:, :, j:j + 1], in_=self.conv_w[l, j:j + 1, :].rearrange("o (c p) -> p c o", p=128), allow_slow_non_contiguous=True), writes=[cw], part=True)
        U = self.al("U", [128, 8, 514], F32)
        S.op("dve", lambda: nc.vector.memset(U[:], 0.0), writes=[U])
        hT4 = self.al("hT4", [128, 4, D], BF16)
        ccs = self.al("ccs", [128, 512], F32)
        acc = self.al("acc", [128, 512], F32)
        PT = self.al("PT", [128, 8, 512], BF16)
        sgc = self.al("sgc", [128, D], BF16)
        yc = self.al("yc", [128, D], BF16)
        nst = self.NT // 4
        for s in range(nst):
            for t in range(4):
                i = s * 4 + t
                S.dma("sp", lambda t=t, i=i: nc.sync.dma_start(out=hT4[:, t, :], in_=self.hTd[i]), reads=[self.B_hT[i]], writes=[hT4], part=True)
            for cc in range(8):
                pcs = []
                for br in range(3):
                    p = self.ps()
                    c0 = br * 1024 + cc * 128

                    def f(p=p, c0=c0):
                        for k in range(8):
                            ins = nc.tensor.matmul(out=p[:].rearrange("p (t n) -> p t n", t=4), lhsT=WB[:, k, c0:c0 + 128],
                                                   rhs=hT4[:, :, k * 128:(k + 1) * 128], start=(k == 0), stop=(k == 7))
                        return ins
                    S.op("pe", f, reads=[WB, hT4], writes=[p])
                    pcs.append(p)
                pcb, pcc, pch = pcs
                S.op("act", lambda pcc=pcc: nc.scalar.copy(out=ccs[:], in_=pcc[:]), reads=[pcc], writes=[ccs])
                S.op("dve", lambda pch=pch, cc=cc: nc.vector.tensor_tensor(out=U[:, cc, 2:514], in0=pch[:], in1=ccs[:], op=ALU.mult), reads=[pch, ccs], writes=[U])
                S.op("dve", lambda cc=cc: nc.vector.tensor_scalar(out=acc[:], in0=U[:, cc, 2:514], scalar1=cw[:, cc, 2:3], scalar2=None, op0=ALU.mult), reads=[U, cw], writes=[acc])
                S.op("dve", lambda cc=cc: nc.vector.scalar_tensor_tensor(out=acc[:], in0=U[:, cc, 1:513], scalar=cw[:, cc, 1:2], in1=acc[:], op0=ALU.mult, op1=ALU.add), reads=[U, cw, acc], writes=[acc])
                S.op("dve", lambda cc=cc: nc.vector.scalar_tensor_tensor(out=acc[:], in0=U[:, cc, 0:512], scalar=cw[:, cc, 0:1], in1=acc[:], op0=ALU.mult, op1=ALU.add), reads=[U, cw, acc], writes=[acc])
                S.op("dve", lambda pcb=pcb, cc=cc: nc.vector.tensor_tensor(out=PT[:, cc, :], in0=pcb[:], in1=acc[:], op=ALU.mult), reads=[pcb, acc], writes=[PT])
                S.op("pool", lambda cc=cc: nc.gpsimd.tensor_copy(out=U[:, cc, 0:2], in_=U[:, cc, 512:514]), reads=[U], writes=[U])
            for t in range(4):
                i = s * 4 + t
                xt = self.xt[i % 2]
                S.dma("sp", lambda xt=xt, i=i: nc.sync.dma_start(out=xt[:], in_=xin[i * 128:(i + 1) * 128, :]), reads=[B_xin[i]], writes=[xt])
                for half in range(2):
                    p = self.ps()

                    def g(p=p, half=half, t=t):
                        for k in range(8):
                            ins = nc.tensor.matmul(out=p[:], lhsT=hT4[:, t, k * 128:(k + 1) * 128], rhs=WB[:, k, 3072 + half * 512:3072 + half * 512 + 512], start=(k == 0), stop=(k == 7))
                        return ins
                    S.op("pe", g, reads=[hT4, WB], writes=[p])
                    S.op("act", lambda p=p, half=half: nc.scalar.activation(out=sgc[:, half * 512:(half + 1) * 512], in_=p[:], func=AF.Sigmoid), reads=[p], writes=[sgc])
                for half in range(2):
                    p = self.ps()

                    def y(p=p, half=half, t=t):
                        for cc in range(8):
                            ins = nc.tensor.matmul(out=p[:], lhsT=PT[:, cc, t * 128:(t + 1) * 128], rhs=self.W2[:, cc, half * 512:half * 512 + 512], start=(cc == 0), stop=(cc == 7))
                        return ins
                    S.op("pe", y, reads=[PT, self.W2], writes=[p])
                    sl = slice(half * 512, half * 512 + 512)
                    S.op("dve", lambda p=p, sl=sl: nc.vector.tensor_tensor(out=yc[:, sl], in0=p[:], in1=sgc[:, sl], op=ALU.mult), reads=[p, sgc], writes=[yc])
                self.out_proj_residual(yc, xt, None, xout, B_xout, i)

    def moe(self, l, xin, B_xin, xout, B_xout, final):
        S, nc = self.S, self.nc
        NT = self.NT
        rw = self.al("rw", [128, 8, 72], F32)
        S.dma("sp", lambda: nc.sync.dma_start(out=rw[:, :, 0:8], in_=self.rg_w[l].rearrange("(k p) n -> p k n", p=128)), writes=[rw])
        S.dma("sp", lambda: nc.sync.dma_start(out=rw[:, :, 8:72], in_=self.re_w[l].rearrange("(k p) n -> p k n", p=128)), writes=[rw], part=True)
        rb = self.al("rb", [128, 72], F32)
        S.dma("sp", lambda: nc.sync.dma_start(out=rb[:, 0:8], in_=self.rg_b[l, :].partition_broadcast(128)), writes=[rb])
        S.dma("sp", lambda: nc.sync.dma_start(out=rb[:, 8:72], in_=self.re_b[l, :].partition_broadcast(128)), writes=[rb], part=True)
        CUM = self.al("CUM", [128, NTILE, 64], F32)
        OH1 = self.al("OH1", [128, NTILE, 64], F32)
        OH2 = self.al("OH2", [128, NTILE, 64], F32)
        W12 = self.al("W12", [128, NTILE, 2], F32)
        run = self.al("run", [128, 64], F32)
        S.op("dve", lambda: nc.vector.memset(run[:], 0.0), writes=[run])
        S.op("dve", lambda: nc.vector.memset(CUM[:], 0.0), writes=[CUM])
        S.op("dve", lambda: nc.vector.memset(OH1[:], 0.0), writes=[OH1])
        S.op("dve", lambda: nc.vector.memset(OH2[:], 0.0), writes=[OH2])
        gm = self.al("gm", [128, 8], F32)
        ohg = self.al("ohg", [128, 8], F32)
        pen = self.al("pen", [128, 8], F32)
        msk = self.al("msk", [128, 64], F32)
        top8 = self.al("top8", [128, 8], F32)
        m01 = self.al("m01", [128, 64], F32)
        gexp = self.al("gexp", [128, 8], F32)
        cnti = self.al("cnti", [128, 64], I32)
        padf = self.al("padf", [128, 64], F32)
        pend = self.al("pend", [128, 64], F32)
        adj = self.al("adj", [128, 64], F32)
        big = self.al("big", [128, NTILE, 64], F32)
        Df = self.al("Df", [128, NTILE, 2], F32)
        Di = self.al("Di", [128, NTILE * 2], I32)
        thr = self.al("thr", [128, NBLK], F32)
        thri = self.al("thri", [128, NBLK], I32)
        bef = self.al("bef", [128, NBLK], F32)
        WI = [self.al("WI%d" % j, [128, NBLK], I32) for j in range(2)]
        vld = self.al("vld", [128, NBLK], F32)
        mark_h2 = self.aoff
        H2 = self.al("H2", [128, NTILE, D], BF16)
        h2s = [self.al("h2_%d" % j, [128, D], F32) for j in range(2)]
        h2Ts = [self.al("h2T%d" % j, [128, D], F32) for j in range(2)]
        LgA = self.al("LgA", [128, NTILE, 72], F32)
        S.op("dve", lambda: nc.vector.memset(LgA[:], 0.0), writes=[LgA])
        gmx = self.al("gmx", [128, NTILE], F32)
        gsm = self.al("gsm", [128, NTILE], F32)
        ohgA = self.al("ohgA", [128, NTILE, 8], F32)
        gshA = self.al("gshA", [128, NTILE, 8], F32)
        mskA = self.al("mskA", [128, NTILE, 64], F32)
        top8A = self.al("top8A", [128, NTILE, 8], F32)
        PRE = self.al("PRE", [128, NTILE, 64], F32)

        def rx(i):
            xt = self.xt[i % 2]
            h2, h2T = h2s[i % 2], h2Ts[i % 2]
            S.dma("sp", lambda xt=xt, i=i: nc.sync.dma_start(out=xt[:], in_=xin[i * 128:(i + 1) * 128, :]), reads=[B_xin[i]], writes=[xt])
            self.norm_mod(xt, h2, "m")
            S.op("act", lambda i=i: nc.scalar.copy(out=H2[:, i, :], in_=h2[:]), reads=[h2], writes=[H2])
            for half in range(2):
                p = self.ps()

                def tr(p=p, half=half):
                    for k in range(4):
                        kk = half * 4 + k
                        ins = nc.tensor.transpose(out=p[:, k * 128:(k + 1) * 128], in_=h2[:, kk * 128:(kk + 1) * 128], identity=self.identf[:])
                    return ins
                S.op("pe", tr, reads=[h2, self.identf], writes=[p])
                S.op("act", lambda p=p, half=half: nc.scalar.copy(out=h2T[:, half * 512:(half + 1) * 512], in_=p[:]), reads=[p], writes=[h2T])
            pl = self.ps()

            def rmm(pl=pl):
                for k in range(8):
                    ins = nc.tensor.matmul(out=pl[:, 0:72], lhsT=h2T[:, k * 128:(k + 1) * 128], rhs=rw[:, k, :], start=(k == 0), stop=(k == 7))
                return ins
            S.op("pe", rmm, reads=[h2T, rw], writes=[pl])
            S.op("dve", lambda pl=pl, i=i: nc.vector.tensor_tensor(out=LgA[:, i, :], in0=pl[:, 0:72], in1=rb[:], op=ALU.add), reads=[pl, rb], writes=[LgA])

        for i in range(NT):
            rx(i)
        T_ = NTILE
        LgG = LgA[:, :, 0:8]
        S.op("dve", lambda: nc.vector.reduce_max(out=gmx[:], in_=LgG, axis=AX.X), reads=[LgA], writes=[gmx])
        S.op("dve", lambda: nc.vector.tensor_tensor(out=ohgA[:], in0=LgG, in1=gmx[:].unsqueeze(2).to_broadcast([128, T_, 8]), op=ALU.is_equal), reads=[LgA, gmx], writes=[ohgA])
        S.op("dve", lambda: nc.vector.tensor_tensor(out=gshA[:], in0=LgG, in1=gmx[:].unsqueeze(2).to_broadcast([128, T_, 8]), op=ALU.subtract), reads=[LgA, gmx], writes=[gshA])
        S.op("act", lambda: nc.scalar.activation(out=gshA[:], in_=gshA[:], func=AF.Exp), reads=[gshA], writes=[gshA])
        S.op("dve", lambda: nc.vector.reduce_sum(out=gsm[:], in_=gshA[:], axis=AX.X), reads=[gshA], writes=[gsm])
        S.op("dve", lambda: nc.vector.reciprocal(out=gsm[:], in_=gsm[:]), reads=[gsm], writes=[gsm])
        S.op("dve", lambda: nc.vector.tensor_scalar(out=ohgA[:], in0=ohgA[:], scalar1=-1.0, scalar2=1e30, op0=ALU.add, op1=ALU.mult), reads=[ohgA], writes=[ohgA])
        S.op("dve", lambda: nc.vector.tensor_tensor(out=mskA[:].rearrange("p t (g j) -> p t g j", g=8), in0=LgA[:, :, 8:72].rearrange("p t (g j) -> p t g j", g=8),
                                                     in1=ohgA[:].unsqueeze(3).to_broadcast([128, T_, 8, 8]), op=ALU.add), reads=[LgA, ohgA], writes=[mskA])
        for i in range(NT):
            S.op("dve", lambda i=i: nc.vector.max(out=top8A[:, i, :], in_=mskA[:, i, :]), reads=[mskA], writes=[top8A])
        S.op("dve", lambda: nc.vector.tensor_tensor(out=OH1[:], in0=mskA[:], in1=top8A[:, :, 0:1].to_broadcast([128, T_, 64]), op=ALU.is_equal), reads=[mskA, top8A], writes=[OH1])
        S.op("dve", lambda: nc.vector.tensor_tensor(out=OH2[:], in0=mskA[:], in1=top8A[:, :, 1:2].to_broadcast([128, T_, 64]), op=ALU.is_equal), reads=[mskA, top8A], writes=[OH2])
        S.op("dve", lambda: nc.vector.tensor_tensor(out=gmx[:], in0=top8A[:, :, 0], in1=top8A[:, :, 1], op=ALU.subtract), reads=[top8A], writes=[gmx])
        S.op("act", lambda: nc.scalar.activation(out=gmx[:], in_=gmx[:], func=AF.Sigmoid), reads=[gmx], writes=[gmx])
        S.op("dve", lambda: nc.vector.tensor_tensor(out=W12[:, :, 0], in0=gmx[:], in1=gsm[:], op=ALU.mult), reads=[gmx, gsm], writes=[W12])
        S.op("dve", lambda: nc.vector.tensor_tensor(out=W12[:, :, 1], in0=gsm[:], in1=W12[:, :, 0], op=ALU.subtract), reads=[gsm, W12], writes=[W12])
        S.op("dve", lambda: nc.vector.tensor_tensor(out=big[:], in0=OH1[:], in1=OH2[:], op=ALU.add), reads=[OH1, OH2], writes=[big])
        S.op("dve", lambda: nc.vector.memset(PRE[:, 0, :], 0.0), writes=[PRE])
        for i in range(1, NT):
            S.op("dve", lambda i=i: nc.vector.tensor_tensor(out=PRE[:, i, :], in0=PRE[:, i - 1, :], in1=big[:, i - 1, :], op=ALU.add), reads=[PRE, big], writes=[PRE])
        S.op("dve", lambda: nc.vector.tensor_tensor(out=run[:], in0=PRE[:, NT - 1, :], in1=big[:, NT - 1, :], op=ALU.add), reads=[PRE, big], writes=[run])
        for i in range(NT):
            pc = self.ps()

            def cmm(pc=pc, i=i):
                nc.tensor.matmul(out=pc[:, 0:64], lhsT=self.Ltri[:], rhs=big[:, i, :], start=True, stop=False)
                return nc.tensor.matmul(out=pc[:, 0:64], lhsT=self.ones[:], rhs=PRE[:, i, :], start=False, stop=True)
            S.op("pe", cmm, reads=[self.Ltri, self.ones, big, PRE], writes=[pc])
            S.op("act", lambda pc=pc, i=i: nc.scalar.copy(out=CUM[:, i, :], in_=pc[:, 0:64]), reads=[pc], writes=[CUM])
        pct = self.ps()
        S.op("pe", lambda: nc.tensor.matmul(out=pct[:, 0:64], lhsT=self.ones[:], rhs=run[:], start=True, stop=True), reads=[self.ones, run], writes=[pct])
        S.op("dve", lambda: nc.vector.tensor_scalar(out=cnti[:], in0=pct[:, 0:64], scalar1=float(SROWS - 1), scalar2=None, op0=ALU.add), reads=[pct], writes=[cnti])
        S.op("dve", lambda: nc.vector.tensor_scalar(out=cnti[:], in0=cnti[:], scalar1=8, scalar2=8, op0=ALU.arith_shift_right, op1=ALU.logical_shift_left), reads=[cnti], writes=[cnti])
        S.op("dve", lambda: nc.vector.tensor_copy(out=padf[:], in_=cnti[:]), reads=[cnti], writes=[padf])
        S.op("dve", lambda: nc.vector.tensor_tensor_scan(out=pend[:], data0=self.ones[:, 0:64], data1=padf[:], initial=0.0, op0=ALU.mult, op1=ALU.add), reads=[self.ones, padf], writes=[pend])
        S.op("dve", lambda: nc.vector.scalar_tensor_tensor(out=adj[:], in0=pend[:], scalar=-1.0, in1=padf[:], op0=ALU.add, op1=ALU.subtract), reads=[pend, padf], writes=[adj])
        S.op("dve", lambda: nc.vector.tensor_tensor(out=CUM[:], in0=CUM[:], in1=adj[:].unsqueeze(1).to_broadcast([128, NTILE, 64]), op=ALU.add), reads=[CUM, adj], writes=[CUM])
        for k, OH in enumerate((OH1, OH2)):
            S.op("dve", lambda OH=OH: nc.vector.tensor_tensor(out=big[:], in0=CUM[:], in1=OH[:], op=ALU.mult), reads=[CUM, OH], writes=[big])
            S.op("dve", lambda k=k: nc.vector.reduce_sum(out=Df[:, :, k], in_=big[:], axis=AX.X), reads=[big], writes=[Df])
        S.op("dve", lambda: nc.vector.tensor_copy(out=Di[:], in_=Df[:].rearrange("p t k -> p (t k)")), reads=[Df], writes=[Di])
        self.tap("Di", Di)
        self.tap("W12", W12, W12[:].rearrange("p t k -> p (t k)"))
        S.op("pool", lambda: nc.gpsimd.iota(thri[:], pattern=[[SROWS, NBLK]], base=0, channel_multiplier=0), writes=[thri])
        S.op("dve", lambda: nc.vector.tensor_copy(out=thr[:], in_=thri[:]), reads=[thri], writes=[thr])
        for q4 in range(NBLK // NTILE):
            bsl = slice(q4 * NTILE, (q4 + 1) * NTILE)
            S.op("dve", lambda bsl=bsl: nc.vector.tensor_tensor(out=big[:], in0=pend[:].unsqueeze(1).to_broadcast([128, NTILE, 64]),
                                                            in1=thr[:, bsl].unsqueeze(2).to_broadcast([128, NTILE, 64]), op=ALU.is_le), reads=[pend, thr], writes=[big])
            S.op("dve", lambda bsl=bsl: nc.vector.reduce_sum(out=bef[:, bsl], in_=big[:], axis=AX.X), reads=[big], writes=[bef])
        S.op("dve", lambda: nc.vector.tensor_scalar(out=vld[:], in0=thr[:], scalar1=pend[:, 63:64], scalar2=None, op0=ALU.is_lt), reads=[thr, pend], writes=[vld])
        S.op("pool", lambda: nc.gpsimd.iota(thri[:], pattern=[[0, NBLK]], base=0, channel_multiplier=2), reads=[thr], writes=[thri])
        S.op("dve", lambda: nc.vector.tensor_copy(out=thr[:], in_=thri[:]), reads=[thri, bef], writes=[thr])
        S.op("dve", lambda: nc.vector.tensor_scalar(out=bef[:], in0=bef[:], scalar1=63.0, scalar2=256.0, op0=ALU.min, op1=ALU.mult), reads=[bef], writes=[bef])
        S.op("dve", lambda: nc.vector.tensor_tensor(out=bef[:], in0=bef[:], in1=thr[:], op=ALU.add), reads=[bef, thr], writes=[bef])
        for kh in range(2):
            S.op("dve", lambda kh=kh: nc.vector.scalar_tensor_tensor(out=thr[:], in0=bef[:], scalar=float(l * NE * 256 + kh) - OOB_BIG, in1=vld[:], op0=ALU.add, op1=ALU.mult), reads=[bef, vld], writes=[thr])
            S.op("dve", lambda kh=kh: nc.vector.tensor_scalar(out=WI[kh][:], in0=thr[:], scalar1=OOB_BIG, scalar2=None, op0=ALU.add), reads=[thr], writes=[WI[kh]])
        self.tap("WI0", WI[0])
        for i in range(NT):
            for k in range(2):
                S.dma("pool", lambda i=i, k=k: nc.gpsimd.indirect_dma_start(out=self.xbuf[:, :], out_offset=bass.IndirectOffsetOnAxis(ap=Di[:, 2 * i + k:2 * i + k + 1], axis=0),
                                                                            in_=H2[:, i, :], in_offset=None), reads=[H2, Di], writes=[self.B_xbuf], part=True)
        self.arelease(mark_h2)
        nslot = NBLK if NT == NTILE else min(NBLK, (2 * NT * 128) // SROWS + 64)
        nblk = 2 * nslot
        order = []
        lo, hi = 0, nslot - 1
        while lo <= hi:
            order.append(lo)
            if hi != lo:
                order.append(hi)
            lo += 1
            hi -= 1

        def rows(b):
            return (2 * order[b // 2] + (b % 2)) * 128
        NW13, NW2 = 3, 3
        w1b = [self.al("w1b%d" % j, [128, 8 * DEXP], BF16) for j in range(NW13)]
        w3b = [self.al("w3b%d" % j, [128, 8 * DEXP], BF16) for j in range(NW13)]
        w2b = [self.al("w2b%d" % j, [128, 4 * D], BF16) for j in range(NW2)]
        xbk = [self.al("xbk%d" % j, [128, D], BF16) for j in range(3)]
        xbT = [self.al("xbT%d" % j, [128, D], BF16) for j in range(2)]
        s1 = [self.al("s1_%d" % j, [128, DEXP], BF16) for j in range(2)]
        gb = [self.al("gb%d" % j, [128, DEXP], BF16) for j in range(2)]
        gT = [self.al("gT%d" % j, [128, DEXP], BF16) for j in range(2)]
        yo = [self.al("yo%d" % j, [128, D], F32) for j in range(2)]
        v1 = self.ew1.rearrange("l e (p kh k4) n -> (l e p kh) (k4 n)", kh=2, k4=4)
        v3 = self.ew3.rearrange("l e (p kh k4) n -> (l e p kh) (k4 n)", kh=2, k4=4)
        v2 = self.ew2.rearrange("l e (p kh k2) n -> (l e p kh) (k2 n)", kh=2, k2=2)

        def gather_w(dst, view, b):
            for kh in range(2):
                def gth(dst=dst, view=view, kh=kh, b=b):
                    if getattr(self, "_bcreg", None) is None:
                        self._bcreg = nc.gpsimd.to_reg(DEPTH * NE * 256 - 1)
                    return nc.gpsimd.indirect_dma_start(
                        out=dst[:, kh * 2048:(kh + 1) * 2048], out_offset=None, in_=view,
                        in_offset=bass.IndirectOffsetOnAxis(ap=WI[kh][:, order[b]:order[b] + 1], axis=0),
                        bounds_check=self._bcreg, oob_is_err=False)
                S.dma("pool", gth, reads=[WI[kh]], writes=[dst], part=(kh == 1))

        pst = {}

        def ldx(b):
            xk = xbk[b % 3]
            S.dma("sp", lambda xk=xk, b=b: nc.sync.dma_start(out=xk[:], in_=self.xbuf[rows(b):rows(b) + 128, :]), reads=[self.B_xbuf], writes=[xk])

        def st1(b):
            self.transpose_strided(xbk[b % 3], xbT[b % 2], 8)

        def st2(b):
            xT = xbT[b % 2]
            p1, p3 = self.ps(), self.ps()
            self.mm_flat(xT, w1b[(b // 2) % NW13], 512, 0, p1, 8)
            self.mm_flat(xT, w3b[(b // 2) % NW13], 512, 0, p3, 8)
            s, g = s1[b % 2], gb[b % 2]
            S.op("act", lambda p1=p1, s=s: nc.scalar.activation(out=s[:], in_=p1[:], func=AF.Silu), reads=[p1], writes=[s])
            S.op("dve", lambda p3=p3, s=s, g=g: nc.vector.tensor_tensor(out=g[:], in0=p3[:], in1=s[:], op=ALU.mult), reads=[p3, s], writes=[g])

        def st3(b):
            self.transpose_strided(gb[b % 2], gT[b % 2], 4)

        def st4(b):
            y = yo[b % 2]
            for half in range(2):
                p = self.ps()
                self.mm_flat(gT[b % 2], w2b[(b // 2) % NW2], 1024, half * 512, p, 4)
                S.op("act", lambda p=p, half=half, y=y: nc.scalar.copy(out=y[:, half * 512:(half + 1) * 512], in_=p[:]), reads=[p], writes=[y])
            S.dma("sp", lambda y=y, b=b: nc.sync.dma_start(out=self.ybuf[rows(b):rows(b) + 128, :], in_=y[:]), reads=[y], writes=[self.B_ybuf], part=True)

        gather_w(w1b[0], v1, 0)
        gather_w(w3b[0], v3, 0)
        ldx(0)
        ldx(1)
        for t in range(nblk + 3):
            if t + 2 < nblk:
                ldx(t + 2)
            if t % 2 == 0:
                w = t // 2
                if w + 1 < nslot:
                    gather_w(w1b[(w + 1) % NW13], v1, w + 1)
                    gather_w(w3b[(w + 1) % NW13], v3, w + 1)
                if w < nslot:
                    gather_w(w2b[w % NW2], v2, w)
            if t < nblk:
                st1(t)
            if 0 <= t - 1 < nblk:
                st2(t - 1)
            if 0 <= t - 2 < nblk:
                st3(t - 2)
            if 0 <= t - 3 < nblk:
                st4(t - 3)
        self.arelease(mark_h2)
        RING = 4
        ya = [self.al("ya%d" % j, [128, D], F32) for j in range(RING)]
        yb_ = [self.al("yb%d" % j, [128, D], F32) for j in range(RING)]
        xc = [self.al("xc%d" % j, [128, D], F32) for j in range(3)]
        mo = [self.al("mo%d" % j, [128, D], F32) for j in range(2)]
        if final:
            S.dma("sp", lambda: nc.sync.dma_start(out=self.rowrep[:], in_=self.fng[0, :].partition_broadcast(128)), writes=[self.rowrep])

        def gat(i):
            for k, yy in enumerate((ya[i % RING], yb_[i % RING])):
                S.dma("pool", lambda yy=yy, i=i, k=k: nc.gpsimd.indirect_dma_start(out=yy[:], out_offset=None, in_=self.ybuf[:, :],
                                                                                  in_offset=bass.IndirectOffsetOnAxis(ap=Di[:, 2 * i + k:2 * i + k + 1], axis=0)),
                      reads=[self.B_ybuf, Di], writes=[yy])

        def ldc(i):
            x_ = xc[i % 3]
            S.dma("sp", lambda x_=x_, i=i: nc.sync.dma_start(out=x_[:], in_=xin[i * 128:(i + 1) * 128, :]), reads=[B_xin[i]], writes=[x_])
        for i in range(min(3, NT)):
            gat(i)
        for i in range(min(2, NT)):
            ldc(i)
        for i in range(NT):
            if i + 3 < NT:
                gat(i + 3)
            if i + 2 < NT:
                ldc(i + 2)
            m, x_, A_, B_ = mo[i % 2], xc[i % 3], ya[i % RING], yb_[i % RING]
            S.op("dve", lambda i=i, m=m, A_=A_: nc.vector.tensor_scalar(out=m[:], in0=A_[:], scalar1=W12[:, i, 0:1], scalar2=None, op0=ALU.mult), reads=[A_, W12], writes=[m])
            S.op("dve", lambda i=i, m=m, B_=B_: nc.vector.scalar_tensor_tensor(out=m[:], in0=B_[:], scalar=W12[:, i, 1:2], in1=m[:], op0=ALU.mult, op1=ALU.add), reads=[B_, W12, m], writes=[m])
            S.op("dve", lambda m=m: nc.vector.tensor_tensor(out=m[:], in0=m[:], in1=self.GATE[:], op=ALU.mult), reads=[m, self.GATE], writes=[m])
            S.op("dve", lambda m=m, x_=x_: nc.vector.tensor_tensor(out=m[:], in0=m[:], in1=x_[:], op=ALU.add), reads=[m, x_], writes=[m])
            if final:
                ss = self.sm("ss_f", [128, 1])
                rstd = self.sm("rstd_f", [128, 1])
                S.op("act", lambda m=m: nc.scalar.activation(out=self.junk[:], in_=m[:], func=AF.Square), reads=[m], writes=[self.junk])
                S.op("dve", lambda: nc.vector.reduce_sum(out=ss[:], in_=self.junk[:], axis=AX.X), reads=[self.junk], writes=[ss])
                self.rstd_from_ss(ss, D, rstd)
                S.op("dve", lambda m=m: nc.vector.scalar_tensor_tensor(out=m[:], in0=m[:], scalar=rstd[:, 0:1], in1=self.rowrep[:], op0=ALU.mult, op1=ALU.mult), reads=[m, rstd, self.rowrep], writes=[m])
            S.dma("sp", lambda i=i, m=m: nc.sync.dma_start(out=xout[i * 128:(i + 1) * 128, :], in_=m[:]), reads=[m], writes=[B_xout[i]])

    def build(self):
        cur, Bcur = self.x, self.B_x
        chain = [(self.xa, self.B_xa), (self.xb, self.B_xb), (self.xm, self.B_xm)]
        last_out = None
        for l in range(self.nlayers):
            if "A" in self.stages or "B" in self.stages:
                self.mod_half(l, 0, self.norm1_g)
            if "A" in self.stages:
                self.pass_a(l, cur, Bcur, self.xa, self.B_xa)
                self.arelease(0)
                last_out = (self.xa, self.B_xa)
            if "B" in self.stages:
                self.pass_b(l, self.xa, self.B_xa, self.xb, self.B_xb)
                self.arelease(0)
                last_out = (self.xb, self.B_xb)
            if "M" in self.stages:
                src = last_out if last_out is not None else (cur, Bcur)
                self.mod_half(l, 1, self.norm2_g)
                final = (l == self.nlayers - 1)
                dst = (self.out, self.B_out) if final else (self.xm, self.B_xm)
                self.moe(l, src[0], src[1], dst[0], dst[1], final)
                self.arelease(0)
                last_out = dst
            cur, Bcur = last_out
        self.last = last_out
        return self

    def finish(self, copy_last_to_out=False):
        S, nc = self.S, self.nc
        if copy_last_to_out and self.last[0] is not self.out:
            for i in range(self.NT):
                xt = self.xt[i % 2]
                S.dma("sp", lambda xt=xt, i=i: nc.sync.dma_start(out=xt[:], in_=self.last[0][i * 128:(i + 1) * 128, :]), reads=[self.last[1][i]], writes=[xt])
                S.dma("sp", lambda xt=xt, i=i: nc.sync.dma_start(out=self.out[i * 128:(i + 1) * 128, :], in_=xt[:]), reads=[xt], writes=[self.B_out[i]])
        S.wait_for("sp", self.B_out[:self.NT] + [self.B_dbg])
        S.emit()
        S.close()
        return nc


WEIGHT_NAMES = ["mod_w", "mod_b", "norm1_g", "w_in", "gate_w2", "gate_b", "gla_norm_g", "conv_w", "w_gla_out",
                "w_conv_out", "w_out", "norm2_g", "router_group_w", "router_group_b", "router_expert_w",
                "router_expert_b", "expert_w1", "expert_w3", "expert_w2"]


def make_in_maps(inputs, ncores=8):
    shared = {n: np.ascontiguousarray(np.asarray(inputs[n], dtype=np.float32)) for n in WEIGHT_NAMES}
    shared["final_norm_g"] = np.ascontiguousarray(np.asarray(inputs["final_norm_g"], dtype=np.float32).reshape(1, D))
    x = np.asarray(inputs["x"], dtype=np.float32)
    c = np.asarray(inputs["c"], dtype=np.float32)
    maps = []
    for b in range(ncores):
        m = dict(shared)
        m["x"] = np.ascontiguousarray(x[b])
        m["c"] = np.ascontiguousarray(c[b:b + 1])
        maps.append(m)
    return maps


def kernel(**inputs):
    nc = K().build().finish()
    res = run_bass_kernel_spmd(nc, make_in_maps(inputs), core_ids=list(range(8)))
    return np.stack([np.asarray(r["out"], dtype=np.float32) for r in res.results], axis=0)
```

```python
import numpy as np
import concourse.bass as bass
import concourse.mybir as mybir
from concourse.bass_utils import run_bass_kernel_spmd

F32 = mybir.dt.float32
BF16 = mybir.dt.bfloat16
I32 = mybir.dt.int32
ALU = mybir.AluOpType
AF = mybir.ActivationFunctionType
AX = mybir.AxisListType

D = 1024
SEQ = 4096
NTILE = SEQ // 128
DEPTH = 2
NE = 64
DEXP = 512
SROWS = 256
NBLK = 96
NSLOT = NBLK * SROWS
EPS = 1e-6
OOB_BIG = 16777216.0
OFF = dict(q=0, k=512, v=1024, gdn=2048, r=2064, cb=3088, cc=4112, ch=5136, ga=6160, gc=7184)
COMPUTE = ("pe", "act", "dve", "pool")


class Buf:
    __slots__ = ("name", "writer", "readers", "wsem", "wcnt", "rsem", "rcnt", "t")

    def __init__(self, name, t=None):
        self.name = name
        self.t = t
        self.writer = None
        self.readers = {}
        self.wsem = None
        self.wcnt = 0
        self.rsem = None
        self.rcnt = 0

    def __getitem__(self, idx):
        return self.t[idx]


class Sched:
    def __init__(self, nc, strict=True):
        self.nc = nc
        self.strict = strict
        self.eng = {"pe": nc.tensor, "act": nc.scalar, "dve": nc.vector,
                    "pool": nc.gpsimd, "sp": nc.sync}
        self.prog = {k: [] for k in self.eng}
        self.cnt = {k: 0 for k in COMPUTE}
        self.sem = {}
        self.seen = {k: {} for k in self.eng}
        self.stack = []
        self.serial = False
        self.dsem = {}
        for k in COMPUTE:
            self.sem[k] = self.new_sem("c_" + k)
        self.nps = 0
        self.npb = 0

    def new_sem(self, name):
        cm = self.nc.semaphore(name)
        s = cm.__enter__()
        self.stack.append(cm)
        return s

    def sbuf(self, name, shape, dt):
        cm = self.nc.sbuf_tensor(name, list(shape), dt)
        t = cm.__enter__()
        self.stack.append(cm)
        return Buf(name, t)

    def psum(self, name, shape, dt):
        cm = self.nc.psum_tensor(name, list(shape), dt)
        t = cm.__enter__()
        self.stack.append(cm)
        return Buf(name, t)

    def _wait(self, q, semkey, sem, val):
        seen = self.seen[q]
        if seen.get(semkey, 0) >= val:
            return
        seen[semkey] = val
        e = self.eng[q]
        self.prog[q].append(lambda e=e, sem=sem, val=val: (e.wait_ge(sem, val), None)[1])

    def _dep(self, q, d):
        if d is None:
            return
        if d[0] == "eng":
            _, k, c = d
            if k == q and (k == "pe" or not self.strict):
                return
            self._wait(q, k, self.sem[k], c)
        else:
            sem, n = self.dsem[d[1]]
            self._wait(q, d[1], sem, 16 * n)

    @staticmethod
    def _addreader(b, tag):
        b.readers[tag[1]] = tag

    def _serial(self, q):
        for k in COMPUTE:
            if self.cnt[k] > 0 and k != q:
                self._wait(q, k, self.sem[k], self.cnt[k])
        for key, (sem, n) in self.dsem.items():
            self._wait(q, key, sem, 16 * n)

    def op(self, q, fn, reads=(), writes=()):
        if self.serial:
            self._serial(q)
        for b in reads:
            self._dep(q, b.writer)
        for b in writes:
            self._dep(q, b.writer)
            for r in b.readers.values():
                self._dep(q, r)
        self.cnt[q] += 1
        c = self.cnt[q]
        sem = self.sem[q]
        self.prog[q].append(lambda fn=fn, sem=sem: fn().then_inc(sem, 1))
        tag = ("eng", q, c)
        for b in reads:
            self._addreader(b, tag)
        for b in writes:
            b.writer = tag
            b.readers = {}

    def dma(self, q, fn, reads=(), writes=(), part=False):
        if self.serial:
            self._serial(q)
        for b in reads:
            self._dep(q, b.writer)
        for b in writes:
            if not (part and b.writer is not None and b.writer[0] == "dma"):
                self._dep(q, b.writer)
            for r in b.readers.values():
                self._dep(q, r)
        if writes and writes[0].t is not None:
            o = writes[0]
            key = "w_" + o.name
        else:
            o = [b for b in reads if b.t is not None][0]
            key = "r_" + o.name
        if key not in self.dsem:
            self.dsem[key] = [self.new_sem(key), 0]
        ent = self.dsem[key]
        ent[1] += 1
        sem = ent[0]
        self.prog[q].append(lambda fn=fn, sem=sem: fn().then_inc(sem, 16))
        tag = ("dma", key)
        for b in reads:
            self._addreader(b, tag)
        for b in writes:
            b.writer = tag
            b.readers = {}
        return tag

    def barrier(self):
        for q in self.eng:
            for k in COMPUTE:
                if self.cnt[k] > 0 and (k != q or (self.strict and k != "pe")):
                    self._wait(q, k, self.sem[k], self.cnt[k])
            for key, (sem, n) in self.dsem.items():
                self._wait(q, key, sem, 16 * n)

    def raw(self, q, fn):
        self.prog[q].append(lambda fn=fn: (fn(), None)[1])

    def wait_for(self, q, bufs):
        for b in bufs:
            self._dep(q, b.writer)
            for r in b.readers.values():
                self._dep(q, r)

    def emit(self):
        with self.nc.Block() as block:
            for k, attr in (("sp", "sync"), ("act", "scalar"), ("dve", "vector"),
                            ("pool", "gpsimd"), ("pe", "tensor")):
                lst = self.prog[k]

                def body(_e, lst=lst):
                    for f in lst:
                        f()
                getattr(block, attr)(body)

    def close(self):
        while self.stack:
            self.stack.pop().__exit__(None, None, None)


class K:
    def __init__(self, nlayers=DEPTH, ntile=NTILE, stages=("A", "B", "M"), dbg=None, strict=True, serial=False):
        self.nlayers = nlayers
        self.NT = ntile
        self.stages = stages
        self.dbg = dbg or {}
        nc = self.nc = bass.Bass("TRN2", target_bir_lowering=False)
        S = self.S = Sched(nc, strict=strict)
        S.serial = serial
        L = DEPTH

        def inp(name, shape, dt=F32):
            return nc.dram_tensor(name, list(shape), dt, kind="ExternalInput").ap()
        self.x = inp("x", [SEQ, D])
        self.c = inp("c", [1, D])
        self.mod_w = inp("mod_w", [L, D, 6 * D])
        self.mod_b = inp("mod_b", [L, 6 * D])
        self.norm1_g = inp("norm1_g", [L, D])
        self.w_in = inp("w_in", [L, D, 8208])
        self.gate_w2 = inp("gate_w2", [L, 16, 512])
        self.gate_b = inp("gate_b", [L, 512])
        self.gla_norm_g = inp("gla_norm_g", [L, D])
        self.conv_w = inp("conv_w", [L, 3, D])
        self.w_gla_out = inp("w_gla_out", [L, D, D])
        self.w_conv_out = inp("w_conv_out", [L, D, D])
        self.w_out = inp("w_out", [L, D, D])
        self.norm2_g = inp("norm2_g", [L, D])
        self.rg_w = inp("router_group_w", [L, D, 8])
        self.rg_b = inp("router_group_b", [L, 8])
        self.re_w = inp("router_expert_w", [L, D, 64])
        self.re_b = inp("router_expert_b", [L, 64])
        self.ew1 = inp("expert_w1", [L, NE, D, DEXP])
        self.ew3 = inp("expert_w3", [L, NE, D, DEXP])
        self.ew2 = inp("expert_w2", [L, NE, DEXP, D])
        self.fng = inp("final_norm_g", [1, D])
        self.out = nc.dram_tensor("out", [SEQ, D], F32, kind="ExternalOutput").ap()

        def scr(name, shape, dt=F32):
            return nc.dram_tensor(name, list(shape), dt, kind="Internal").ap()
        self.xa = scr("xa", [SEQ, D])
        self.xb = scr("xb", [SEQ, D])
        self.xm = scr("xm", [SEQ, D])
        self.hTd = scr("hTd", [NTILE, 128, D], BF16)
        self.xbuf = scr("xbuf", [NSLOT, D], BF16)
        self.ybuf = scr("ybuf", [NSLOT, D], F32)
        self.B_x = [Buf("x%d" % i) for i in range(NTILE)]
        self.B_xa = [Buf("xa%d" % i) for i in range(NTILE)]
        self.B_xb = [Buf("xb%d" % i) for i in range(NTILE)]
        self.B_xm = [Buf("xm%d" % i) for i in range(NTILE)]
        self.B_hT = [Buf("hT%d" % i) for i in range(NTILE)]
        self.B_out = [Buf("out%d" % i) for i in range(NTILE)]
        self.B_xbuf = Buf("xbuf")
        self.B_ybuf = Buf("ybuf")
        self.B_dbg = Buf("dbg")
        self.dbg_out = {}
        for name, (shape, dt) in self.dbg.items():
            self.dbg_out[name] = nc.dram_tensor("dbg_" + name, list(shape), dt, kind="ExternalOutput").ap()

        self.PS = [S.psum("ps%d" % i, [128, 512], F32) for i in range(6)]
        self.PB = [S.psum("pb%d" % i, [128, 1024], BF16) for i in range(2)]
        self._ips = 0
        self._ipb = 0
        self.ARENA = 164 * 1024
        self.arena = S.sbuf("arena", [128, self.ARENA // 2], BF16)
        self.aoff = 0
        self.consts()
        self.common = {}

    def ps(self):
        b = self.PS[self._ips % len(self.PS)]
        self._ips += 1
        return b

    def pb(self):
        b = self.PB[self._ipb % len(self.PB)]
        self._ipb += 1
        return b

    def tap(self, name, buf, ap=None):
        if name not in self.dbg_out:
            return
        S, nc = self.S, self.nc
        dst = self.dbg_out[name]
        src = buf.t[:] if ap is None else ap
        S.dma("sp", lambda: nc.sync.dma_start(out=dst, in_=src), reads=[buf], writes=[self.B_dbg], part=True)

    def consts(self):
        S, nc = self.S, self.nc
        io = S.sbuf("io", [128, 128], I32)
        iof = S.sbuf("iof", [128, 128], F32)
        self.ident = S.sbuf("ident", [128, 128], BF16)
        self.identf = S.sbuf("identf", [128, 128], F32)
        self.Uinc = S.sbuf("Uinc", [128, 128], F32)
        self.Urev = S.sbuf("Urev", [128, 128], F32)
        self.Ltri = S.sbuf("Ltri", [128, 128], F32)
        self.ones = S.sbuf("ones", [128, 128], F32)
        self.mask4 = S.sbuf("mask4", [128, 512], F32)
        S.op("pool", lambda: nc.gpsimd.iota(io[:], pattern=[[1, 128]], base=0, channel_multiplier=-1), writes=[io])
        S.op("dve", lambda: nc.vector.tensor_copy(out=iof[:], in_=io[:]), reads=[io], writes=[iof])
        S.op("dve", lambda: nc.vector.tensor_single_scalar(out=self.ident[:], in_=iof[:], scalar=0.0, op=ALU.is_equal), reads=[iof], writes=[self.ident])
        S.op("dve", lambda: nc.vector.tensor_single_scalar(out=self.identf[:], in_=iof[:], scalar=0.0, op=ALU.is_equal), reads=[iof], writes=[self.identf])
        S.op("dve", lambda: nc.vector.tensor_single_scalar(out=self.Ltri[:], in_=iof[:], scalar=0.0, op=ALU.is_ge), reads=[iof], writes=[self.Ltri])
        S.op("dve", lambda: nc.vector.tensor_scalar(out=self.Uinc[:], in0=iof[:], scalar1=0.0, scalar2=-1.0 / 16, op0=ALU.is_ge, op1=ALU.mult), reads=[iof], writes=[self.Uinc])
        S.op("dve", lambda: nc.vector.tensor_scalar(out=self.Urev[:], in0=iof[:], scalar1=0.0, scalar2=-1.0 / 16, op0=ALU.is_lt, op1=ALU.mult), reads=[iof], writes=[self.Urev])
        S.op("dve", lambda: nc.vector.memset(self.ones[:], 1.0), writes=[self.ones])
        for h in range(4):
            S.op("dve", lambda h=h: nc.vector.tensor_copy(out=self.mask4[:, h * 128:(h + 1) * 128], in_=self.Ltri[:]), reads=[self.Ltri], writes=[self.mask4])
        self.io_ = io
        cT = S.sbuf("cT", [128, 8], F32)
        self.scT = S.sbuf("scT", [128, 8, 128], F32)
        S.dma("sp", lambda: nc.sync.dma_start(out=cT[:], in_=self.c.rearrange("o (k p) -> p (o k)", p=128), allow_slow_non_contiguous=True), writes=[cT])
        S.op("act", lambda: nc.scalar.activation(out=cT[:], in_=cT[:], func=AF.Silu), reads=[cT], writes=[cT])
        S.op("dve", lambda: nc.vector.tensor_copy(out=self.scT[:], in_=cT[:].unsqueeze(2).to_broadcast([128, 8, 128])), reads=[cT], writes=[self.scT])
        self._imod = 0
        self.SH = S.sbuf("SH", [128, D], F32)
        self.G = S.sbuf("G", [128, D], F32)
        self.GATE = S.sbuf("GATE", [128, D], F32)
        self.xt = [S.sbuf("xt%d" % i, [128, D], F32) for i in range(2)]
        self.junk = S.sbuf("junk", [128, D], BF16)
        self.t32 = [S.sbuf("t32_%d" % i, [128, D], F32) for i in range(2)]
        self.rowrep = self.t32[0]
        self.small = {}

    def al(self, name, shape, dt):
        esz = 2 if dt == BF16 else 4
        n = 1
        for s in shape[1:]:
            n *= s
        nbytes = n * esz
        off = self.aoff
        self.aoff += (nbytes + 31) // 32 * 32
        assert self.aoff <= self.ARENA, (name, self.aoff)
        ap = self.arena.t[0:shape[0], off // 2:(off + nbytes) // 2]
        if dt != BF16:
            ap = ap.bitcast(dt)
        if len(shape) == 3:
            ap = ap.rearrange("p (a b) -> p a b", a=shape[1])
        return Buf(name, ap)

    def arelease(self, mark=0):
        self.S.barrier()
        self.aoff = mark

    def sm(self, name, shape, dt=F32):
        if name not in self.small:
            self.small[name] = self.S.sbuf(name, shape, dt)
        return self.small[name]

    def mod_half(self, l, half, norm_g):
        S, nc = self.S, self.nc
        dsts = [self.SH, self.G, self.GATE]
        mark = self.aoff
        modw = [self.al("modw%d" % i, [128, 8, 512], F32) for i in range(2)]
        modbias = [self.al("modbias%d" % i, [128, 512], F32) for i in range(2)]
        S.dma("sp", lambda: nc.sync.dma_start(out=self.rowrep[:], in_=norm_g[l, :].partition_broadcast(128)), writes=[self.rowrep])
        for j in range(6):
            col = half * 3072 + j * 512
            wb = modw[self._imod % 2]
            bb = modbias[self._imod % 2]
            self._imod += 1
            S.dma("sp", lambda wb=wb, col=col: nc.sync.dma_start(out=wb[:], in_=self.mod_w[l, :, col:col + 512].rearrange("(k p) n -> p k n", p=128)), writes=[wb])
            S.dma("sp", lambda bb=bb, col=col: nc.sync.dma_start(out=bb[:], in_=self.mod_b[l, col:col + 512].partition_broadcast(128)), writes=[bb])
            p = self.ps()

            def mm(p=p, wb=wb):
                for k in range(8):
                    i = nc.tensor.matmul(out=p[:], lhsT=self.scT[:, k, :], rhs=wb[:, k, :], start=(k == 0), stop=(k == 7))
                return i
            S.op("pe", mm, reads=[self.scT, wb], writes=[p])
            dst = dsts[j // 2]
            sl = slice((j % 2) * 512, (j % 2) * 512 + 512)
            S.op("dve", lambda p=p, bb=bb, dst=dst, sl=sl: nc.vector.tensor_tensor(out=dst[:, sl], in0=p[:], in1=bb[:], op=ALU.add), reads=[p, bb], writes=[dst])
        S.op("dve", lambda: nc.vector.scalar_tensor_tensor(out=self.G[:], in0=self.G[:], scalar=1.0, in1=self.rowrep[:], op0=ALU.add, op1=ALU.mult), reads=[self.G, self.rowrep], writes=[self.G])
        self.arelease(mark)

    def load_w(self, dst, src_rows_cols, dst_sl=None):
        S, nc = self.S, self.nc
        n = src_rows_cols.shape[1]
        o0 = 0 if dst_sl is None else dst_sl
        for c0 in range(0, n, 512):
            c1 = min(n, c0 + 512)
            S.dma("pool", lambda c0=c0, c1=c1: nc.gpsimd.dma_start(out=dst[:, :, o0 + c0:o0 + c1], in_=src_rows_cols[:, c0:c1].rearrange("(k p) n -> p k n", p=128)), writes=[dst], part=True)

    def rstd_from_ss(self, ss, n, rstd):
        S, nc = self.S, self.nc
        S.op("act", lambda: nc.scalar.activation(out=rstd[:], in_=ss[:], func=AF.Sqrt, scale=1.0 / n, bias=EPS), reads=[ss], writes=[rstd])
        S.op("dve", lambda: nc.vector.reciprocal(out=rstd[:], in_=rstd[:]), reads=[rstd], writes=[rstd])

    def norm_mod(self, xt, hout, key):
        S, nc = self.S, self.nc
        ss = self.sm("ss_" + key, [128, 1])
        rstd = self.sm("rstd_" + key, [128, 1])
        S.op("act", lambda: nc.scalar.activation(out=self.junk[:], in_=xt[:], func=AF.Square), reads=[xt], writes=[self.junk])
        S.op("dve", lambda: nc.vector.reduce_sum(out=ss[:], in_=self.junk[:], axis=AX.X), reads=[self.junk], writes=[ss])
        self.rstd_from_ss(ss, D, rstd)
        t = self.t32[0]
        S.op("dve", lambda: nc.vector.scalar_tensor_tensor(out=t[:], in0=xt[:], scalar=rstd[:, 0:1], in1=self.G[:], op0=ALU.mult, op1=ALU.mult), reads=[xt, rstd, self.G], writes=[t])
        S.op("dve", lambda: nc.vector.tensor_tensor(out=hout[:], in0=t[:], in1=self.SH[:], op=ALU.add), reads=[t, self.SH], writes=[hout])

    def transpose8(self, src, dst, n=8):
        S, nc = self.S, self.nc
        p = self.pb()

        def tr():
            for k in range(n):
                i = nc.tensor.transpose(out=p[:, k * 128:(k + 1) * 128], in_=src[:, k * 128:(k + 1) * 128], identity=self.ident[:])
            return i
        S.op("pe", tr, reads=[src, self.ident], writes=[p])
        S.op("act", lambda: nc.scalar.copy(out=dst[:, 0:n * 128], in_=p[:, 0:n * 128]), reads=[p], writes=[dst])

    def transpose_strided(self, src, dst, n):
        S, nc = self.S, self.nc
        p = self.pb()

        def tr():
            for j in range(n):
                i = nc.tensor.transpose(out=p[:, j * 128:(j + 1) * 128], in_=src[:, j:n * 128:n], identity=self.ident[:])
            return i
        S.op("pe", tr, reads=[src, self.ident], writes=[p])
        S.op("act", lambda: nc.scalar.copy(out=dst[:, 0:n * 128], in_=p[:, 0:n * 128]), reads=[p], writes=[dst])

    def mm_flat(self, lhsT, W, rowlen, col, out_ps, nk):
        nc = self.nc

        def mm():
            for j in range(nk):
                i = nc.tensor.matmul(out=out_ps[:], lhsT=lhsT[:, j * 128:(j + 1) * 128], rhs=W[:, j * rowlen + col:j * rowlen + col + 512], start=(j == 0), stop=(j == nk - 1))
            return i
        self.S.op("pe", mm, reads=[lhsT, W], writes=[out_ps])

    def mm_tok(self, lhsT, W, col, out_ps, nk=8):
        nc = self.nc

        def mm():
            for k in range(nk):
                i = nc.tensor.matmul(out=out_ps[:], lhsT=lhsT[:, k * 128:(k + 1) * 128], rhs=W[:, k, col:col + 512], start=(k == 0), stop=(k == nk - 1))
            return i
        self.S.op("pe", mm, reads=[lhsT, W], writes=[out_ps])

    def out_proj_residual(self, yb, xin, gate_first, xout_dram, B_out, i):
        S, nc = self.S, self.nc
        yT = self.sm("yT", [128, D], BF16)
        self.transpose8(yb, yT)
        xo = self.t32[1]
        for half in range(2):
            p = self.ps()
            self.mm_tok(yT, self.WO, half * 512, p)
            sl = slice(half * 512, half * 512 + 512)
            S.op("dve", lambda p=p, sl=sl: nc.vector.tensor_tensor(out=xo[:, sl], in0=p[:], in1=self.GATE[:, sl], op=ALU.mult), reads=[p, self.GATE], writes=[xo])
        S.op("pool", lambda: nc.gpsimd.tensor_tensor(out=xo[:], in0=xo[:], in1=xin[:], op=ALU.add), reads=[xo, xin], writes=[xo])
        S.dma("sp", lambda: nc.sync.dma_start(out=xout_dram[i * 128:(i + 1) * 128, :], in_=xo[:]), reads=[xo], writes=[B_out[i]])

    def pass_a(self, l, xin, B_xin, xout, B_xout):
        S, nc = self.S, self.nc
        NT = self.NT
        WA = self.al("WA", [128, 8, 4112], BF16)
        self.W2 = self.al("W2", [128, 8, D], BF16)
        self.WO = self.al("WO", [128, 8, D], BF16)
        wi = self.w_in[l]
        self.load_w(WA, wi[:, 0:2048], 0)
        self.load_w(WA, wi[:, 2064:3088], 2048)
        self.load_w(WA, wi[:, 6160:7184], 3072)
        self.load_w(WA, wi[:, 2048:2064], 4096)
        self.load_w(self.W2, self.w_gla_out[l])
        self.load_w(self.WO, self.w_out[l])
        cq, ck, cv, cr, cga, cgd = 0, 512, 1024, 2048, 3072, 4096
        gw = self.al("gw", [32, 512], F32)
        S.op("dve", lambda: nc.vector.memset(gw[:], 0.0), writes=[gw])
        S.dma("sp", lambda: nc.sync.dma_start(out=gw[0:16, :], in_=self.gate_w2[l]), writes=[gw])
        S.dma("sp", lambda: nc.sync.dma_start(out=gw[16:17, :], in_=self.gate_b[l:l + 1, :]), writes=[gw], part=True)
        gng = self.al("gng", [128, D], F32)
        S.dma("sp", lambda: nc.sync.dma_start(out=gng[:], in_=self.gla_norm_g[l, :].partition_broadcast(128)), writes=[gng])
        St = self.al("St", [128, 4, 256], F32)
        Sb = self.al("Sb", [128, 4, 256], BF16)
        S.op("dve", lambda: nc.vector.memset(St[:], 0.0), writes=[St])
        S.op("dve", lambda: nc.vector.memset(Sb[:], 0.0), writes=[Sb])
        gda = [self.al("gda%d" % j, [32, 128], F32) for j in range(2)]
        for j in range(2):
            S.op("dve", lambda j=j: nc.vector.memset(gda[j][:], 0.0), writes=[gda[j]])
            S.dma("sp", lambda j=j: nc.sync.dma_start(out=gda[j][16:17, :], in_=self.ones[0:1, :]), reads=[self.ones], writes=[gda[j]])
        hb = self.al("hb", [128, D], BF16)
        hT = [self.al("hT%d" % j, [128, D], BF16) for j in range(2)]
        xts = [self.xt[0], self.xt[1], self.al("xt2", [128, D], F32)]
        qraw = [self.al("qraw%d" % j, [128, 512], F32) for j in range(2)]
        kraw = [self.al("kraw%d" % j, [128, 512], F32) for j in range(2)]
        ktok = [self.al("ktok%d" % j, [128, 512], BF16) for j in range(2)]
        vb = [self.al("vb%d" % j, [128, D], BF16) for j in range(2)]
        sr = [self.al("sr%d" % j, [128, D], BF16) for j in range(2)]
        sga = [self.al("sga%d" % j, [128, D], BF16) for j in range(2)]
        sp_ = self.al("sp_", [128, 512], F32)
        Eb = self.al("Eb", [128, 512], F32)
        Enb = self.al("Enb", [128, 512], F32)
        Es = self.al("Es", [128, 512], F32)
        qTt = self.al("qTt", [128, 512], BF16)
        kTt = self.al("kTt", [128, 512], BF16)
        ks = self.al("ks", [128, 512], BF16)
        am = self.al("am", [128, 512], BF16)
        on = self.al("on", [128, D], BF16)
        og = self.al("og", [128, D], BF16)
        ogT = self.al("ogT", [128, D], BF16)
        yg = self.al("yg", [128, D], BF16)
        junk2 = self.al("junk2", [128, D], BF16)
        ss4 = self.al("ss4", [128, 4], F32)
        rs4 = self.al("rs4", [128, 4], F32)
        xps = [self.PS[4], self.PS[5]]
        yps = self.PS[0:4]
        cnt = {"x": 0, "y": 0}

        def px():
            cnt["x"] += 1
            return xps[cnt["x"] % 2]

        def py():
            cnt["y"] += 1
            return yps[cnt["y"] % 4]

        def mmk(lhsT, col, p):
            def mm():
                for k in range(8):
                    ins = nc.tensor.matmul(out=p[:], lhsT=lhsT[:, k * 128:(k + 1) * 128], rhs=WA[:, k, col:col + 512], start=(k == 0), stop=(k == 7))
                return ins
            S.op("pe", mm, reads=[lhsT, WA], writes=[p])

        def fm(p, c0, h_T, m=128, nh=4):
            def f():
                for h in range(nh):
                    for k in range(8):
                        ins = nc.tensor.matmul(out=p[0:m, h * 128:(h + 1) * 128], lhsT=WA[:, k, c0 + h * m:c0 + (h + 1) * m],
                                               rhs=h_T[:, k * 128:(k + 1) * 128], start=(k == 0), stop=(k == 7))
                return ins
            S.op("pe", f, reads=[WA, h_T], writes=[p])

        def xsteps(i):
            j = i % 2
            xt, h_T = xts[i % 3], hT[j]
            st = []

            def x0a():
                S.dma("sp", lambda: nc.sync.dma_start(out=xt[:], in_=xin[i * 128:(i + 1) * 128, :]), reads=[B_xin[i]], writes=[xt])
                self.norm_mod(xt, hb, "a")
            st.append(x0a)

            def x0b():
                self.transpose8(hb, h_T)
                S.dma("sp", lambda: nc.sync.dma_start(out=self.hTd[i], in_=h_T[:]), reads=[h_T], writes=[self.B_hT[i]])
            st.append(x0b)

            def x1():
                p = px()
                fm(p, cq, h_T)
                S.op("act", lambda: nc.scalar.copy(out=qraw[j][:], in_=p[:]), reads=[p], writes=[qraw[j]])
            st.append(x1)

            def x2():
                p = px()
                fm(p, ck, h_T)
                S.op("dve", lambda: nc.vector.tensor_copy(out=kraw[j][:], in_=p[:]), reads=[p], writes=[kraw[j]])
            st.append(x2)

            def x3():
                p = px()
                fm(p, cgd, h_T, m=16, nh=1)
                S.op("act", lambda: nc.scalar.copy(out=gda[j][0:16, :], in_=p[0:16, 0:128]), reads=[p], writes=[gda[j]])
            st.append(x3)

            def x4():
                p = px()
                mmk(h_T, ck, p)
                S.op("dve", lambda: nc.vector.tensor_copy(out=ktok[j][:], in_=p[:]), reads=[p], writes=[ktok[j]])
            st.append(x4)
            for half in range(2):
                def xv(half=half):
                    p = px()
                    mmk(h_T, cv + half * 512, p)
                    S.op("act", lambda: nc.scalar.copy(out=vb[j][:, half * 512:(half + 1) * 512], in_=p[:]), reads=[p], writes=[vb[j]])
                st.append(xv)
            for half in range(2):
                def xr(half=half):
                    p = px()
                    mmk(h_T, cr + half * 512, p)
                    S.op("act", lambda: nc.scalar.activation(out=sr[j][:, half * 512:(half + 1) * 512], in_=p[:], func=AF.Silu), reads=[p], writes=[sr[j]])
                st.append(xr)
            for half in range(2):
                def xg(half=half):
                    p = px()
                    mmk(h_T, cga + half * 512, p)
                    S.op("act", lambda: nc.scalar.activation(out=sga[j][:, half * 512:(half + 1) * 512], in_=p[:], func=AF.Sigmoid), reads=[p], writes=[sga[j]])
                st.append(xg)
            return st

        def ysteps(i):
            j = i % 2
            xt = xts[i % 3]
            st = []

            def y0():
                plg = py()
                S.op("pe", lambda: nc.tensor.matmul(out=plg[:], lhsT=gda[j][:], rhs=gw[:], start=True, stop=True), reads=[gda[j], gw], writes=[plg])
                S.op("act", lambda: nc.scalar.activation(out=Es[:], in_=plg[:], func=AF.Exp, scale=-1.0), reads=[plg], writes=[Es])
                S.op("act", lambda: nc.scalar.activation(out=sp_[:], in_=Es[:], func=AF.Ln, bias=1.0), reads=[Es], writes=[sp_])
            st.append(y0)

            def y1():
                pbT, prev = py(), py()

                def cum1():
                    for h in range(4):
                        ins = nc.tensor.matmul(out=pbT[:, h * 128:(h + 1) * 128], lhsT=sp_[:, h * 128:(h + 1) * 128], rhs=self.Uinc[:], start=True, stop=True)
                    return ins
                S.op("pe", cum1, reads=[sp_, self.Uinc], writes=[pbT])
                S.op("pe", lambda: nc.tensor.matmul(out=prev[:], lhsT=self.Urev[:], rhs=sp_[:], start=True, stop=True), reads=[sp_, self.Urev], writes=[prev])
                S.op("act", lambda: nc.scalar.activation(out=Eb[:], in_=pbT[:], func=AF.Exp), reads=[pbT], writes=[Eb])
                S.op("act", lambda: nc.scalar.activation(out=Enb[:], in_=pbT[:], func=AF.Exp, scale=-1.0), reads=[pbT], writes=[Enb])
                S.op("act", lambda: nc.scalar.activation(out=Es[:], in_=prev[:], func=AF.Exp), reads=[prev], writes=[Es])
            st.append(y1)

            def y2():
                S.op("dve", lambda: nc.vector.scalar_tensor_tensor(out=qTt[:], in0=qraw[j][:], scalar=128 ** -0.5, in1=Eb[:], op0=ALU.mult, op1=ALU.mult), reads=[qraw[j], Eb], writes=[qTt])
                S.op("dve", lambda: nc.vector.tensor_tensor(out=kTt[:], in0=kraw[j][:], in1=Enb[:], op=ALU.mult), reads=[kraw[j], Enb], writes=[kTt])
                S.op("pool", lambda: nc.gpsimd.tensor_tensor(out=ks[:], in0=ktok[j][:], in1=Es[:], op=ALU.mult), reads=[ktok[j], Es], writes=[ks])
            st.append(y2)

            def y3():
                pat = py()

                def att():
                    for h in range(4):
                        ins = nc.tensor.matmul(out=pat[:, h * 128:(h + 1) * 128], lhsT=kTt[:, h * 128:(h + 1) * 128], rhs=qTt[:, h * 128:(h + 1) * 128], start=True, stop=True)
                    return ins
                S.op("pe", att, reads=[kTt, qTt], writes=[pat])
                S.op("dve", lambda: nc.vector.tensor_tensor(out=am[:], in0=pat[:], in1=self.mask4[:], op=ALU.mult), reads=[pat, self.mask4], writes=[am])
            st.append(y3)
            hold = {}

            def y4():
                po = [py(), py()]
                pds = [py(), py()]
                hold["po"] = po

                def omm():
                    for h in range(4):
                        o_ap = po[h // 2][:, (h % 2) * 256:(h % 2) * 256 + 256]
                        nc.tensor.matmul(out=o_ap, lhsT=am[:, h * 128:(h + 1) * 128], rhs=vb[j][:, h * 256:(h + 1) * 256], start=True, stop=False)
                        ins = nc.tensor.matmul(out=o_ap, lhsT=qTt[:, h * 128:(h + 1) * 128], rhs=Sb[:, h, :], start=False, stop=True)
                    return ins
                S.op("pe", omm, reads=[am, vb[j], qTt, Sb], writes=po)

                def dsm():
                    for h in range(4):
                        ins = nc.tensor.matmul(out=pds[h // 2][:, (h % 2) * 256:(h % 2) * 256 + 256], lhsT=ks[:, h * 128:(h + 1) * 128], rhs=vb[j][:, h * 256:(h + 1) * 256], start=True, stop=True)
                    return ins
                S.op("pe", dsm, reads=[ks, vb[j]], writes=pds)
                for h in range(4):
                    S.op("dve", lambda h=h: nc.vector.scalar_tensor_tensor(out=St[:, h, :], in0=St[:, h, :], scalar=Eb[:, h * 128 + 127:h * 128 + 128],
                                                                            in1=pds[h // 2][:, (h % 2) * 256:(h % 2) * 256 + 256], op0=ALU.mult, op1=ALU.add),
                         reads=[St, Eb, pds[h // 2]], writes=[St])
                S.op("pool", lambda: nc.gpsimd.tensor_copy(out=Sb[:], in_=St[:]), reads=[St], writes=[Sb])
            st.append(y4)

            def y5():
                po = hold["po"]
                for hh in range(2):
                    S.op("act", lambda hh=hh: nc.scalar.activation(out=junk2[:, hh * 512:(hh + 1) * 512], in_=po[hh][:], func=AF.Square), reads=[po[hh]], writes=[junk2])
                S.op("dve", lambda: nc.vector.reduce_sum(out=ss4[:], in_=junk2[:].rearrange("p (h v) -> p h v", h=4), axis=AX.X), reads=[junk2], writes=[ss4])
                self.rstd_from_ss(ss4, 256, rs4)
                for h in range(4):
                    S.op("dve", lambda h=h: nc.vector.scalar_tensor_tensor(out=on[:, h * 256:(h + 1) * 256], in0=po[h // 2][:, (h % 2) * 256:(h % 2) * 256 + 256], scalar=rs4[:, h:h + 1],
                                                                            in1=gng[:, h * 256:(h + 1) * 256], op0=ALU.mult, op1=ALU.mult), reads=[po[h // 2], rs4, gng], writes=[on])
                S.op("pool", lambda: nc.gpsimd.tensor_tensor(out=og[:], in0=on[:], in1=sr[j][:], op=ALU.mult), reads=[on, sr[j]], writes=[og])
            st.append(y5)

            def y6a():
                self.transpose8(og, ogT)
            st.append(y6a)

            def y6b():
                for half in range(2):
                    p = py()
                    self.mm_tok(ogT, self.W2, half * 512, p)
                    sl = slice(half * 512, half * 512 + 512)
                    S.op("dve", lambda p=p, sl=sl: nc.vector.tensor_tensor(out=yg[:, sl], in0=p[:], in1=sga[j][:, sl], op=ALU.mult), reads=[p, sga[j]], writes=[yg])
            st.append(y6b)

            yT = self.sm("yT", [128, D], BF16)

            def y7a():
                self.transpose8(yg, yT)
            st.append(y7a)

            def y7b():
                xo = self.t32[1]
                for half in range(2):
                    p = py()
                    self.mm_tok(yT, self.WO, half * 512, p)
                    sl = slice(half * 512, half * 512 + 512)
                    S.op("dve", lambda p=p, sl=sl: nc.vector.tensor_tensor(out=xo[:, sl], in0=p[:], in1=self.GATE[:, sl], op=ALU.mult), reads=[p, self.GATE], writes=[xo])
                S.op("pool", lambda: nc.gpsimd.tensor_tensor(out=xo[:], in0=xo[:], in1=xt[:], op=ALU.add), reads=[xo, xt], writes=[xo])
                S.dma("sp", lambda: nc.sync.dma_start(out=xout[i * 128:(i + 1) * 128, :], in_=xo[:]), reads=[xo], writes=[B_xout[i]])
            st.append(y7b)
            return st

        Y = [ysteps(i) for i in range(NT)]
        X = [xsteps(i) for i in range(NT)]
        for f in X[0]:
            f()
        for i in range(-1, NT):
            a, bq, c = i, i + 1, i + 2
            ha, hb_, hc = (a >= 0), (bq < NT), (c < NT)
            if hc:
                X[c][0]()
            if ha:
                Y[a][6]()
            if hb_:
                Y[bq][0]()
            if ha:
                Y[a][7]()
            if hb_:
                Y[bq][1]()
            if ha:
                Y[a][8]()
                Y[a][9]()
            if hc:
                X[c][1]()
            if hb_:
                Y[bq][2]()
                Y[bq][3]()
            if hc:
                X[c][2]()
                X[c][3]()
            if hb_:
                Y[bq][4]()
                Y[bq][5]()
            if hc:
                for f in X[c][4:]:
                    f()

    def pass_b(self, l, xin, B_xin, xout, B_xout):
        S, nc = self.S, self.nc
        WB = self.al("WB", [128, 8, 4096], BF16)
        self.W2 = self.al("W2", [128, 8, D], BF16)
        self.WO = self.al("WO", [128, 8, D], BF16)
        self.load_w(self.WO, self.w_out[l])
        wi = self.w_in[l]
        self.load_w(WB, wi[:, 3088:6160], 0)
        self.load_w(WB, wi[:, 7184:8208], 3072)
        self.load_w(self.W2, self.w_conv_out[l])
        cw = self.al("cw", [128, 8, 3], F32)
        for j in range(3):
            S.dma("sp", lambda j=j: nc.sync.dma_start(out=cw[:, :, j:j + 1], in_=self.conv_w[l, j:j + 1, :].rearrange("o (c p) -> p c o", p=128), allow_slow_non_contiguous=True), writes=[cw], part=True)
        U = self.al("U", [128, 8, 514], F32)
        S.op("dve", lambda: nc.vector.memset(U[:], 0.0), writes=[U])
        hT4 = self.al("hT4", [128, 4, D], BF16)
        ccs = self.al("ccs", [128, 512], F32)
        acc = self.al("acc", [128, 512], F32)
        PT = self.al("PT", [128, 8, 512], BF16)
        sgc = self.al("sgc", [128, D], BF16)
        yc = self.al("yc", [128, D], BF16)
        nst = self.NT // 4
        for s in range(nst):
            for t in range(4):
                i = s * 4 + t
                S.dma("sp", lambda t=t, i=i: nc.sync.dma_start(out=hT4[:, t, :], in_=self.hTd[i]), reads=[self.B_hT[i]], writes=[hT4], part=True)
            for cc in range(8):
                pcs = []
                for br in range(3):
                    p = self.ps()
                    c0 = br * 1024 + cc * 128

                    def f(p=p, c0=c0):
                        for k in range(8):
                            ins = nc.tensor.matmul(out=p[:].rearrange("p (t n) -> p t n", t=4), lhsT=WB[:, k, c0:c0 + 128],
                                                   rhs=hT4[:, :, k * 128:(k + 1) * 128], start=(k == 0), stop=(k == 7))
                        return ins
                    S.op("pe", f, reads=[WB, hT4], writes=[p])
                    pcs.append(p)
                pcb, pcc, pch = pcs
                S.op("act", lambda pcc=pcc: nc.scalar.copy(out=ccs[:], in_=pcc[:]), reads=[pcc], writes=[ccs])
                S.op("dve", lambda pch=pch, cc=cc: nc.vector.tensor_tensor(out=U[:, cc, 2:514], in0=pch[:], in1=ccs[:], op=ALU.mult), reads=[pch, ccs], writes=[U])
                S.op("dve", lambda cc=cc: nc.vector.tensor_scalar(out=acc[:], in0=U[:, cc, 2:514], scalar1=cw[:, cc, 2:3], scalar2=None, op0=ALU.mult), reads=[U, cw], writes=[acc])
                S.op("dve", lambda cc=cc: nc.vector.scalar_tensor_tensor(out=acc[:], in0=U[:, cc, 1:513], scalar=cw[:, cc, 1:2], in1=acc[:], op0=ALU.mult, op1=ALU.add), reads=[U, cw, acc], writes=[acc])
                S.op("dve", lambda cc=cc: nc.vector.scalar_tensor_tensor(out=acc[:], in0=U[:, cc, 0:512], scalar=cw[:, cc, 0:1], in1=acc[:], op0=ALU.mult, op1=ALU.add), reads=[U, cw, acc], writes=[acc])
                S.op("dve", lambda pcb=pcb, cc=cc: nc.vector.tensor_tensor(out=PT[:, cc, :], in0=pcb[:], in1=acc[:], op=ALU.mult), reads=[pcb, acc], writes=[PT])
                S.op("pool", lambda cc=cc: nc.gpsimd.tensor_copy(out=U[:, cc, 0:2], in_=U[:, cc, 512:514]), reads=[U], writes=[U])
            for t in range(4):
                i = s * 4 + t
                xt = self.xt[i % 2]
                S.dma("sp", lambda xt=xt, i=i: nc.sync.dma_start(out=xt[:], in_=xin[i * 128:(i + 1) * 128, :]), reads=[B_xin[i]], writes=[xt])
                for half in range(2):
                    p = self.ps()

                    def g(p=p, half=half, t=t):
                        for k in range(8):
                            ins = nc.tensor.matmul(out=p[:], lhsT=hT4[:, t, k * 128:(k + 1) * 128], rhs=WB[:, k, 3072 + half * 512:3072 + half * 512 + 512], start=(k == 0), stop=(k == 7))
                        return ins
                    S.op("pe", g, reads=[hT4, WB], writes=[p])
                    S.op("act", lambda p=p, half=half: nc.scalar.activation(out=sgc[:, half * 512:(half + 1) * 512], in_=p[:], func=AF.Sigmoid), reads=[p], writes=[sgc])
                for half in range(2):
                    p = self.ps()

                    def y(p=p, half=half, t=t):
                        for cc in range(8):
                            ins = nc.tensor.matmul(out=p[:], lhsT=PT[:, cc, t * 128:(t + 1) * 128], rhs=self.W2[:, cc, half * 512:half * 512 + 512], start=(cc == 0), stop=(cc == 7))
                        return ins
                    S.op("pe", y, reads=[PT, self.W2], writes=[p])
                    sl = slice(half * 512, half * 512 + 512)
                    S.op("dve", lambda p=p, sl=sl: nc.vector.tensor_tensor(out=yc[:, sl], in0=p[:], in1=sgc[:, sl], op=ALU.mult), reads=[p, sgc], writes=[yc])
                self.out_proj_residual(yc, xt, None, xout, B_xout, i)

    def moe(self, l, xin, B_xin, xout, B_xout, final):
        S, nc = self.S, self.nc
        NT = self.NT
        rw = self.al("rw", [128, 8, 72], F32)
        S.dma("sp", lambda: nc.sync.dma_start(out=rw[:, :, 0:8], in_=self.rg_w[l].rearrange("(k p) n -> p k n", p=128)), writes=[rw])
        S.dma("sp", lambda: nc.sync.dma_start(out=rw[:, :, 8:72], in_=self.re_w[l].rearrange("(k p) n -> p k n", p=128)), writes=[rw], part=True)
        rb = self.al("rb", [128, 72], F32)
        S.dma("sp", lambda: nc.sync.dma_start(out=rb[:, 0:8], in_=self.rg_b[l, :].partition_broadcast(128)), writes=[rb])
        S.dma("sp", lambda: nc.sync.dma_start(out=rb[:, 8:72], in_=self.re_b[l, :].partition_broadcast(128)), writes=[rb], part=True)
        CUM = self.al("CUM", [128, NTILE, 64], F32)
        OH1 = self.al("OH1", [128, NTILE, 64], F32)
        OH2 = self.al("OH2", [128, NTILE, 64], F32)
        W12 = self.al("W12", [128, NTILE, 2], F32)
        run = self.al("run", [128, 64], F32)
        S.op("dve", lambda: nc.vector.memset(run[:], 0.0), writes=[run])
        S.op("dve", lambda: nc.vector.memset(CUM[:], 0.0), writes=[CUM])
        S.op("dve", lambda: nc.vector.memset(OH1[:], 0.0), writes=[OH1])
        S.op("dve", lambda: nc.vector.memset(OH2[:], 0.0), writes=[OH2])
        gm = self.al("gm", [128, 8], F32)
        ohg = self.al("ohg", [128, 8], F32)
        pen = self.al("pen", [128, 8], F32)
        msk = self.al("msk", [128, 64], F32)
        top8 = self.al("top8", [128, 8], F32)
        m01 = self.al("m01", [128, 64], F32)
        gexp = self.al("gexp", [128, 8], F32)
        cnti = self.al("cnti", [128, 64], I32)
        padf = self.al("padf", [128, 64], F32)
        pend = self.al("pend", [128, 64], F32)
        adj = self.al("adj", [128, 64], F32)
        big = self.al("big", [128, NTILE, 64], F32)
        Df = self.al("Df", [128, NTILE, 2], F32)
        Di = self.al("Di", [128, NTILE * 2], I32)
        thr = self.al("thr", [128, NBLK], F32)
        thri = self.al("thri", [128, NBLK], I32)
        bef = self.al("bef", [128, NBLK], F32)
        WI = [self.al("WI%d" % j, [128, NBLK], I32) for j in range(2)]
        vld = self.al("vld", [128, NBLK], F32)
        mark_h2 = self.aoff
        H2 = self.al("H2", [128, NTILE, D], BF16)
        h2s = [self.al("h2_%d" % j, [128, D], F32) for j in range(2)]
        h2Ts = [self.al("h2T%d" % j, [128, D], F32) for j in range(2)]
        LgA = self.al("LgA", [128, NTILE, 72], F32)
        S.op("dve", lambda: nc.vector.memset(LgA[:], 0.0), writes=[LgA])
        gmx = self.al("gmx", [128, NTILE], F32)
        gsm = self.al("gsm", [128, NTILE], F32)
        ohgA = self.al("ohgA", [128, NTILE, 8], F32)
        gshA = self.al("gshA", [128, NTILE, 8], F32)
        mskA = self.al("mskA", [128, NTILE, 64], F32)
        top8A = self.al("top8A", [128, NTILE, 8], F32)
        PRE = self.al("PRE", [128, NTILE, 64], F32)

        def rxa(i):
            xt = self.xt[i % 2]
            h2 = h2s[i % 2]
            S.dma("sp", lambda xt=xt, i=i: nc.sync.dma_start(out=xt[:], in_=xin[i * 128:(i + 1) * 128, :]), reads=[B_xin[i]], writes=[xt])
            self.norm_mod(xt, h2, "m")
            S.op("act", lambda i=i: nc.scalar.copy(out=H2[:, i, :], in_=h2[:]), reads=[h2], writes=[H2])

        def rxb(i):
            h2, h2T = h2s[i % 2], h2Ts[i % 2]
            for half in range(2):
                p = self.ps()

                def tr(p=p, half=half):
                    for k in range(4):
                        kk = half * 4 + k
                        ins = nc.tensor.transpose(out=p[:, k * 128:(k + 1) * 128], in_=h2[:, kk * 128:(kk + 1) * 128], identity=self.identf[:])
                    return ins
                S.op("pe", tr, reads=[h2, self.identf], writes=[p])
                S.op("act", lambda p=p, half=half: nc.scalar.copy(out=h2T[:, half * 512:(half + 1) * 512], in_=p[:]), reads=[p], writes=[h2T])
            pl = self.ps()

            def rmm(pl=pl):
                for k in range(8):
                    ins = nc.tensor.matmul(out=pl[:, 0:72], lhsT=h2T[:, k * 128:(k + 1) * 128], rhs=rw[:, k, :], start=(k == 0), stop=(k == 7))
                return ins
            S.op("pe", rmm, reads=[h2T, rw], writes=[pl])
            S.op("dve", lambda pl=pl, i=i: nc.vector.tensor_tensor(out=LgA[:, i, :], in0=pl[:, 0:72], in1=rb[:], op=ALU.add), reads=[pl, rb], writes=[LgA])

        rxa(0)
        for i in range(NT):
            if i + 1 < NT:
                rxa(i + 1)
            rxb(i)
        T_ = NTILE
        LgG = LgA[:, :, 0:8]
        S.op("dve", lambda: nc.vector.reduce_max(out=gmx[:], in_=LgG, axis=AX.X), reads=[LgA], writes=[gmx])
        S.op("dve", lambda: nc.vector.tensor_tensor(out=ohgA[:], in0=LgG, in1=gmx[:].unsqueeze(2).to_broadcast([128, T_, 8]), op=ALU.is_equal), reads=[LgA, gmx], writes=[ohgA])
        S.op("dve", lambda: nc.vector.tensor_tensor(out=gshA[:], in0=LgG, in1=gmx[:].unsqueeze(2).to_broadcast([128, T_, 8]), op=ALU.subtract), reads=[LgA, gmx], writes=[gshA])
        S.op("act", lambda: nc.scalar.activation(out=gshA[:], in_=gshA[:], func=AF.Exp), reads=[gshA], writes=[gshA])
        S.op("dve", lambda: nc.vector.reduce_sum(out=gsm[:], in_=gshA[:], axis=AX.X), reads=[gshA], writes=[gsm])
        S.op("dve", lambda: nc.vector.reciprocal(out=gsm[:], in_=gsm[:]), reads=[gsm], writes=[gsm])
        S.op("dve", lambda: nc.vector.tensor_scalar(out=ohgA[:], in0=ohgA[:], scalar1=-1.0, scalar2=1e30, op0=ALU.add, op1=ALU.mult), reads=[ohgA], writes=[ohgA])
        S.op("dve", lambda: nc.vector.tensor_tensor(out=mskA[:].rearrange("p t (g j) -> p t g j", g=8), in0=LgA[:, :, 8:72].rearrange("p t (g j) -> p t g j", g=8),
                                                     in1=ohgA[:].unsqueeze(3).to_broadcast([128, T_, 8, 8]), op=ALU.add), reads=[LgA, ohgA], writes=[mskA])
        for i in range(NT):
            S.op("dve", lambda i=i: nc.vector.max(out=top8A[:, i, :], in_=mskA[:, i, :]), reads=[mskA], writes=[top8A])
        S.op("dve", lambda: nc.vector.tensor_tensor(out=OH1[:], in0=mskA[:], in1=top8A[:, :, 0:1].to_broadcast([128, T_, 64]), op=ALU.is_equal), reads=[mskA, top8A], writes=[OH1])
        S.op("dve", lambda: nc.vector.tensor_tensor(out=OH2[:], in0=mskA[:], in1=top8A[:, :, 1:2].to_broadcast([128, T_, 64]), op=ALU.is_equal), reads=[mskA, top8A], writes=[OH2])
        S.op("dve", lambda: nc.vector.tensor_tensor(out=gmx[:], in0=top8A[:, :, 0], in1=top8A[:, :, 1], op=ALU.subtract), reads=[top8A], writes=[gmx])
        S.op("act", lambda: nc.scalar.activation(out=gmx[:], in_=gmx[:], func=AF.Sigmoid), reads=[gmx], writes=[gmx])
        S.op("dve", lambda: nc.vector.tensor_tensor(out=W12[:, :, 0], in0=gmx[:], in1=gsm[:], op=ALU.mult), reads=[gmx, gsm], writes=[W12])
        S.op("dve", lambda: nc.vector.tensor_tensor(out=W12[:, :, 1], in0=gsm[:], in1=W12[:, :, 0], op=ALU.subtract), reads=[gsm, W12], writes=[W12])
        S.op("dve", lambda: nc.vector.tensor_tensor(out=big[:], in0=OH1[:], in1=OH2[:], op=ALU.add), reads=[OH1, OH2], writes=[big])
        S.op("dve", lambda: nc.vector.memset(PRE[:, 0, :], 0.0), writes=[PRE])
        for i in range(1, NT):
            S.op("dve", lambda i=i: nc.vector.tensor_tensor(out=PRE[:, i, :], in0=PRE[:, i - 1, :], in1=big[:, i - 1, :], op=ALU.add), reads=[PRE, big], writes=[PRE])
        S.op("dve", lambda: nc.vector.tensor_tensor(out=run[:], in0=PRE[:, NT - 1, :], in1=big[:, NT - 1, :], op=ALU.add), reads=[PRE, big], writes=[run])
        for i in range(NT):
            pc = self.ps()

            def cmm(pc=pc, i=i):
                nc.tensor.matmul(out=pc[:, 0:64], lhsT=self.Ltri[:], rhs=big[:, i, :], start=True, stop=False)
                return nc.tensor.matmul(out=pc[:, 0:64], lhsT=self.ones[:], rhs=PRE[:, i, :], start=False, stop=True)
            S.op("pe", cmm, reads=[self.Ltri, self.ones, big, PRE], writes=[pc])
            S.op("act", lambda pc=pc, i=i: nc.scalar.copy(out=CUM[:, i, :], in_=pc[:, 0:64]), reads=[pc], writes=[CUM])
        pct = self.ps()
        S.op("pe", lambda: nc.tensor.matmul(out=pct[:, 0:64], lhsT=self.ones[:], rhs=run[:], start=True, stop=True), reads=[self.ones, run], writes=[pct])
        S.op("dve", lambda: nc.vector.tensor_scalar(out=cnti[:], in0=pct[:, 0:64], scalar1=float(SROWS - 1), scalar2=None, op0=ALU.add), reads=[pct], writes=[cnti])
        S.op("dve", lambda: nc.vector.tensor_scalar(out=cnti[:], in0=cnti[:], scalar1=8, scalar2=8, op0=ALU.arith_shift_right, op1=ALU.logical_shift_left), reads=[cnti], writes=[cnti])
        S.op("dve", lambda: nc.vector.tensor_copy(out=padf[:], in_=cnti[:]), reads=[cnti], writes=[padf])
        S.op("dve", lambda: nc.vector.tensor_tensor_scan(out=pend[:], data0=self.ones[:, 0:64], data1=padf[:], initial=0.0, op0=ALU.mult, op1=ALU.add), reads=[self.ones, padf], writes=[pend])
        S.op("dve", lambda: nc.vector.scalar_tensor_tensor(out=adj[:], in0=pend[:], scalar=-1.0, in1=padf[:], op0=ALU.add, op1=ALU.subtract), reads=[pend, padf], writes=[adj])
        S.op("dve", lambda: nc.vector.tensor_tensor(out=CUM[:], in0=CUM[:], in1=adj[:].unsqueeze(1).to_broadcast([128, NTILE, 64]), op=ALU.add), reads=[CUM, adj], writes=[CUM])
        for k, OH in enumerate((OH1, OH2)):
            S.op("dve", lambda OH=OH: nc.vector.tensor_tensor(out=big[:], in0=CUM[:], in1=OH[:], op=ALU.mult), reads=[CUM, OH], writes=[big])
            S.op("dve", lambda k=k: nc.vector.reduce_sum(out=Df[:, :, k], in_=big[:], axis=AX.X), reads=[big], writes=[Df])
        S.op("dve", lambda: nc.vector.tensor_copy(out=Di[:], in_=Df[:].rearrange("p t k -> p (t k)")), reads=[Df], writes=[Di])
        self.tap("Di", Di)
        self.tap("W12", W12, W12[:].rearrange("p t k -> p (t k)"))
        S.op("pool", lambda: nc.gpsimd.iota(thri[:], pattern=[[SROWS, NBLK]], base=0, channel_multiplier=0), writes=[thri])
        S.op("dve", lambda: nc.vector.tensor_copy(out=thr[:], in_=thri[:]), reads=[thri], writes=[thr])
        for q4 in range(NBLK // NTILE):
            bsl = slice(q4 * NTILE, (q4 + 1) * NTILE)
            S.op("dve", lambda bsl=bsl: nc.vector.tensor_tensor(out=big[:], in0=pend[:].unsqueeze(1).to_broadcast([128, NTILE, 64]),
                                                            in1=thr[:, bsl].unsqueeze(2).to_broadcast([128, NTILE, 64]), op=ALU.is_le), reads=[pend, thr], writes=[big])
            S.op("dve", lambda bsl=bsl: nc.vector.reduce_sum(out=bef[:, bsl], in_=big[:], axis=AX.X), reads=[big], writes=[bef])
        S.op("dve", lambda: nc.vector.tensor_scalar(out=vld[:], in0=thr[:], scalar1=pend[:, 63:64], scalar2=None, op0=ALU.is_lt), reads=[thr, pend], writes=[vld])
        S.op("pool", lambda: nc.gpsimd.iota(thri[:], pattern=[[0, NBLK]], base=0, channel_multiplier=2), reads=[thr], writes=[thri])
        S.op("dve", lambda: nc.vector.tensor_copy(out=thr[:], in_=thri[:]), reads=[thri, bef], writes=[thr])
        S.op("dve", lambda: nc.vector.tensor_scalar(out=bef[:], in0=bef[:], scalar1=63.0, scalar2=256.0, op0=ALU.min, op1=ALU.mult), reads=[bef], writes=[bef])
        S.op("dve", lambda: nc.vector.tensor_tensor(out=bef[:], in0=bef[:], in1=thr[:], op=ALU.add), reads=[bef, thr], writes=[bef])
        for kh in range(2):
            S.op("dve", lambda kh=kh: nc.vector.scalar_tensor_tensor(out=thr[:], in0=bef[:], scalar=float(l * NE * 256 + kh) - OOB_BIG, in1=vld[:], op0=ALU.add, op1=ALU.mult), reads=[bef, vld], writes=[thr])
            S.op("dve", lambda kh=kh: nc.vector.tensor_scalar(out=WI[kh][:], in0=thr[:], scalar1=OOB_BIG, scalar2=None, op0=ALU.add), reads=[thr], writes=[WI[kh]])
        self.tap("WI0", WI[0])
        for i in range(NT):
            for k in range(2):
                S.dma("pool", lambda i=i, k=k: nc.gpsimd.indirect_dma_start(out=self.xbuf[:, :], out_offset=bass.IndirectOffsetOnAxis(ap=Di[:, 2 * i + k:2 * i + k + 1], axis=0),
                                                                            in_=H2[:, i, :], in_offset=None), reads=[H2, Di], writes=[self.B_xbuf], part=True)
        self.arelease(mark_h2)
        nslot = NBLK if NT == NTILE else min(NBLK, (2 * NT * 128) // SROWS + 64)
        nblk = 2 * nslot
        order = []
        lo, hi = 0, nslot - 1
        while lo <= hi:
            order.append(lo)
            if hi != lo:
                order.append(hi)
            lo += 1
            hi -= 1

        def rows(b):
            return (2 * order[b // 2] + (b % 2)) * 128
        NW13, NW2 = 3, 3
        w1b = [self.al("w1b%d" % j, [128, 8 * DEXP], BF16) for j in range(NW13)]
        w3b = [self.al("w3b%d" % j, [128, 8 * DEXP], BF16) for j in range(NW13)]
        w2b = [self.al("w2b%d" % j, [128, 4 * D], BF16) for j in range(NW2)]
        xbk = [self.al("xbk%d" % j, [128, D], BF16) for j in range(3)]
        xbT = [self.al("xbT%d" % j, [128, D], BF16) for j in range(2)]
        s1 = [self.al("s1_%d" % j, [128, DEXP], BF16) for j in range(2)]
        gb = [self.al("gb%d" % j, [128, DEXP], BF16) for j in range(2)]
        gT = [self.al("gT%d" % j, [128, DEXP], BF16) for j in range(2)]
        yo = [self.al("yo%d" % j, [128, D], F32) for j in range(2)]
        v1 = self.ew1.rearrange("l e (p kh k4) n -> (l e p kh) (k4 n)", kh=2, k4=4)
        v3 = self.ew3.rearrange("l e (p kh k4) n -> (l e p kh) (k4 n)", kh=2, k4=4)
        v2 = self.ew2.rearrange("l e (p kh k2) n -> (l e p kh) (k2 n)", kh=2, k2=2)

        def gather_w(dst, view, b):
            for kh in range(2):
                def gth(dst=dst, view=view, kh=kh, b=b):
                    if getattr(self, "_bcreg", None) is None:
                        self._bcreg = nc.gpsimd.to_reg(DEPTH * NE * 256 - 1)
                    return nc.gpsimd.indirect_dma_start(
                        out=dst[:, kh * 2048:(kh + 1) * 2048], out_offset=None, in_=view,
                        in_offset=bass.IndirectOffsetOnAxis(ap=WI[kh][:, order[b]:order[b] + 1], axis=0),
                        bounds_check=self._bcreg, oob_is_err=False)
                S.dma("pool", gth, reads=[WI[kh]], writes=[dst], part=(kh == 1))

        pst = {}

        def ldx(b):
            xk = xbk[b % 3]
            S.dma("sp", lambda xk=xk, b=b: nc.sync.dma_start(out=xk[:], in_=self.xbuf[rows(b):rows(b) + 128, :]), reads=[self.B_xbuf], writes=[xk])

        def st1(b):
            self.transpose_strided(xbk[b % 3], xbT[b % 2], 8)

        def st2(b):
            xT = xbT[b % 2]
            p1, p3 = self.ps(), self.ps()
            self.mm_flat(xT, w1b[(b // 2) % NW13], 512, 0, p1, 8)
            self.mm_flat(xT, w3b[(b // 2) % NW13], 512, 0, p3, 8)
            s, g = s1[b % 2], gb[b % 2]
            S.op("act", lambda p1=p1, s=s: nc.scalar.activation(out=s[:], in_=p1[:], func=AF.Silu), reads=[p1], writes=[s])
            S.op("dve", lambda p3=p3, s=s, g=g: nc.vector.tensor_tensor(out=g[:], in0=p3[:], in1=s[:], op=ALU.mult), reads=[p3, s], writes=[g])

        def st3(b):
            self.transpose_strided(gb[b % 2], gT[b % 2], 4)

        def st4(b):
            y = yo[b % 2]
            for half in range(2):
                p = self.ps()
                self.mm_flat(gT[b % 2], w2b[(b // 2) % NW2], 1024, half * 512, p, 4)
                S.op("act", lambda p=p, half=half, y=y: nc.scalar.copy(out=y[:, half * 512:(half + 1) * 512], in_=p[:]), reads=[p], writes=[y])
            S.dma("sp", lambda y=y, b=b: nc.sync.dma_start(out=self.ybuf[rows(b):rows(b) + 128, :], in_=y[:]), reads=[y], writes=[self.B_ybuf], part=True)

        gather_w(w1b[0], v1, 0)
        gather_w(w3b[0], v3, 0)
        ldx(0)
        ldx(1)
        for t in range(nblk + 3):
            if t + 2 < nblk:
                ldx(t + 2)
            if t % 2 == 0:
                w = t // 2
                if w + 1 < nslot:
                    gather_w(w1b[(w + 1) % NW13], v1, w + 1)
                    gather_w(w3b[(w + 1) % NW13], v3, w + 1)
                if w < nslot:
                    gather_w(w2b[w % NW2], v2, w)
            if t < nblk:
                st1(t)
            if 0 <= t - 1 < nblk:
                st2(t - 1)
            if 0 <= t - 2 < nblk:
                st3(t - 2)
            if 0 <= t - 3 < nblk:
                st4(t - 3)
        self.arelease(mark_h2)
        RING = 4
        ya = [self.al("ya%d" % j, [128, D], F32) for j in range(RING)]
        yb_ = [self.al("yb%d" % j, [128, D], F32) for j in range(RING)]
        xc = [self.al("xc%d" % j, [128, D], F32) for j in range(3)]
        mo = [self.al("mo%d" % j, [128, D], F32) for j in range(2)]
        if final:
            S.dma("sp", lambda: nc.sync.dma_start(out=self.rowrep[:], in_=self.fng[0, :].partition_broadcast(128)), writes=[self.rowrep])

        def gat(i):
            for k, yy in enumerate((ya[i % RING], yb_[i % RING])):
                S.dma("pool", lambda yy=yy, i=i, k=k: nc.gpsimd.indirect_dma_start(out=yy[:], out_offset=None, in_=self.ybuf[:, :],
                                                                                  in_offset=bass.IndirectOffsetOnAxis(ap=Di[:, 2 * i + k:2 * i + k + 1], axis=0)),
                      reads=[self.B_ybuf, Di], writes=[yy])

        def ldc(i):
            x_ = xc[i % 3]
            S.dma("sp", lambda x_=x_, i=i: nc.sync.dma_start(out=x_[:], in_=xin[i * 128:(i + 1) * 128, :]), reads=[B_xin[i]], writes=[x_])
        for i in range(min(3, NT)):
            gat(i)
        for i in range(min(2, NT)):
            ldc(i)
        for i in range(NT):
            if i + 3 < NT:
                gat(i + 3)
            if i + 2 < NT:
                ldc(i + 2)
            m, x_, A_, B_ = mo[i % 2], xc[i % 3], ya[i % RING], yb_[i % RING]
            S.op("dve", lambda i=i, m=m, A_=A_: nc.vector.tensor_scalar(out=m[:], in0=A_[:], scalar1=W12[:, i, 0:1], scalar2=None, op0=ALU.mult), reads=[A_, W12], writes=[m])
            S.op("dve", lambda i=i, m=m, B_=B_: nc.vector.scalar_tensor_tensor(out=m[:], in0=B_[:], scalar=W12[:, i, 1:2], in1=m[:], op0=ALU.mult, op1=ALU.add), reads=[B_, W12, m], writes=[m])
            S.op("dve", lambda m=m: nc.vector.tensor_tensor(out=m[:], in0=m[:], in1=self.GATE[:], op=ALU.mult), reads=[m, self.GATE], writes=[m])
            S.op("dve", lambda m=m, x_=x_: nc.vector.tensor_tensor(out=m[:], in0=m[:], in1=x_[:], op=ALU.add), reads=[m, x_], writes=[m])
            if final:
                ss = self.sm("ss_f", [128, 1])
                rstd = self.sm("rstd_f", [128, 1])
                S.op("act", lambda m=m: nc.scalar.activation(out=self.junk[:], in_=m[:], func=AF.Square), reads=[m], writes=[self.junk])
                S.op("dve", lambda: nc.vector.reduce_sum(out=ss[:], in_=self.junk[:], axis=AX.X), reads=[self.junk], writes=[ss])
                self.rstd_from_ss(ss, D, rstd)
                S.op("dve", lambda m=m: nc.vector.scalar_tensor_tensor(out=m[:], in0=m[:], scalar=rstd[:, 0:1], in1=self.rowrep[:], op0=ALU.mult, op1=ALU.mult), reads=[m, rstd, self.rowrep], writes=[m])
            S.dma("sp", lambda i=i, m=m: nc.sync.dma_start(out=xout[i * 128:(i + 1) * 128, :], in_=m[:]), reads=[m], writes=[B_xout[i]])

    def build(self):
        cur, Bcur = self.x, self.B_x
        chain = [(self.xa, self.B_xa), (self.xb, self.B_xb), (self.xm, self.B_xm)]
        last_out = None
        for l in range(self.nlayers):
            if "A" in self.stages or "B" in self.stages:
                self.mod_half(l, 0, self.norm1_g)
            if "A" in self.stages:
                self.pass_a(l, cur, Bcur, self.xa, self.B_xa)
                self.arelease(0)
                last_out = (self.xa, self.B_xa)
            if "B" in self.stages:
                self.pass_b(l, self.xa, self.B_xa, self.xb, self.B_xb)
                self.arelease(0)
                last_out = (self.xb, self.B_xb)
            if "M" in self.stages:
                src = last_out if last_out is not None else (cur, Bcur)
                self.mod_half(l, 1, self.norm2_g)
                final = (l == self.nlayers - 1)
                dst = (self.out, self.B_out) if final else (self.xm, self.B_xm)
                self.moe(l, src[0], src[1], dst[0], dst[1], final)
                self.arelease(0)
                last_out = dst
            cur, Bcur = last_out
        self.last = last_out
        return self

    def finish(self, copy_last_to_out=False):
        S, nc = self.S, self.nc
        if copy_last_to_out and self.last[0] is not self.out:
            for i in range(self.NT):
                xt = self.xt[i % 2]
                S.dma("sp", lambda xt=xt, i=i: nc.sync.dma_start(out=xt[:], in_=self.last[0][i * 128:(i + 1) * 128, :]), reads=[self.last[1][i]], writes=[xt])
                S.dma("sp", lambda xt=xt, i=i: nc.sync.dma_start(out=self.out[i * 128:(i + 1) * 128, :], in_=xt[:]), reads=[xt], writes=[self.B_out[i]])
        S.wait_for("sp", self.B_out[:self.NT] + [self.B_dbg])
        S.emit()
        S.close()
        return nc


WEIGHT_NAMES = ["mod_w", "mod_b", "norm1_g", "w_in", "gate_w2", "gate_b", "gla_norm_g", "conv_w", "w_gla_out",
                "w_conv_out", "w_out", "norm2_g", "router_group_w", "router_group_b", "router_expert_w",
                "router_expert_b", "expert_w1", "expert_w3", "expert_w2"]


def make_in_maps(inputs, ncores=8):
    shared = {n: np.ascontiguousarray(np.asarray(inputs[n], dtype=np.float32)) for n in WEIGHT_NAMES}
    shared["final_norm_g"] = np.ascontiguousarray(np.asarray(inputs["final_norm_g"], dtype=np.float32).reshape(1, D))
    x = np.asarray(inputs["x"], dtype=np.float32)
    c = np.asarray(inputs["c"], dtype=np.float32)
    maps = []
    for b in range(ncores):
        m = dict(shared)
        m["x"] = np.ascontiguousarray(x[b])
        m["c"] = np.ascontiguousarray(c[b:b + 1])
        maps.append(m)
    return maps


def kernel(**inputs):
    nc = K().build().finish()
    res = run_bass_kernel_spmd(nc, make_in_maps(inputs), core_ids=list(range(8)))
    return np.stack([np.asarray(r["out"], dtype=np.float32) for r in res.results], axis=0)
```

```python
import numpy as np
import concourse.bass as bass
import concourse.mybir as mybir
from concourse.bass_utils import run_bass_kernel_spmd

F32 = mybir.dt.float32
BF16 = mybir.dt.bfloat16
I32 = mybir.dt.int32
ALU = mybir.AluOpType
AF = mybir.ActivationFunctionType
AX = mybir.AxisListType

D = 1024
SEQ = 4096
NTILE = SEQ // 128
DEPTH = 2
NE = 64
DEXP = 512
SROWS = 256
NBLK = 96
NSLOT = NBLK * SROWS
EPS = 1e-6
OOB_BIG = 16777216.0
OFF = dict(q=0, k=512, v=1024, gdn=2048, r=2064, cb=3088, cc=4112, ch=5136, ga=6160, gc=7184)
COMPUTE = ("pe", "act", "dve", "pool")


class Buf:
    __slots__ = ("name", "writer", "readers", "wsem", "wcnt", "rsem", "rcnt", "t")

    def __init__(self, name, t=None):
        self.name = name
        self.t = t
        self.writer = None
        self.readers = {}
        self.wsem = None
        self.wcnt = 0
        self.rsem = None
        self.rcnt = 0

    def __getitem__(self, idx):
        return self.t[idx]


class Sched:
    def __init__(self, nc, strict=True):
        self.nc = nc
        self.strict = strict
        self.eng = {"pe": nc.tensor, "act": nc.scalar, "dve": nc.vector,
                    "pool": nc.gpsimd, "sp": nc.sync}
        self.prog = {k: [] for k in self.eng}
        self.cnt = {k: 0 for k in COMPUTE}
        self.sem = {}
        self.seen = {k: {} for k in self.eng}
        self.stack = []
        self.serial = False
        self.dsem = {}
        for k in COMPUTE:
            self.sem[k] = self.new_sem("c_" + k)
        self.nps = 0
        self.npb = 0

    def new_sem(self, name):
        cm = self.nc.semaphore(name)
        s = cm.__enter__()
        self.stack.append(cm)
        return s

    def sbuf(self, name, shape, dt):
        cm = self.nc.sbuf_tensor(name, list(shape), dt)
        t = cm.__enter__()
        self.stack.append(cm)
        return Buf(name, t)

    def psum(self, name, shape, dt):
        cm = self.nc.psum_tensor(name, list(shape), dt)
        t = cm.__enter__()
        self.stack.append(cm)
        return Buf(name, t)

    def _wait(self, q, semkey, sem, val):
        seen = self.seen[q]
        if seen.get(semkey, 0) >= val:
            return
        seen[semkey] = val
        e = self.eng[q]
        self.prog[q].append(lambda e=e, sem=sem, val=val: (e.wait_ge(sem, val), None)[1])

    def _dep(self, q, d):
        if d is None:
            return
        if d[0] == "eng":
            _, k, c = d
            if k == q and (k == "pe" or not self.strict):
                return
            self._wait(q, k, self.sem[k], c)
        else:
            sem, n = self.dsem[d[1]]
            self._wait(q, d[1], sem, 16 * n)

    @staticmethod
    def _addreader(b, tag):
        b.readers[tag[1]] = tag

    def _serial(self, q):
        for k in COMPUTE:
            if self.cnt[k] > 0 and k != q:
                self._wait(q, k, self.sem[k], self.cnt[k])
        for key, (sem, n) in self.dsem.items():
            self._wait(q, key, sem, 16 * n)

    def op(self, q, fn, reads=(), writes=()):
        if self.serial:
            self._serial(q)
        for b in reads:
            self._dep(q, b.writer)
        for b in writes:
            self._dep(q, b.writer)
            for r in b.readers.values():
                self._dep(q, r)
        self.cnt[q] += 1
        c = self.cnt[q]
        sem = self.sem[q]
        self.prog[q].append(lambda fn=fn, sem=sem: fn().then_inc(sem, 1))
        tag = ("eng", q, c)
        for b in reads:
            self._addreader(b, tag)
        for b in writes:
            b.writer = tag
            b.readers = {}

    def dma(self, q, fn, reads=(), writes=(), part=False):
        if self.serial:
            self._serial(q)
        for b in reads:
            self._dep(q, b.writer)
        for b in writes:
            if not (part and b.writer is not None and b.writer[0] == "dma"):
                self._dep(q, b.writer)
            for r in b.readers.values():
                self._dep(q, r)
        if writes and writes[0].t is not None:
            o = writes[0]
            key = "w_" + o.name
        else:
            o = [b for b in reads if b.t is not None][0]
            key = "r_" + o.name
        if key not in self.dsem:
            self.dsem[key] = [self.new_sem(key), 0]
        ent = self.dsem[key]
        ent[1] += 1
        sem = ent[0]
        self.prog[q].append(lambda fn=fn, sem=sem: fn().then_inc(sem, 16))
        tag = ("dma", key)
        for b in reads:
            self._addreader(b, tag)
        for b in writes:
            b.writer = tag
            b.readers = {}
        return tag

    def barrier(self):
        for q in self.eng:
            for k in COMPUTE:
                if self.cnt[k] > 0 and (k != q or (self.strict and k != "pe")):
                    self._wait(q, k, self.sem[k], self.cnt[k])
            for key, (sem, n) in self.dsem.items():
                self._wait(q, key, sem, 16 * n)

    def raw(self, q, fn):
        self.prog[q].append(lambda fn=fn: (fn(), None)[1])

    def wait_for(self, q, bufs):
        for b in bufs:
            self._dep(q, b.writer)
            for r in b.readers.values():
                self._dep(q, r)

    def emit(self):
        with self.nc.Block() as block:
            for k, attr in (("sp", "sync"), ("act", "scalar"), ("dve", "vector"),
                            ("pool", "gpsimd"), ("pe", "tensor")):
                lst = self.prog[k]

                def body(_e, lst=lst):
                    for f in lst:
                        f()
                getattr(block, attr)(body)

    def close(self):
        while self.stack:
            self.stack.pop().__exit__(None, None, None)


class K:
    def __init__(self, nlayers=DEPTH, ntile=NTILE, stages=("A", "B", "M"), dbg=None, strict=True, serial=False):
        self.nlayers = nlayers
        self.NT = ntile
        self.stages = stages
        self.dbg = dbg or {}
        nc = self.nc = bass.Bass("TRN2", target_bir_lowering=False)
        S = self.S = Sched(nc, strict=strict)
        S.serial = serial
        L = DEPTH

        def inp(name, shape, dt=F32):
            return nc.dram_tensor(name, list(shape), dt, kind="ExternalInput").ap()
        self.x = inp("x", [SEQ, D])
        self.c = inp("c", [1, D])
        self.mod_w = inp("mod_w", [L, D, 6 * D])
        self.mod_b = inp("mod_b", [L, 6 * D])
        self.norm1_g = inp("norm1_g", [L, D])
        self.w_in = inp("w_in", [L, D, 8208])
        self.gate_w2 = inp("gate_w2", [L, 16, 512])
        self.gate_b = inp("gate_b", [L, 512])
        self.gla_norm_g = inp("gla_norm_g", [L, D])
        self.conv_w = inp("conv_w", [L, 3, D])
        self.w_gla_out = inp("w_gla_out", [L, D, D])
        self.w_conv_out = inp("w_conv_out", [L, D, D])
        self.w_out = inp("w_out", [L, D, D])
        self.norm2_g = inp("norm2_g", [L, D])
        self.rg_w = inp("router_group_w", [L, D, 8])
        self.rg_b = inp("router_group_b", [L, 8])
        self.re_w = inp("router_expert_w", [L, D, 64])
        self.re_b = inp("router_expert_b", [L, 64])
        self.ew1 = inp("expert_w1", [L, NE, D, DEXP])
        self.ew3 = inp("expert_w3", [L, NE, D, DEXP])
        self.ew2 = inp("expert_w2", [L, NE, DEXP, D])
        self.fng = inp("final_norm_g", [1, D])
        self.out = nc.dram_tensor("out", [SEQ, D], F32, kind="ExternalOutput").ap()

        def scr(name, shape, dt=F32):
            return nc.dram_tensor(name, list(shape), dt, kind="Internal").ap()
        self.xa = scr("xa", [SEQ, D])
        self.xb = scr("xb", [SEQ, D])
        self.xm = scr("xm", [SEQ, D])
        self.hTd = scr("hTd", [NTILE, 128, D], BF16)
        self.xbuf = scr("xbuf", [NSLOT, D], BF16)
        self.ybuf = scr("ybuf", [NSLOT, D], F32)
        self.B_x = [Buf("x%d" % i) for i in range(NTILE)]
        self.B_xa = [Buf("xa%d" % i) for i in range(NTILE)]
        self.B_xb = [Buf("xb%d" % i) for i in range(NTILE)]
        self.B_xm = [Buf("xm%d" % i) for i in range(NTILE)]
        self.B_hT = [Buf("hT%d" % i) for i in range(NTILE)]
        self.B_out = [Buf("out%d" % i) for i in range(NTILE)]
        self.B_xbuf = Buf("xbuf")
        self.B_ybuf = Buf("ybuf")
        self.B_dbg = Buf("dbg")
        self.dbg_out = {}
        for name, (shape, dt) in self.dbg.items():
            self.dbg_out[name] = nc.dram_tensor("dbg_" + name, list(shape), dt, kind="ExternalOutput").ap()

        self.PS = [S.psum("ps%d" % i, [128, 512], F32) for i in range(6)]
        self.PB = [S.psum("pb%d" % i, [128, 1024], BF16) for i in range(2)]
        self._ips = 0
        self._ipb = 0
        self.ARENA = 164 * 1024
        self.arena = S.sbuf("arena", [128, self.ARENA // 2], BF16)
        self.aoff = 0
        self.consts()
        self.common = {}

    def ps(self):
        b = self.PS[self._ips % len(self.PS)]
        self._ips += 1
        return b

    def pb(self):
        b = self.PB[self._ipb % len(self.PB)]
        self._ipb += 1
        return b

    def tap(self, name, buf, ap=None):
        if name not in self.dbg_out:
            return
        S, nc = self.S, self.nc
        dst = self.dbg_out[name]
        src = buf.t[:] if ap is None else ap
        S.dma("sp", lambda: nc.sync.dma_start(out=dst, in_=src), reads=[buf], writes=[self.B_dbg], part=True)

    def consts(self):
        S, nc = self.S, self.nc
        io = S.sbuf("io", [128, 128], I32)
        iof = S.sbuf("iof", [128, 128], F32)
        self.ident = S.sbuf("ident", [128, 128], BF16)
        self.identf = S.sbuf("identf", [128, 128], F32)
        self.Uinc = S.sbuf("Uinc", [128, 128], F32)
        self.Urev = S.sbuf("Urev", [128, 128], F32)
        self.Ltri = S.sbuf("Ltri", [128, 128], F32)
        self.ones = S.sbuf("ones", [128, 128], F32)
        self.mask4 = S.sbuf("mask4", [128, 512], F32)
        S.op("pool", lambda: nc.gpsimd.iota(io[:], pattern=[[1, 128]], base=0, channel_multiplier=-1), writes=[io])
        S.op("dve", lambda: nc.vector.tensor_copy(out=iof[:], in_=io[:]), reads=[io], writes=[iof])
        S.op("dve", lambda: nc.vector.tensor_single_scalar(out=self.ident[:], in_=iof[:], scalar=0.0, op=ALU.is_equal), reads=[iof], writes=[self.ident])
        S.op("dve", lambda: nc.vector.tensor_single_scalar(out=self.identf[:], in_=iof[:], scalar=0.0, op=ALU.is_equal), reads=[iof], writes=[self.identf])
        S.op("dve", lambda: nc.vector.tensor_single_scalar(out=self.Ltri[:], in_=iof[:], scalar=0.0, op=ALU.is_ge), reads=[iof], writes=[self.Ltri])
        S.op("dve", lambda: nc.vector.tensor_scalar(out=self.Uinc[:], in0=iof[:], scalar1=0.0, scalar2=-1.0 / 16, op0=ALU.is_ge, op1=ALU.mult), reads=[iof], writes=[self.Uinc])
        S.op("dve", lambda: nc.vector.tensor_scalar(out=self.Urev[:], in0=iof[:], scalar1=0.0, scalar2=-1.0 / 16, op0=ALU.is_lt, op1=ALU.mult), reads=[iof], writes=[self.Urev])
        S.op("dve", lambda: nc.vector.memset(self.ones[:], 1.0), writes=[self.ones])
        for h in range(4):
            S.op("dve", lambda h=h: nc.vector.tensor_copy(out=self.mask4[:, h * 128:(h + 1) * 128], in_=self.Ltri[:]), reads=[self.Ltri], writes=[self.mask4])
        self.io_ = io
        cT = S.sbuf("cT", [128, 8], F32)
        self.scT = S.sbuf("scT", [128, 8, 128], F32)
        S.dma("sp", lambda: nc.sync.dma_start(out=cT[:], in_=self.c.rearrange("o (k p) -> p (o k)", p=128), allow_slow_non_contiguous=True), writes=[cT])
        S.op("act", lambda: nc.scalar.activation(out=cT[:], in_=cT[:], func=AF.Silu), reads=[cT], writes=[cT])
        S.op("dve", lambda: nc.vector.tensor_copy(out=self.scT[:], in_=cT[:].unsqueeze(2).to_broadcast([128, 8, 128])), reads=[cT], writes=[self.scT])
        self._imod = 0
        self.SH = S.sbuf("SH", [128, D], F32)
        self.G = S.sbuf("G", [128, D], F32)
        self.GATE = S.sbuf("GATE", [128, D], F32)
        self.xt = [S.sbuf("xt%d" % i, [128, D], F32) for i in range(2)]
        self.junk = S.sbuf("junk", [128, D], BF16)
        self.t32 = [S.sbuf("t32_%d" % i, [128, D], F32) for i in range(2)]
        self.rowrep = self.t32[0]
        self.small = {}

    def al(self, name, shape, dt):
        esz = 2 if dt == BF16 else 4
        n = 1
        for s in shape[1:]:
            n *= s
        nbytes = n * esz
        off = self.aoff
        self.aoff += (nbytes + 31) // 32 * 32
        assert self.aoff <= self.ARENA, (name, self.aoff)
        ap = self.arena.t[0:shape[0], off // 2:(off + nbytes) // 2]
        if dt != BF16:
            ap = ap.bitcast(dt)
        if len(shape) == 3:
            ap = ap.rearrange("p (a b) -> p a b", a=shape[1])
        return Buf(name, ap)

    def arelease(self, mark=0):
        self.S.barrier()
        self.aoff = mark

    def sm(self, name, shape, dt=F32):
        if name not in self.small:
            self.small[name] = self.S.sbuf(name, shape, dt)
        return self.small[name]

    def mod_half(self, l, half, norm_g):
        S, nc = self.S, self.nc
        dsts = [self.SH, self.G, self.GATE]
        mark = self.aoff
        modw = [self.al("modw%d" % i, [128, 8, 512], F32) for i in range(2)]
        modbias = [self.al("modbias%d" % i, [128, 512], F32) for i in range(2)]
        S.dma("sp", lambda: nc.sync.dma_start(out=self.rowrep[:], in_=norm_g[l, :].partition_broadcast(128)), writes=[self.rowrep])
        for j in range(6):
            col = half * 3072 + j * 512
            wb = modw[self._imod % 2]
            bb = modbias[self._imod % 2]
            self._imod += 1
            S.dma("sp", lambda wb=wb, col=col: nc.sync.dma_start(out=wb[:], in_=self.mod_w[l, :, col:col + 512].rearrange("(k p) n -> p k n", p=128)), writes=[wb])
            S.dma("sp", lambda bb=bb, col=col: nc.sync.dma_start(out=bb[:], in_=self.mod_b[l, col:col + 512].partition_broadcast(128)), writes=[bb])
            p = self.ps()

            def mm(p=p, wb=wb):
                for k in range(8):
                    i = nc.tensor.matmul(out=p[:], lhsT=self.scT[:, k, :], rhs=wb[:, k, :], start=(k == 0), stop=(k == 7))
                return i
            S.op("pe", mm, reads=[self.scT, wb], writes=[p])
            dst = dsts[j // 2]
            sl = slice((j % 2) * 512, (j % 2) * 512 + 512)
            S.op("dve", lambda p=p, bb=bb, dst=dst, sl=sl: nc.vector.tensor_tensor(out=dst[:, sl], in0=p[:], in1=bb[:], op=ALU.add), reads=[p, bb], writes=[dst])
        S.op("dve", lambda: nc.vector.scalar_tensor_tensor(out=self.G[:], in0=self.G[:], scalar=1.0, in1=self.rowrep[:], op0=ALU.add, op1=ALU.mult), reads=[self.G, self.rowrep], writes=[self.G])
        self.arelease(mark)

    def load_w(self, dst, src_rows_cols, dst_sl=None):
        S, nc = self.S, self.nc
        n = src_rows_cols.shape[1]
        o0 = 0 if dst_sl is None else dst_sl
        for c0 in range(0, n, 512):
            c1 = min(n, c0 + 512)
            S.dma("pool", lambda c0=c0, c1=c1: nc.gpsimd.dma_start(out=dst[:, :, o0 + c0:o0 + c1], in_=src_rows_cols[:, c0:c1].rearrange("(k p) n -> p k n", p=128)), writes=[dst], part=True)

    def rstd_from_ss(self, ss, n, rstd):
        S, nc = self.S, self.nc
        S.op("act", lambda: nc.scalar.activation(out=rstd[:], in_=ss[:], func=AF.Sqrt, scale=1.0 / n, bias=EPS), reads=[ss], writes=[rstd])
        S.op("dve", lambda: nc.vector.reciprocal(out=rstd[:], in_=rstd[:]), reads=[rstd], writes=[rstd])

    def norm_mod(self, xt, hout, key):
        S, nc = self.S, self.nc
        ss = self.sm("ss_" + key, [128, 1])
        rstd = self.sm("rstd_" + key, [128, 1])
        S.op("act", lambda: nc.scalar.activation(out=self.junk[:], in_=xt[:], func=AF.Square), reads=[xt], writes=[self.junk])
        S.op("dve", lambda: nc.vector.reduce_sum(out=ss[:], in_=self.junk[:], axis=AX.X), reads=[self.junk], writes=[ss])
        self.rstd_from_ss(ss, D, rstd)
        t = self.t32[0]
        S.op("dve", lambda: nc.vector.scalar_tensor_tensor(out=t[:], in0=xt[:], scalar=rstd[:, 0:1], in1=self.G[:], op0=ALU.mult, op1=ALU.mult), reads=[xt, rstd, self.G], writes=[t])
        S.op("dve", lambda: nc.vector.tensor_tensor(out=hout[:], in0=t[:], in1=self.SH[:], op=ALU.add), reads=[t, self.SH], writes=[hout])

    def transpose8(self, src, dst, n=8):
        S, nc = self.S, self.nc
        p = self.pb()

        def tr():
            for k in range(n):
                i = nc.tensor.transpose(out=p[:, k * 128:(k + 1) * 128], in_=src[:, k * 128:(k + 1) * 128], identity=self.ident[:])
            return i
        S.op("pe", tr, reads=[src, self.ident], writes=[p])
        S.op("act", lambda: nc.scalar.copy(out=dst[:, 0:n * 128], in_=p[:, 0:n * 128]), reads=[p], writes=[dst])

    def transpose_strided(self, src, dst, n):
        S, nc = self.S, self.nc
        p = self.pb()

        def tr():
            for j in range(n):
                i = nc.tensor.transpose(out=p[:, j * 128:(j + 1) * 128], in_=src[:, j:n * 128:n], identity=self.ident[:])
            return i
        S.op("pe", tr, reads=[src, self.ident], writes=[p])
        S.op("act", lambda: nc.scalar.copy(out=dst[:, 0:n * 128], in_=p[:, 0:n * 128]), reads=[p], writes=[dst])

    def mm_flat(self, lhsT, W, rowlen, col, out_ps, nk):
        nc = self.nc

        def mm():
            for j in range(nk):
                i = nc.tensor.matmul(out=out_ps[:], lhsT=lhsT[:, j * 128:(j + 1) * 128], rhs=W[:, j * rowlen + col:j * rowlen + col + 512], start=(j == 0), stop=(j == nk - 1))
            return i
        self.S.op("pe", mm, reads=[lhsT, W], writes=[out_ps])

    def mm_tok(self, lhsT, W, col, out_ps, nk=8):
        nc = self.nc

        def mm():
            for k in range(nk):
                i = nc.tensor.matmul(out=out_ps[:], lhsT=lhsT[:, k * 128:(k + 1) * 128], rhs=W[:, k, col:col + 512], start=(k == 0), stop=(k == nk - 1))
            return i
        self.S.op("pe", mm, reads=[lhsT, W], writes=[out_ps])

    def out_proj_residual(self, yb, xin, gate_first, xout_dram, B_out, i):
        S, nc = self.S, self.nc
        yT = self.sm("yT", [128, D], BF16)
        self.transpose8(yb, yT)
        xo = self.t32[1]
        for half in range(2):
            p = self.ps()
            self.mm_tok(yT, self.WO, half * 512, p)
            sl = slice(half * 512, half * 512 + 512)
            S.op("dve", lambda p=p, sl=sl: nc.vector.tensor_tensor(out=xo[:, sl], in0=p[:], in1=self.GATE[:, sl], op=ALU.mult), reads=[p, self.GATE], writes=[xo])
        S.op("pool", lambda: nc.gpsimd.tensor_tensor(out=xo[:], in0=xo[:], in1=xin[:], op=ALU.add), reads=[xo, xin], writes=[xo])
        S.dma("sp", lambda: nc.sync.dma_start(out=xout_dram[i * 128:(i + 1) * 128, :], in_=xo[:]), reads=[xo], writes=[B_out[i]])

    def pass_a(self, l, xin, B_xin, xout, B_xout):
        S, nc = self.S, self.nc
        NT = self.NT
        WA = self.al("WA", [128, 8, 4112], BF16)
        self.W2 = self.al("W2", [128, 8, D], BF16)
        self.WO = self.al("WO", [128, 8, D], BF16)
        wi = self.w_in[l]
        self.load_w(WA, wi[:, 0:2048], 0)
        self.load_w(WA, wi[:, 2064:3088], 2048)
        self.load_w(WA, wi[:, 6160:7184], 3072)
        self.load_w(WA, wi[:, 2048:2064], 4096)
        self.load_w(self.W2, self.w_gla_out[l])
        self.load_w(self.WO, self.w_out[l])
        cq, ck, cv, cr, cga, cgd = 0, 512, 1024, 2048, 3072, 4096
        gw = self.al("gw", [32, 512], F32)
        S.op("dve", lambda: nc.vector.memset(gw[:], 0.0), writes=[gw])
        S.dma("sp", lambda: nc.sync.dma_start(out=gw[0:16, :], in_=self.gate_w2[l]), writes=[gw])
        S.dma("sp", lambda: nc.sync.dma_start(out=gw[16:17, :], in_=self.gate_b[l:l + 1, :]), writes=[gw], part=True)
        gng = self.al("gng", [128, D], F32)
        S.dma("sp", lambda: nc.sync.dma_start(out=gng[:], in_=self.gla_norm_g[l, :].partition_broadcast(128)), writes=[gng])
        St = self.al("St", [128, 4, 256], F32)
        Sb = self.al("Sb", [128, 4, 256], BF16)
        S.op("dve", lambda: nc.vector.memset(St[:], 0.0), writes=[St])
        S.op("dve", lambda: nc.vector.memset(Sb[:], 0.0), writes=[Sb])
        gda = [self.al("gda%d" % j, [32, 128], F32) for j in range(2)]
        for j in range(2):
            S.op("dve", lambda j=j: nc.vector.memset(gda[j][:], 0.0), writes=[gda[j]])
            S.dma("sp", lambda j=j: nc.sync.dma_start(out=gda[j][16:17, :], in_=self.ones[0:1, :]), reads=[self.ones], writes=[gda[j]])
        hb = self.al("hb", [128, D], BF16)
        hT = [self.al("hT%d" % j, [128, D], BF16) for j in range(2)]
        xts = [self.xt[0], self.xt[1], self.al("xt2", [128, D], F32)]
        qraw = [self.al("qraw%d" % j, [128, 512], F32) for j in range(2)]
        kraw = [self.al("kraw%d" % j, [128, 512], F32) for j in range(2)]
        ktok = [self.al("ktok%d" % j, [128, 512], BF16) for j in range(2)]
        vb = [self.al("vb%d" % j, [128, D], BF16) for j in range(2)]
        sr = [self.al("sr%d" % j, [128, D], BF16) for j in range(2)]
        sga = [self.al("sga%d" % j, [128, D], BF16) for j in range(2)]
        sp_ = self.al("sp_", [128, 512], F32)
        Eb = self.al("Eb", [128, 512], F32)
        Enb = self.al("Enb", [128, 512], F32)
        Es = self.al("Es", [128, 512], F32)
        qTt = self.al("qTt", [128, 512], BF16)
        kTt = self.al("kTt", [128, 512], BF16)
        ks = self.al("ks", [128, 512], BF16)
        am = self.al("am", [128, 512], BF16)
        on = self.al("on", [128, D], BF16)
        og = self.al("og", [128, D], BF16)
        ogT = self.al("ogT", [128, D], BF16)
        yg = self.al("yg", [128, D], BF16)
        junk2 = self.al("junk2", [128, D], BF16)
        ss4 = self.al("ss4", [128, 4], F32)
        rs4 = self.al("rs4", [128, 4], F32)
        xps = [self.PS[4], self.PS[5]]
        yps = self.PS[0:4]
        cnt = {"x": 0, "y": 0}

        def px():
            cnt["x"] += 1
            return xps[cnt["x"] % 2]

        def py():
            cnt["y"] += 1
            return yps[cnt["y"] % 4]

        def mmk(lhsT, col, p):
            def mm():
                for k in range(8):
                    ins = nc.tensor.matmul(out=p[:], lhsT=lhsT[:, k * 128:(k + 1) * 128], rhs=WA[:, k, col:col + 512], start=(k == 0), stop=(k == 7))
                return ins
            S.op("pe", mm, reads=[lhsT, WA], writes=[p])

        def fm(p, c0, h_T, m=128, nh=4):
            def f():
                for h in range(nh):
                    for k in range(8):
                        ins = nc.tensor.matmul(out=p[0:m, h * 128:(h + 1) * 128], lhsT=WA[:, k, c0 + h * m:c0 + (h + 1) * m],
                                               rhs=h_T[:, k * 128:(k + 1) * 128], start=(k == 0), stop=(k == 7))
                return ins
            S.op("pe", f, reads=[WA, h_T], writes=[p])

        def xsteps(i):
            j = i % 2
            xt, h_T = xts[i % 3], hT[j]
            st = []

            def x0a():
                S.dma("sp", lambda: nc.sync.dma_start(out=xt[:], in_=xin[i * 128:(i + 1) * 128, :]), reads=[B_xin[i]], writes=[xt])
                self.norm_mod(xt, hb, "a")
            st.append(x0a)

            def x0b():
                self.transpose8(hb, h_T)
                S.dma("sp", lambda: nc.sync.dma_start(out=self.hTd[i], in_=h_T[:]), reads=[h_T], writes=[self.B_hT[i]])
            st.append(x0b)

            def x1():
                p = px()
                fm(p, cq, h_T)
                S.op("act", lambda: nc.scalar.copy(out=qraw[j][:], in_=p[:]), reads=[p], writes=[qraw[j]])
            st.append(x1)

            def x2():
                p = px()
                fm(p, ck, h_T)
                S.op("dve", lambda: nc.vector.tensor_copy(out=kraw[j][:], in_=p[:]), reads=[p], writes=[kraw[j]])
            st.append(x2)

            def x3():
                p = px()
                fm(p, cgd, h_T, m=16, nh=1)
                S.op("act", lambda: nc.scalar.copy(out=gda[j][0:16, :], in_=p[0:16, 0:128]), reads=[p], writes=[gda[j]])
            st.append(x3)

            def x4():
                p = px()
                mmk(h_T, ck, p)
                S.op("dve", lambda: nc.vector.tensor_copy(out=ktok[j][:], in_=p[:]), reads=[p], writes=[ktok[j]])
            st.append(x4)
            for half in range(2):
                def xv(half=half):
                    p = px()
                    mmk(h_T, cv + half * 512, p)
                    S.op("act", lambda: nc.scalar.copy(out=vb[j][:, half * 512:(half + 1) * 512], in_=p[:]), reads=[p], writes=[vb[j]])
                st.append(xv)
            for half in range(2):
                def xr(half=half):
                    p = px()
                    mmk(h_T, cr + half * 512, p)
                    S.op("act", lambda: nc.scalar.activation(out=sr[j][:, half * 512:(half + 1) * 512], in_=p[:], func=AF.Silu), reads=[p], writes=[sr[j]])
                st.append(xr)
            for half in range(2):
                def xg(half=half):
                    p = px()
                    mmk(h_T, cga + half * 512, p)
                    S.op("act", lambda: nc.scalar.activation(out=sga[j][:, half * 512:(half + 1) * 512], in_=p[:], func=AF.Sigmoid), reads=[p], writes=[sga[j]])
                st.append(xg)
            return st

        def ysteps(i):
            j = i % 2
            xt = xts[i % 3]
            st = []

            def y0():
                plg = py()
                S.op("pe", lambda: nc.tensor.matmul(out=plg[:], lhsT=gda[j][:], rhs=gw[:], start=True, stop=True), reads=[gda[j], gw], writes=[plg])
                S.op("act", lambda: nc.scalar.activation(out=Es[:], in_=plg[:], func=AF.Exp, scale=-1.0), reads=[plg], writes=[Es])
                S.op("act", lambda: nc.scalar.activation(out=sp_[:], in_=Es[:], func=AF.Ln, bias=1.0), reads=[Es], writes=[sp_])
            st.append(y0)

            def y1():
                pbT, prev = py(), py()

                def cum1():
                    for h in range(4):
                        ins = nc.tensor.matmul(out=pbT[:, h * 128:(h + 1) * 128], lhsT=sp_[:, h * 128:(h + 1) * 128], rhs=self.Uinc[:], start=True, stop=True)
                    return ins
                S.op("pe", cum1, reads=[sp_, self.Uinc], writes=[pbT])
                S.op("pe", lambda: nc.tensor.matmul(out=prev[:], lhsT=self.Urev[:], rhs=sp_[:], start=True, stop=True), reads=[sp_, self.Urev], writes=[prev])
                S.op("act", lambda: nc.scalar.activation(out=Eb[:], in_=pbT[:], func=AF.Exp), reads=[pbT], writes=[Eb])
                S.op("act", lambda: nc.scalar.activation(out=Enb[:], in_=pbT[:], func=AF.Exp, scale=-1.0), reads=[pbT], writes=[Enb])
                S.op("act", lambda: nc.scalar.activation(out=Es[:], in_=prev[:], func=AF.Exp), reads=[prev], writes=[Es])
            st.append(y1)

            def y2():
                S.op("dve", lambda: nc.vector.scalar_tensor_tensor(out=qTt[:], in0=qraw[j][:], scalar=128 ** -0.5, in1=Eb[:], op0=ALU.mult, op1=ALU.mult), reads=[qraw[j], Eb], writes=[qTt])
                S.op("dve", lambda: nc.vector.tensor_tensor(out=kTt[:], in0=kraw[j][:], in1=Enb[:], op=ALU.mult), reads=[kraw[j], Enb], writes=[kTt])
                S.op("pool", lambda: nc.gpsimd.tensor_tensor(out=ks[:], in0=ktok[j][:], in1=Es[:], op=ALU.mult), reads=[ktok[j], Es], writes=[ks])
            st.append(y2)

            def y3():
                pat = py()

                def att():
                    for h in range(4):
                        ins = nc.tensor.matmul(out=pat[:, h * 128:(h + 1) * 128], lhsT=kTt[:, h * 128:(h + 1) * 128], rhs=qTt[:, h * 128:(h + 1) * 128], start=True, stop=True)
                    return ins
                S.op("pe", att, reads=[kTt, qTt], writes=[pat])
                S.op("dve", lambda: nc.vector.tensor_tensor(out=am[:], in0=pat[:], in1=self.mask4[:], op=ALU.mult), reads=[pat, self.mask4], writes=[am])
            st.append(y3)
            hold = {}

            def y4():
                po = [py(), py()]
                pds = [py(), py()]
                hold["po"] = po

                def omm():
                    for h in range(4):
                        o_ap = po[h // 2][:, (h % 2) * 256:(h % 2) * 256 + 256]
                        nc.tensor.matmul(out=o_ap, lhsT=am[:, h * 128:(h + 1) * 128], rhs=vb[j][:, h * 256:(h + 1) * 256], start=True, stop=False)
                        ins = nc.tensor.matmul(out=o_ap, lhsT=qTt[:, h * 128:(h + 1) * 128], rhs=Sb[:, h, :], start=False, stop=True)
                    return ins
                S.op("pe", omm, reads=[am, vb[j], qTt, Sb], writes=po)

                def dsm():
                    for h in range(4):
                        ins = nc.tensor.matmul(out=pds[h // 2][:, (h % 2) * 256:(h % 2) * 256 + 256], lhsT=ks[:, h * 128:(h + 1) * 128], rhs=vb[j][:, h * 256:(h + 1) * 256], start=True, stop=True)
                    return ins
                S.op("pe", dsm, reads=[ks, vb[j]], writes=pds)
                for h in range(4):
                    S.op("dve", lambda h=h: nc.vector.scalar_tensor_tensor(out=St[:, h, :], in0=St[:, h, :], scalar=Eb[:, h * 128 + 127:h * 128 + 128],
                                                                            in1=pds[h // 2][:, (h % 2) * 256:(h % 2) * 256 + 256], op0=ALU.mult, op1=ALU.add),
                         reads=[St, Eb, pds[h // 2]], writes=[St])
                S.op("act", lambda: nc.scalar.copy(out=Sb[:], in_=St[:]), reads=[St], writes=[Sb])
            st.append(y4)

            def y5():
                po = hold["po"]
                for hh in range(2):
                    S.op("act", lambda hh=hh: nc.scalar.activation(out=junk2[:, hh * 512:(hh + 1) * 512], in_=po[hh][:], func=AF.Square), reads=[po[hh]], writes=[junk2])
                S.op("dve", lambda: nc.vector.reduce_sum(out=ss4[:], in_=junk2[:].rearrange("p (h v) -> p h v", h=4), axis=AX.X), reads=[junk2], writes=[ss4])
                self.rstd_from_ss(ss4, 256, rs4)
                for h in range(4):
                    S.op("dve", lambda h=h: nc.vector.scalar_tensor_tensor(out=on[:, h * 256:(h + 1) * 256], in0=po[h // 2][:, (h % 2) * 256:(h % 2) * 256 + 256], scalar=rs4[:, h:h + 1],
                                                                            in1=gng[:, h * 256:(h + 1) * 256], op0=ALU.mult, op1=ALU.mult), reads=[po[h // 2], rs4, gng], writes=[on])
                S.op("dve", lambda: nc.vector.tensor_tensor(out=og[:], in0=on[:], in1=sr[j][:], op=ALU.mult), reads=[on, sr[j]], writes=[og])
            st.append(y5)

            def y6a():
                self.transpose8(og, ogT)
            st.append(y6a)

            def y6b():
                for half in range(2):
                    p = py()
                    self.mm_tok(ogT, self.W2, half * 512, p)
                    sl = slice(half * 512, half * 512 + 512)
                    S.op("dve", lambda p=p, sl=sl: nc.vector.tensor_tensor(out=yg[:, sl], in0=p[:], in1=sga[j][:, sl], op=ALU.mult), reads=[p, sga[j]], writes=[yg])
            st.append(y6b)

            yT = self.sm("yT", [128, D], BF16)

            def y7a():
                self.transpose8(yg, yT)
            st.append(y7a)

            def y7b():
                xo = self.t32[1]
                for half in range(2):
                    p = py()
                    self.mm_tok(yT, self.WO, half * 512, p)
                    sl = slice(half * 512, half * 512 + 512)
                    S.op("dve", lambda p=p, sl=sl: nc.vector.tensor_tensor(out=xo[:, sl], in0=p[:], in1=self.GATE[:, sl], op=ALU.mult), reads=[p, self.GATE], writes=[xo])
                S.op("pool", lambda: nc.gpsimd.tensor_tensor(out=xo[:], in0=xo[:], in1=xt[:], op=ALU.add), reads=[xo, xt], writes=[xo])
                S.dma("sp", lambda: nc.sync.dma_start(out=xout[i * 128:(i + 1) * 128, :], in_=xo[:]), reads=[xo], writes=[B_xout[i]])
            st.append(y7b)
            return st

        Y = [ysteps(i) for i in range(NT)]
        X = [xsteps(i) for i in range(NT)]
        for f in X[0]:
            f()
        for i in range(-1, NT):
            a, bq, c = i, i + 1, i + 2
            ha, hb_, hc = (a >= 0), (bq < NT), (c < NT)
            if hc:
                X[c][0]()
            if ha:
                Y[a][6]()
            if hb_:
                Y[bq][0]()
            if ha:
                Y[a][7]()
            if hb_:
                Y[bq][1]()
            if ha:
                Y[a][8]()
                Y[a][9]()
            if hc:
                X[c][1]()
            if hb_:
                Y[bq][2]()
                Y[bq][3]()
            if hc:
                X[c][2]()
                X[c][3]()
            if hb_:
                Y[bq][4]()
                Y[bq][5]()
            if hc:
                for f in X[c][4:]:
                    f()

    def pass_b(self, l, xin, B_xin, xout, B_xout):
        S, nc = self.S, self.nc
        WB = self.al("WB", [128, 8, 4096], BF16)
        self.W2 = self.al("W2", [128, 8, D], BF16)
        self.WO = self.al("WO", [128, 8, D], BF16)
        self.load_w(self.WO, self.w_out[l])
        wi = self.w_in[l]
        self.load_w(WB, wi[:, 3088:6160], 0)
        self.load_w(WB, wi[:, 7184:8208], 3072)
        self.load_w(self.W2, self.w_conv_out[l])
        cw = self.al("cw", [128, 8, 3], F32)
        for j in range(3):
            S.dma("sp", lambda j=j: nc.sync.dma_start(out=cw[:, :, j:j + 1], in_=self.conv_w[l, j:j + 1, :].rearrange("o (c p) -> p c o", p=128), allow_slow_non_contiguous=True), writes=[cw], part=True)
        U = self.al("U", [128, 8, 514], F32)
        S.op("dve", lambda: nc.vector.memset(U[:], 0.0), writes=[U])
        hT4 = self.al("hT4", [128, 4, D], BF16)
        ccs = self.al("ccs", [128, 512], F32)
        acc = self.al("acc", [128, 512], F32)
        PT = self.al("PT", [128, 8, 512], BF16)
        sgc = self.al("sgc", [128, D], BF16)
        yc = self.al("yc", [128, D], BF16)
        nst = self.NT // 4
        for s in range(nst):
            for t in range(4):
                i = s * 4 + t
                S.dma("sp", lambda t=t, i=i: nc.sync.dma_start(out=hT4[:, t, :], in_=self.hTd[i]), reads=[self.B_hT[i]], writes=[hT4], part=True)
            for cc in range(8):
                pcs = []
                for br in range(3):
                    p = self.ps()
                    c0 = br * 1024 + cc * 128

                    def f(p=p, c0=c0):
                        for k in range(8):
                            ins = nc.tensor.matmul(out=p[:].rearrange("p (t n) -> p t n", t=4), lhsT=WB[:, k, c0:c0 + 128],
                                                   rhs=hT4[:, :, k * 128:(k + 1) * 128], start=(k == 0), stop=(k == 7))
                        return ins
                    S.op("pe", f, reads=[WB, hT4], writes=[p])
                    pcs.append(p)
                pcb, pcc, pch = pcs
                S.op("act", lambda pcc=pcc: nc.scalar.copy(out=ccs[:], in_=pcc[:]), reads=[pcc], writes=[ccs])
                S.op("dve", lambda pch=pch, cc=cc: nc.vector.tensor_tensor(out=U[:, cc, 2:514], in0=pch[:], in1=ccs[:], op=ALU.mult), reads=[pch, ccs], writes=[U])
                S.op("dve", lambda cc=cc: nc.vector.tensor_scalar(out=acc[:], in0=U[:, cc, 2:514], scalar1=cw[:, cc, 2:3], scalar2=None, op0=ALU.mult), reads=[U, cw], writes=[acc])
                S.op("dve", lambda cc=cc: nc.vector.scalar_tensor_tensor(out=acc[:], in0=U[:, cc, 1:513], scalar=cw[:, cc, 1:2], in1=acc[:], op0=ALU.mult, op1=ALU.add), reads=[U, cw, acc], writes=[acc])
                S.op("dve", lambda cc=cc: nc.vector.scalar_tensor_tensor(out=acc[:], in0=U[:, cc, 0:512], scalar=cw[:, cc, 0:1], in1=acc[:], op0=ALU.mult, op1=ALU.add), reads=[U, cw, acc], writes=[acc])
                S.op("dve", lambda pcb=pcb, cc=cc: nc.vector.tensor_tensor(out=PT[:, cc, :], in0=pcb[:], in1=acc[:], op=ALU.mult), reads=[pcb, acc], writes=[PT])
                S.op("pool", lambda cc=cc: nc.gpsimd.tensor_copy(out=U[:, cc, 0:2], in_=U[:, cc, 512:514]), reads=[U], writes=[U])
            for t in range(4):
                i = s * 4 + t
                xt = self.xt[i % 2]
                S.dma("sp", lambda xt=xt, i=i: nc.sync.dma_start(out=xt[:], in_=xin[i * 128:(i + 1) * 128, :]), reads=[B_xin[i]], writes=[xt])
                for half in range(2):
                    p = self.ps()

                    def g(p=p, half=half, t=t):
                        for k in range(8):
                            ins = nc.tensor.matmul(out=p[:], lhsT=hT4[:, t, k * 128:(k + 1) * 128], rhs=WB[:, k, 3072 + half * 512:3072 + half * 512 + 512], start=(k == 0), stop=(k == 7))
                        return ins
                    S.op("pe", g, reads=[hT4, WB], writes=[p])
                    S.op("act", lambda p=p, half=half: nc.scalar.activation(out=sgc[:, half * 512:(half + 1) * 512], in_=p[:], func=AF.Sigmoid), reads=[p], writes=[sgc])
                for half in range(2):
                    p = self.ps()

                    def y(p=p, half=half, t=t):
                        for cc in range(8):
                            ins = nc.tensor.matmul(out=p[:], lhsT=PT[:, cc, t * 128:(t + 1) * 128], rhs=self.W2[:, cc, half * 512:half * 512 + 512], start=(cc == 0), stop=(cc == 7))
                        return ins
                    S.op("pe", y, reads=[PT, self.W2], writes=[p])
                    sl = slice(half * 512, half * 512 + 512)
                    S.op("dve", lambda p=p, sl=sl: nc.vector.tensor_tensor(out=yc[:, sl], in0=p[:], in1=sgc[:, sl], op=ALU.mult), reads=[p, sgc], writes=[yc])
                self.out_proj_residual(yc, xt, None, xout, B_xout, i)

    def moe(self, l, xin, B_xin, xout, B_xout, final):
        S, nc = self.S, self.nc
        NT = self.NT
        rw = self.al("rw", [128, 8, 72], F32)
        S.dma("sp", lambda: nc.sync.dma_start(out=rw[:, :, 0:8], in_=self.rg_w[l].rearrange("(k p) n -> p k n", p=128)), writes=[rw])
        S.dma("sp", lambda: nc.sync.dma_start(out=rw[:, :, 8:72], in_=self.re_w[l].rearrange("(k p) n -> p k n", p=128)), writes=[rw], part=True)
        rb = self.al("rb", [128, 72], F32)
        S.dma("sp", lambda: nc.sync.dma_start(out=rb[:, 0:8], in_=self.rg_b[l, :].partition_broadcast(128)), writes=[rb])
        S.dma("sp", lambda: nc.sync.dma_start(out=rb[:, 8:72], in_=self.re_b[l, :].partition_broadcast(128)), writes=[rb], part=True)
        CUM = self.al("CUM", [128, NTILE, 64], F32)
        OH1 = self.al("OH1", [128, NTILE, 64], F32)
        OH2 = self.al("OH2", [128, NTILE, 64], F32)
        W12 = self.al("W12", [128, NTILE, 2], F32)
        run = self.al("run", [128, 64], F32)
        S.op("dve", lambda: nc.vector.memset(run[:], 0.0), writes=[run])
        S.op("dve", lambda: nc.vector.memset(CUM[:], 0.0), writes=[CUM])
        S.op("dve", lambda: nc.vector.memset(OH1[:], 0.0), writes=[OH1])
        S.op("dve", lambda: nc.vector.memset(OH2[:], 0.0), writes=[OH2])
        gm = self.al("gm", [128, 8], F32)
        ohg = self.al("ohg", [128, 8], F32)
        pen = self.al("pen", [128, 8], F32)
        msk = self.al("msk", [128, 64], F32)
        top8 = self.al("top8", [128, 8], F32)
        m01 = self.al("m01", [128, 64], F32)
        gexp = self.al("gexp", [128, 8], F32)
        cnti = self.al("cnti", [128, 64], I32)
        padf = self.al("padf", [128, 64], F32)
        pend = self.al("pend", [128, 64], F32)
        adj = self.al("adj", [128, 64], F32)
        big = self.al("big", [128, NTILE, 64], F32)
        Df = self.al("Df", [128, NTILE, 2], F32)
        Di = self.al("Di", [128, NTILE * 2], I32)
        thr = self.al("thr", [128, NBLK], F32)
        thri = self.al("thri", [128, NBLK], I32)
        bef = self.al("bef", [128, NBLK], F32)
        WI = [self.al("WI%d" % j, [128, NBLK], I32) for j in range(2)]
        vld = self.al("vld", [128, NBLK], F32)
        mark_h2 = self.aoff
        H2 = self.al("H2", [128, NTILE, D], BF16)
        h2s = [self.al("h2_%d" % j, [128, D], F32) for j in range(2)]
        h2Ts = [self.al("h2T%d" % j, [128, D], F32) for j in range(2)]
        LgA = self.al("LgA", [128, NTILE, 72], F32)
        S.op("dve", lambda: nc.vector.memset(LgA[:], 0.0), writes=[LgA])
        gmx = self.al("gmx", [128, NTILE], F32)
        gsm = self.al("gsm", [128, NTILE], F32)
        ohgA = self.al("ohgA", [128, NTILE, 8], F32)
        gshA = self.al("gshA", [128, NTILE, 8], F32)
        mskA = self.al("mskA", [128, NTILE, 64], F32)
        top8A = self.al("top8A", [128, NTILE, 8], F32)
        PRE = self.al("PRE", [128, NTILE, 64], F32)

        def rxa(i):
            xt = self.xt[i % 2]
            h2 = h2s[i % 2]
            S.dma("sp", lambda xt=xt, i=i: nc.sync.dma_start(out=xt[:], in_=xin[i * 128:(i + 1) * 128, :]), reads=[B_xin[i]], writes=[xt])
            self.norm_mod(xt, h2, "m")
            S.op("act", lambda i=i: nc.scalar.copy(out=H2[:, i, :], in_=h2[:]), reads=[h2], writes=[H2])

        def rxb(i):
            h2, h2T = h2s[i % 2], h2Ts[i % 2]
            for half in range(2):
                p = self.ps()

                def tr(p=p, half=half):
                    for k in range(4):
                        kk = half * 4 + k
                        ins = nc.tensor.transpose(out=p[:, k * 128:(k + 1) * 128], in_=h2[:, kk * 128:(kk + 1) * 128], identity=self.identf[:])
                    return ins
                S.op("pe", tr, reads=[h2, self.identf], writes=[p])
                S.op("act", lambda p=p, half=half: nc.scalar.copy(out=h2T[:, half * 512:(half + 1) * 512], in_=p[:]), reads=[p], writes=[h2T])
            pl = self.ps()

            def rmm(pl=pl):
                for k in range(8):
                    ins = nc.tensor.matmul(out=pl[:, 0:72], lhsT=h2T[:, k * 128:(k + 1) * 128], rhs=rw[:, k, :], start=(k == 0), stop=(k == 7))
                return ins
            S.op("pe", rmm, reads=[h2T, rw], writes=[pl])
            S.op("dve", lambda pl=pl, i=i: nc.vector.tensor_tensor(out=LgA[:, i, :], in0=pl[:, 0:72], in1=rb[:], op=ALU.add), reads=[pl, rb], writes=[LgA])

        rxa(0)
        for i in range(NT):
            if i + 1 < NT:
                rxa(i + 1)
            rxb(i)
        T_ = NTILE
        LgG = LgA[:, :, 0:8]
        S.op("dve", lambda: nc.vector.reduce_max(out=gmx[:], in_=LgG, axis=AX.X), reads=[LgA], writes=[gmx])
        S.op("dve", lambda: nc.vector.tensor_tensor(out=ohgA[:], in0=LgG, in1=gmx[:].unsqueeze(2).to_broadcast([128, T_, 8]), op=ALU.is_equal), reads=[LgA, gmx], writes=[ohgA])
        S.op("dve", lambda: nc.vector.tensor_tensor(out=gshA[:], in0=LgG, in1=gmx[:].unsqueeze(2).to_broadcast([128, T_, 8]), op=ALU.subtract), reads=[LgA, gmx], writes=[gshA])
        S.op("act", lambda: nc.scalar.activation(out=gshA[:], in_=gshA[:], func=AF.Exp), reads=[gshA], writes=[gshA])
        S.op("dve", lambda: nc.vector.reduce_sum(out=gsm[:], in_=gshA[:], axis=AX.X), reads=[gshA], writes=[gsm])
        S.op("dve", lambda: nc.vector.reciprocal(out=gsm[:], in_=gsm[:]), reads=[gsm], writes=[gsm])
        S.op("dve", lambda: nc.vector.tensor_scalar(out=ohgA[:], in0=ohgA[:], scalar1=-1.0, scalar2=1e30, op0=ALU.add, op1=ALU.mult), reads=[ohgA], writes=[ohgA])
        S.op("dve", lambda: nc.vector.tensor_tensor(out=mskA[:].rearrange("p t (g j) -> p t g j", g=8), in0=LgA[:, :, 8:72].rearrange("p t (g j) -> p t g j", g=8),
                                                     in1=ohgA[:].unsqueeze(3).to_broadcast([128, T_, 8, 8]), op=ALU.add), reads=[LgA, ohgA], writes=[mskA])
        for i in range(NT):
            S.op("dve", lambda i=i: nc.vector.max(out=top8A[:, i, :], in_=mskA[:, i, :]), reads=[mskA], writes=[top8A])
        S.op("dve", lambda: nc.vector.tensor_tensor(out=OH1[:], in0=mskA[:], in1=top8A[:, :, 0:1].to_broadcast([128, T_, 64]), op=ALU.is_equal), reads=[mskA, top8A], writes=[OH1])
        S.op("dve", lambda: nc.vector.tensor_tensor(out=OH2[:], in0=mskA[:], in1=top8A[:, :, 1:2].to_broadcast([128, T_, 64]), op=ALU.is_equal), reads=[mskA, top8A], writes=[OH2])
        S.op("dve", lambda: nc.vector.tensor_tensor(out=gmx[:], in0=top8A[:, :, 0], in1=top8A[:, :, 1], op=ALU.subtract), reads=[top8A], writes=[gmx])
        S.op("act", lambda: nc.scalar.activation(out=gmx[:], in_=gmx[:], func=AF.Sigmoid), reads=[gmx], writes=[gmx])
        S.op("dve", lambda: nc.vector.tensor_tensor(out=W12[:, :, 0], in0=gmx[:], in1=gsm[:], op=ALU.mult), reads=[gmx, gsm], writes=[W12])
        S.op("dve", lambda: nc.vector.tensor_tensor(out=W12[:, :, 1], in0=gsm[:], in1=W12[:, :, 0], op=ALU.subtract), reads=[gsm, W12], writes=[W12])
        S.op("dve", lambda: nc.vector.tensor_tensor(out=big[:], in0=OH1[:], in1=OH2[:], op=ALU.add), reads=[OH1, OH2], writes=[big])
        S.op("dve", lambda: nc.vector.memset(PRE[:, 0, :], 0.0), writes=[PRE])
        for i in range(1, NT):
            S.op("dve", lambda i=i: nc.vector.tensor_tensor(out=PRE[:, i, :], in0=PRE[:, i - 1, :], in1=big[:, i - 1, :], op=ALU.add), reads=[PRE, big], writes=[PRE])
        S.op("dve", lambda: nc.vector.tensor_tensor(out=run[:], in0=PRE[:, NT - 1, :], in1=big[:, NT - 1, :], op=ALU.add), reads=[PRE, big], writes=[run])
        for i in range(NT):
            pc = self.ps()

            def cmm(pc=pc, i=i):
                nc.tensor.matmul(out=pc[:, 0:64], lhsT=self.Ltri[:], rhs=big[:, i, :], start=True, stop=False)
                return nc.tensor.matmul(out=pc[:, 0:64], lhsT=self.ones[:], rhs=PRE[:, i, :], start=False, stop=True)
            S.op("pe", cmm, reads=[self.Ltri, self.ones, big, PRE], writes=[pc])
            S.op("act", lambda pc=pc, i=i: nc.scalar.copy(out=CUM[:, i, :], in_=pc[:, 0:64]), reads=[pc], writes=[CUM])
        pct = self.ps()
        S.op("pe", lambda: nc.tensor.matmul(out=pct[:, 0:64], lhsT=self.ones[:], rhs=run[:], start=True, stop=True), reads=[self.ones, run], writes=[pct])
        S.op("dve", lambda: nc.vector.tensor_scalar(out=cnti[:], in0=pct[:, 0:64], scalar1=float(SROWS - 1), scalar2=None, op0=ALU.add), reads=[pct], writes=[cnti])
        S.op("dve", lambda: nc.vector.tensor_scalar(out=cnti[:], in0=cnti[:], scalar1=8, scalar2=8, op0=ALU.arith_shift_right, op1=ALU.logical_shift_left), reads=[cnti], writes=[cnti])
        S.op("dve", lambda: nc.vector.tensor_copy(out=padf[:], in_=cnti[:]), reads=[cnti], writes=[padf])
        S.op("dve", lambda: nc.vector.tensor_tensor_scan(out=pend[:], data0=self.ones[:, 0:64], data1=padf[:], initial=0.0, op0=ALU.mult, op1=ALU.add), reads=[self.ones, padf], writes=[pend])
        S.op("dve", lambda: nc.vector.scalar_tensor_tensor(out=adj[:], in0=pend[:], scalar=-1.0, in1=padf[:], op0=ALU.add, op1=ALU.subtract), reads=[pend, padf], writes=[adj])
        S.op("dve", lambda: nc.vector.tensor_tensor(out=CUM[:], in0=CUM[:], in1=adj[:].unsqueeze(1).to_broadcast([128, NTILE, 64]), op=ALU.add), reads=[CUM, adj], writes=[CUM])
        for k, OH in enumerate((OH1, OH2)):
            S.op("dve", lambda OH=OH: nc.vector.tensor_tensor(out=big[:], in0=CUM[:], in1=OH[:], op=ALU.mult), reads=[CUM, OH], writes=[big])
            S.op("dve", lambda k=k: nc.vector.reduce_sum(out=Df[:, :, k], in_=big[:], axis=AX.X), reads=[big], writes=[Df])
        S.op("dve", lambda: nc.vector.tensor_copy(out=Di[:], in_=Df[:].rearrange("p t k -> p (t k)")), reads=[Df], writes=[Di])
        self.tap("Di", Di)
        self.tap("W12", W12, W12[:].rearrange("p t k -> p (t k)"))
        S.op("pool", lambda: nc.gpsimd.iota(thri[:], pattern=[[SROWS, NBLK]], base=0, channel_multiplier=0), writes=[thri])
        S.op("dve", lambda: nc.vector.tensor_copy(out=thr[:], in_=thri[:]), reads=[thri], writes=[thr])
        for q4 in range(NBLK // NTILE):
            bsl = slice(q4 * NTILE, (q4 + 1) * NTILE)
            S.op("dve", lambda bsl=bsl: nc.vector.tensor_tensor(out=big[:], in0=pend[:].unsqueeze(1).to_broadcast([128, NTILE, 64]),
                                                            in1=thr[:, bsl].unsqueeze(2).to_broadcast([128, NTILE, 64]), op=ALU.is_le), reads=[pend, thr], writes=[big])
            S.op("dve", lambda bsl=bsl: nc.vector.reduce_sum(out=bef[:, bsl], in_=big[:], axis=AX.X), reads=[big], writes=[bef])
        S.op("dve", lambda: nc.vector.tensor_scalar(out=vld[:], in0=thr[:], scalar1=pend[:, 63:64], scalar2=None, op0=ALU.is_lt), reads=[thr, pend], writes=[vld])
        S.op("pool", lambda: nc.gpsimd.iota(thri[:], pattern=[[0, NBLK]], base=0, channel_multiplier=2), reads=[thr], writes=[thri])
        S.op("dve", lambda: nc.vector.tensor_copy(out=thr[:], in_=thri[:]), reads=[thri, bef], writes=[thr])
        S.op("dve", lambda: nc.vector.tensor_scalar(out=bef[:], in0=bef[:], scalar1=63.0, scalar2=256.0, op0=ALU.min, op1=ALU.mult), reads=[bef], writes=[bef])
        S.op("dve", lambda: nc.vector.tensor_tensor(out=bef[:], in0=bef[:], in1=thr[:], op=ALU.add), reads=[bef, thr], writes=[bef])
        for kh in range(2):
            S.op("dve", lambda kh=kh: nc.vector.scalar_tensor_tensor(out=thr[:], in0=bef[:], scalar=float(l * NE * 256 + kh) - OOB_BIG, in1=vld[:], op0=ALU.add, op1=ALU.mult), reads=[bef, vld], writes=[thr])
            S.op("dve", lambda kh=kh: nc.vector.tensor_scalar(out=WI[kh][:], in0=thr[:], scalar1=OOB_BIG, scalar2=None, op0=ALU.add), reads=[thr], writes=[WI[kh]])
        self.tap("WI0", WI[0])
        for i in range(NT):
            for k in range(2):
                S.dma("pool", lambda i=i, k=k: nc.gpsimd.indirect_dma_start(out=self.xbuf[:, :], out_offset=bass.IndirectOffsetOnAxis(ap=Di[:, 2 * i + k:2 * i + k + 1], axis=0),
                                                                            in_=H2[:, i, :], in_offset=None), reads=[H2, Di], writes=[self.B_xbuf], part=True)
        self.arelease(mark_h2)
        nslot = NBLK if NT == NTILE else min(NBLK, (2 * NT * 128) // SROWS + 64)
        nblk = 2 * nslot
        order = []
        lo, hi = 0, nslot - 1
        while lo <= hi:
            order.append(lo)
            if hi != lo:
                order.append(hi)
            lo += 1
            hi -= 1

        def rows(b):
            return (2 * order[b // 2] + (b % 2)) * 128
        NW13, NW2 = 3, 3
        w1b = [self.al("w1b%d" % j, [128, 8 * DEXP], BF16) for j in range(NW13)]
        w3b = [self.al("w3b%d" % j, [128, 8 * DEXP], BF16) for j in range(NW13)]
        w2b = [self.al("w2b%d" % j, [128, 4 * D], BF16) for j in range(NW2)]
        xbk = [self.al("xbk%d" % j, [128, D], BF16) for j in range(3)]
        xbT = [self.al("xbT%d" % j, [128, D], BF16) for j in range(2)]
        s1 = [self.al("s1_%d" % j, [128, DEXP], BF16) for j in range(2)]
        gb = [self.al("gb%d" % j, [128, DEXP], BF16) for j in range(2)]
        gT = [self.al("gT%d" % j, [128, DEXP], BF16) for j in range(2)]
        yo = [self.al("yo%d" % j, [128, D], F32) for j in range(2)]
        v1 = self.ew1.rearrange("l e (p kh k4) n -> (l e p kh) (k4 n)", kh=2, k4=4)
        v3 = self.ew3.rearrange("l e (p kh k4) n -> (l e p kh) (k4 n)", kh=2, k4=4)
        v2 = self.ew2.rearrange("l e (p kh k2) n -> (l e p kh) (k2 n)", kh=2, k2=2)

        def gather_w(dst, view, b):
            for kh in range(2):
                def gth(dst=dst, view=view, kh=kh, b=b):
                    if getattr(self, "_bcreg", None) is None:
                        self._bcreg = nc.gpsimd.to_reg(DEPTH * NE * 256 - 1)
                    return nc.gpsimd.indirect_dma_start(
                        out=dst[:, kh * 2048:(kh + 1) * 2048], out_offset=None, in_=view,
                        in_offset=bass.IndirectOffsetOnAxis(ap=WI[kh][:, order[b]:order[b] + 1], axis=0),
                        bounds_check=self._bcreg, oob_is_err=False)
                S.dma("pool", gth, reads=[WI[kh]], writes=[dst], part=(kh == 1))

        pst = {}

        def ldx(b):
            xk = xbk[b % 3]
            S.dma("sp", lambda xk=xk, b=b: nc.sync.dma_start(out=xk[:], in_=self.xbuf[rows(b):rows(b) + 128, :]), reads=[self.B_xbuf], writes=[xk])

        def st1(b):
            self.transpose_strided(xbk[b % 3], xbT[b % 2], 8)

        def st2(b):
            xT = xbT[b % 2]
            p1, p3 = self.ps(), self.ps()
            self.mm_flat(xT, w1b[(b // 2) % NW13], 512, 0, p1, 8)
            self.mm_flat(xT, w3b[(b // 2) % NW13], 512, 0, p3, 8)
            s, g = s1[b % 2], gb[b % 2]
            S.op("act", lambda p1=p1, s=s: nc.scalar.activation(out=s[:], in_=p1[:], func=AF.Silu), reads=[p1], writes=[s])
            S.op("dve", lambda p3=p3, s=s, g=g: nc.vector.tensor_tensor(out=g[:], in0=p3[:], in1=s[:], op=ALU.mult), reads=[p3, s], writes=[g])

        def st3(b):
            self.transpose_strided(gb[b % 2], gT[b % 2], 4)

        def st4(b):
            y = yo[b % 2]
            for half in range(2):
                p = self.ps()
                self.mm_flat(gT[b % 2], w2b[(b // 2) % NW2], 1024, half * 512, p, 4)
                S.op("act", lambda p=p, half=half, y=y: nc.scalar.copy(out=y[:, half * 512:(half + 1) * 512], in_=p[:]), reads=[p], writes=[y])
            S.dma("sp", lambda y=y, b=b: nc.sync.dma_start(out=self.ybuf[rows(b):rows(b) + 128, :], in_=y[:]), reads=[y], writes=[self.B_ybuf], part=True)

        gather_w(w1b[0], v1, 0)
        gather_w(w3b[0], v3, 0)
        ldx(0)
        ldx(1)
        for t in range(nblk + 3):
            if t + 2 < nblk:
                ldx(t + 2)
            if t % 2 == 0:
                w = t // 2
                if w + 1 < nslot:
                    gather_w(w1b[(w + 1) % NW13], v1, w + 1)
                    gather_w(w3b[(w + 1) % NW13], v3, w + 1)
                if w < nslot:
                    gather_w(w2b[w % NW2], v2, w)
            if t < nblk:
                st1(t)
            if 0 <= t - 1 < nblk:
                st2(t - 1)
            if 0 <= t - 2 < nblk:
                st3(t - 2)
            if 0 <= t - 3 < nblk:
                st4(t - 3)
        self.arelease(mark_h2)
        RING = 4
        ya = [self.al("ya%d" % j, [128, D], F32) for j in range(RING)]
        yb_ = [self.al("yb%d" % j, [128, D], F32) for j in range(RING)]
        xc = [self.al("xc%d" % j, [128, D], F32) for j in range(3)]
        mo = [self.al("mo%d" % j, [128, D], F32) for j in range(2)]
        if final:
            S.dma("sp", lambda: nc.sync.dma_start(out=self.rowrep[:], in_=self.fng[0, :].partition_broadcast(128)), writes=[self.rowrep])

        def gat(i):
            for k, yy in enumerate((ya[i % RING], yb_[i % RING])):
                S.dma("pool", lambda yy=yy, i=i, k=k: nc.gpsimd.indirect_dma_start(out=yy[:], out_offset=None, in_=self.ybuf[:, :],
                                                                                  in_offset=bass.IndirectOffsetOnAxis(ap=Di[:, 2 * i + k:2 * i + k + 1], axis=0)),
                      reads=[self.B_ybuf, Di], writes=[yy])

        def ldc(i):
            x_ = xc[i % 3]
            S.dma("sp", lambda x_=x_, i=i: nc.sync.dma_start(out=x_[:], in_=xin[i * 128:(i + 1) * 128, :]), reads=[B_xin[i]], writes=[x_])
        for i in range(min(3, NT)):
            gat(i)
        for i in range(min(2, NT)):
            ldc(i)
        for i in range(NT):
            if i + 3 < NT:
                gat(i + 3)
            if i + 2 < NT:
                ldc(i + 2)
            m, x_, A_, B_ = mo[i % 2], xc[i % 3], ya[i % RING], yb_[i % RING]
            S.op("dve", lambda i=i, m=m, A_=A_: nc.vector.tensor_scalar(out=m[:], in0=A_[:], scalar1=W12[:, i, 0:1], scalar2=None, op0=ALU.mult), reads=[A_, W12], writes=[m])
            S.op("dve", lambda i=i, m=m, B_=B_: nc.vector.scalar_tensor_tensor(out=m[:], in0=B_[:], scalar=W12[:, i, 1:2], in1=m[:], op0=ALU.mult, op1=ALU.add), reads=[B_, W12, m], writes=[m])
            S.op("dve", lambda m=m: nc.vector.tensor_tensor(out=m[:], in0=m[:], in1=self.GATE[:], op=ALU.mult), reads=[m, self.GATE], writes=[m])
            S.op("dve", lambda m=m, x_=x_: nc.vector.tensor_tensor(out=m[:], in0=m[:], in1=x_[:], op=ALU.add), reads=[m, x_], writes=[m])
            if final:
                ss = self.sm("ss_f", [128, 1])
                rstd = self.sm("rstd_f", [128, 1])
                S.op("act", lambda m=m: nc.scalar.activation(out=self.junk[:], in_=m[:], func=AF.Square), reads=[m], writes=[self.junk])
                S.op("dve", lambda: nc.vector.reduce_sum(out=ss[:], in_=self.junk[:], axis=AX.X), reads=[self.junk], writes=[ss])
                self.rstd_from_ss(ss, D, rstd)
                S.op("dve", lambda m=m: nc.vector.scalar_tensor_tensor(out=m[:], in0=m[:], scalar=rstd[:, 0:1], in1=self.rowrep[:], op0=ALU.mult, op1=ALU.mult), reads=[m, rstd, self.rowrep], writes=[m])
            S.dma("sp", lambda i=i, m=m: nc.sync.dma_start(out=xout[i * 128:(i + 1) * 128, :], in_=m[:]), reads=[m], writes=[B_xout[i]])

    def build(self):
        cur, Bcur = self.x, self.B_x
        chain = [(self.xa, self.B_xa), (self.xb, self.B_xb), (self.xm, self.B_xm)]
        last_out = None
        for l in range(self.nlayers):
            if "A" in self.stages or "B" in self.stages:
                self.mod_half(l, 0, self.norm1_g)
            if "A" in self.stages:
                self.pass_a(l, cur, Bcur, self.xa, self.B_xa)
                self.arelease(0)
                last_out = (self.xa, self.B_xa)
            if "B" in self.stages:
                self.pass_b(l, self.xa, self.B_xa, self.xb, self.B_xb)
                self.arelease(0)
                last_out = (self.xb, self.B_xb)
            if "M" in self.stages:
                src = last_out if last_out is not None else (cur, Bcur)
                self.mod_half(l, 1, self.norm2_g)
                final = (l == self.nlayers - 1)
                dst = (self.out, self.B_out) if final else (self.xm, self.B_xm)
                self.moe(l, src[0], src[1], dst[0], dst[1], final)
                self.arelease(0)
                last_out = dst
            cur, Bcur = last_out
        self.last = last_out
        return self

    def finish(self, copy_last_to_out=False):
        S, nc = self.S, self.nc
        if copy_last_to_out and self.last[0] is not self.out:
            for i in range(self.NT):
                xt = self.xt[i % 2]
                S.dma("sp", lambda xt=xt, i=i: nc.sync.dma_start(out=xt[:], in_=self.last[0][i * 128:(i + 1) * 128, :]), reads=[self.last[1][i]], writes=[xt])
                S.dma("sp", lambda xt=xt, i=i: nc.sync.dma_start(out=self.out[i * 128:(i + 1) * 128, :], in_=xt[:]), reads=[xt], writes=[self.B_out[i]])
        S.wait_for("sp", self.B_out[:self.NT] + [self.B_dbg])
        S.emit()
        S.close()
        return nc


WEIGHT_NAMES = ["mod_w", "mod_b", "norm1_g", "w_in", "gate_w2", "gate_b", "gla_norm_g", "conv_w", "w_gla_out",
                "w_conv_out", "w_out", "norm2_g", "router_group_w", "router_group_b", "router_expert_w",
                "router_expert_b", "expert_w1", "expert_w3", "expert_w2"]


def make_in_maps(inputs, ncores=8):
    shared = {n: np.ascontiguousarray(np.asarray(inputs[n], dtype=np.float32)) for n in WEIGHT_NAMES}
    shared["final_norm_g"] = np.ascontiguousarray(np.asarray(inputs["final_norm_g"], dtype=np.float32).reshape(1, D))
    x = np.asarray(inputs["x"], dtype=np.float32)
    c = np.asarray(inputs["c"], dtype=np.float32)
    maps = []
    for b in range(ncores):
        m = dict(shared)
        m["x"] = np.ascontiguousarray(x[b])
        m["c"] = np.ascontiguousarray(c[b:b + 1])
        maps.append(m)
    return maps


def kernel(**inputs):
    nc = K().build().finish()
    res = run_bass_kernel_spmd(nc, make_in_maps(inputs), core_ids=list(range(8)))
    return np.stack([np.asarray(r["out"], dtype=np.float32) for r in res.results], axis=0)
```
